# Optimizing a Trainium2 kernel written in Bass

```python
import jax
import jax.numpy as jnp
from jax import lax
import numpy as np

D_MODEL = 2048
BATCH = 16
SEQ = 2048
DEPTH = 2

CTX_LEN = 256
GRID_W = 64
NORM_EPS = 1e-6

GLA_HEADS = 4
GLA_DK = 64
GLA_DV = 128
GLA_LOW_RANK = 16
GLA_TAU = 16.0
GLA_CHUNK = 64

SWA_Q_HEADS = 8
SWA_KV_HEADS = 2
SWA_HEAD_DIM = 64
SWA_WINDOW = 128
SWA_BLOCK = 128
ROPE_BASE = 10000.0

NA_HEADS = 8
NA_HEAD_DIM = 64
NA_KR = 8
NA_KC = 16

SGU_GROUPS = 4
SGU_GROUP_CH = 128
SGU_WIDTH = SGU_GROUPS * SGU_GROUP_CH
SGU_CHUNK = 128

N_BRANCH = 4
BRANCH_WIDTH = 512

N_EXPERTS = 16
D_FF_EXPERT = 2048
EC_CAPACITY_FACTOR = 2

IN_SIZES = (
    GLA_HEADS * GLA_DK, GLA_HEADS * GLA_DK, GLA_HEADS * GLA_DV, GLA_HEADS * GLA_DV,
    GLA_LOW_RANK, GLA_LOW_RANK,
    SWA_Q_HEADS * SWA_HEAD_DIM, SWA_KV_HEADS * SWA_HEAD_DIM, SWA_KV_HEADS * SWA_HEAD_DIM,
    NA_HEADS * NA_HEAD_DIM, NA_HEADS * NA_HEAD_DIM, NA_HEADS * NA_HEAD_DIM,
    SGU_WIDTH, SGU_WIDTH,
)
IN_COLS = sum(IN_SIZES)
IN_SPLIT_POINTS = tuple(int(p) for p in np.cumsum(IN_SIZES)[:-1])

kernel_name = 'hybrid_dit_gla_swa_natten_sgu_ecmoe'


def rms_norm(x, g):
    xf = x.astype(jnp.float32)
    y = xf * lax.rsqrt(jnp.mean(xf * xf, axis=-1, keepdims=True) + NORM_EPS)
    return (y * g.astype(jnp.float32)).astype(x.dtype)


def layer_norm(x, g, b):
    xf = x.astype(jnp.float32)
    mu = jnp.mean(xf, axis=-1, keepdims=True)
    var = jnp.mean(jnp.square(xf - mu), axis=-1, keepdims=True)
    y = (xf - mu) * lax.rsqrt(var + NORM_EPS) * g.astype(jnp.float32) + b.astype(jnp.float32)
    return y.astype(x.dtype)


def modulate(x, g, shift, scale):
    return rms_norm(x, g) * (1 + scale) + shift


def axial_rope(x):
    T, d = x.shape[1], x.shape[-1]
    quarter = d // 4
    freqs = ROPE_BASE ** (-jnp.arange(quarter, dtype=jnp.float32) / quarter)
    t = jnp.arange(T)
    row = (t // GRID_W).astype(jnp.float32)
    col = (t % GRID_W).astype(jnp.float32)
    ang = jnp.concatenate([row[:, None] * freqs, col[:, None] * freqs], axis=-1)
    cos = jnp.cos(ang)[None, :, None, :]
    sin = jnp.sin(ang)[None, :, None, :]
    x1 = x[..., : d // 2].astype(jnp.float32)
    x2 = x[..., d // 2:].astype(jnp.float32)
    return jnp.concatenate([x1 * cos - x2 * sin, x1 * sin + x2 * cos], axis=-1).astype(x.dtype)


def softmax_with_sink(logits, sink):
    sink_col = jnp.broadcast_to(sink, logits.shape[:-1] + (1,))
    return jax.nn.softmax(jnp.concatenate([logits, sink_col], axis=-1), axis=-1)[..., :-1]


def gla_scan(q, k, v, log_a, s0):
    B, T, H, _ = q.shape
    n = T // GLA_CHUNK

    def chunkify(t):
        return t.reshape(B, n, GLA_CHUNK, H, t.shape[-1]).transpose(1, 0, 3, 2, 4)

    qc, kc, vc, ac = chunkify(q), chunkify(k), chunkify(v), chunkify(log_a)
    b = jnp.cumsum(ac, axis=3)
    b_last = b[:, :, :, -1:]
    q_in = qc * jnp.exp(b)
    k_in = kc * jnp.exp(-b)
    k_to_end = kc * jnp.exp(b_last - b)
    mask = jnp.tril(jnp.ones((GLA_CHUNK, GLA_CHUNK), dtype=bool))
    attn = jnp.where(mask, jnp.einsum('nbhld,nbhmd->nbhlm', q_in, k_in), 0.0)
    intra = jnp.einsum('nbhlm,nbhmv->nbhlv', attn, vc)

    def step(s, inp):
        q_i, kte_i, v_i, bl_i = inp
        inter = jnp.einsum('bhld,bhdv->bhlv', q_i, s)
        s = s * jnp.exp(bl_i)[:, :, 0, :, None] + jnp.einsum('bhld,bhlv->bhdv', kte_i, v_i)
        return s, inter

    s_fin, inter = lax.scan(step, s0, (q_in, k_to_end, vc, b_last))
    o = (intra + inter).transpose(1, 0, 3, 2, 4).reshape(B, T, H, v.shape[-1])
    return o, s_fin


def gla_final_state(k, v, log_a):
    b = jnp.cumsum(log_a, axis=1)
    w = jnp.exp(b[:, -1:] - b)
    return jnp.einsum('bthd,bthv->bhdv', k * w, v)


def gla_heads(q, k, v, a_f, a_b, w_a2, b_a2):
    B, T, _ = q.shape
    f32 = jnp.float32
    q = q.reshape(B, T, GLA_HEADS, GLA_DK).astype(f32) * GLA_DK ** -0.5
    k = k.reshape(B, T, GLA_HEADS, GLA_DK).astype(f32)
    v = v.reshape(B, T, GLA_HEADS, GLA_DV).astype(f32)
    la_f = jax.nn.log_sigmoid((a_f @ w_a2[0] + b_a2[0]).astype(f32)).reshape(B, T, GLA_HEADS, GLA_DK) / GLA_TAU
    la_b = jax.nn.log_sigmoid((a_b @ w_a2[1] + b_a2[1]).astype(f32)).reshape(B, T, GLA_HEADS, GLA_DK) / GLA_TAU
    return q, k, v, la_f, la_b


def gla_output(o, r, g):
    B, T = o.shape[:2]
    o = o * lax.rsqrt(jnp.mean(o * o, axis=-1, keepdims=True) + NORM_EPS) * g.astype(jnp.float32)
    return (o.reshape(B, T, GLA_HEADS * GLA_DV) * jax.nn.silu(r.astype(jnp.float32))).astype(r.dtype)


def gla_mixer(pl, pc, w_a2, b_a2, g_out, need_ctx):
    ql, kl, vl, la_fl, la_bl = gla_heads(pl[0], pl[1], pl[2], pl[4], pl[5], w_a2, b_a2)
    qc, kc, vc, la_fc, la_bc = gla_heads(pc[0], pc[1], pc[2], pc[4], pc[5], w_a2, b_a2)

    def flip(t):
        return t[:, ::-1]

    y_ctx = None
    if need_ctx:
        zero = jnp.zeros((qc.shape[0], GLA_HEADS, GLA_DK, GLA_DV), jnp.float32)
        oc_f, s_f = gla_scan(qc, kc, vc, la_fc, zero)
        oc_b, s_b = gla_scan(flip(qc), flip(kc), flip(vc), flip(la_bc), zero)
        y_ctx = gla_output(oc_f + flip(oc_b), pc[3], g_out)
    else:
        s_f = gla_final_state(kc, vc, la_fc)
        s_b = gla_final_state(flip(kc), flip(vc), flip(la_bc))
    ol_f, _ = gla_scan(ql, kl, vl, la_fl, s_f)
    ol_b, _ = gla_scan(flip(ql), flip(kl), flip(vl), flip(la_bl), s_b)
    y_lat = gla_output(ol_f + flip(ol_b), pl[3], g_out)
    return y_lat, y_ctx


def band3(t, nb, blk):
    tb = t.reshape((t.shape[0], nb, blk) + t.shape[2:])
    tp = jnp.pad(tb, [(0, 0), (1, 1)] + [(0, 0)] * (tb.ndim - 2))
    return jnp.concatenate([tp[:, :-2], tp[:, 1:-1], tp[:, 2:]], axis=2)


def swa_mixer(pl, pc, sink, need_ctx):
    B, T, _ = pl[0].shape
    L = pc[0].shape[1]
    G = SWA_Q_HEADS // SWA_KV_HEADS
    d = SWA_HEAD_DIM
    nb = T // SWA_BLOCK
    scale = d ** -0.5
    q = axial_rope(pl[0].reshape(B, T, SWA_Q_HEADS, d))
    k = axial_rope(pl[1].reshape(B, T, SWA_KV_HEADS, d))
    v = pl[2].reshape(B, T, SWA_KV_HEADS, d)
    kc = pc[1].reshape(B, L, SWA_KV_HEADS, d)
    vc = pc[2].reshape(B, L, SWA_KV_HEADS, d)
    sink_g = sink.reshape(SWA_KV_HEADS, G).astype(jnp.float32)

    qb = q.reshape(B, nb, SWA_BLOCK, SWA_KV_HEADS, G, d)
    kb = band3(k, nb, SWA_BLOCK)
    vb = band3(v, nb, SWA_BLOCK)
    n_loc = 3 * SWA_BLOCK
    qi = jnp.arange(SWA_BLOCK)
    kj = jnp.arange(n_loc)
    k_pos = jnp.arange(nb)[:, None] * SWA_BLOCK - SWA_BLOCK + kj[None, :]
    in_window = jnp.abs(kj[None, :] - SWA_BLOCK - qi[:, None]) <= SWA_WINDOW
    valid = in_window[None] & ((k_pos >= 0) & (k_pos < T))[:, None, :]
    s_loc = jnp.einsum('bnqhgd,bnkhd->bnhgqk', qb, kb).astype(jnp.float32) * scale
    s_loc = jnp.where(valid[None, :, None, None], s_loc, -jnp.inf)
    s_ctx = jnp.einsum('bnqhgd,blhd->bnhgql', qb, kc).astype(jnp.float32) * scale
    p = softmax_with_sink(jnp.concatenate([s_loc, s_ctx], axis=-1),
                          sink_g[None, None, :, :, None, None]).astype(v.dtype)
    o = (jnp.einsum('bnhgqk,bnkhd->bnqhgd', p[..., :n_loc], vb)
         + jnp.einsum('bnhgql,blhd->bnqhgd', p[..., n_loc:], vc))
    y_lat = o.reshape(B, T, SWA_Q_HEADS * d)

    y_ctx = None
    if need_ctx:
        qc = pc[0].reshape(B, L, SWA_KV_HEADS, G, d)
        s = jnp.einsum('bqhgd,bkhd->bhgqk', qc, kc).astype(jnp.float32) * scale
        pcx = softmax_with_sink(s, sink_g[None, :, :, None, None]).astype(vc.dtype)
        y_ctx = jnp.einsum('bhgqk,bkhd->bqhgd', pcx, vc).reshape(B, L, SWA_Q_HEADS * d)
    return y_lat, y_ctx


def na_mixer(pl, pc, rpb, rows, need_ctx):
    B, T, _ = pl[0].shape
    L = pc[0].shape[1]
    H, d = NA_HEADS, NA_HEAD_DIM
    kr = min(NA_KR, rows)
    scale = d ** -0.5
    q = pl[0].reshape(B, rows, GRID_W, H, d)
    k = pl[1].reshape(B, rows, GRID_W, H, d)
    v = pl[2].reshape(B, rows, GRID_W, H, d)
    kc = pc[1].reshape(B, L, H, d)
    vc = pc[2].reshape(B, L, H, d)

    r = jnp.arange(rows)
    row_idx = jnp.clip(r - kr // 2, 0, rows - kr)[:, None] + jnp.arange(kr)[None, :]
    col = jnp.arange(GRID_W)
    col_start = jnp.clip(col - NA_KC // 2, 0, GRID_W - NA_KC)
    col_ok = (col[None, :] >= col_start[:, None]) & (col[None, :] < col_start[:, None] + NA_KC)
    row_off = row_idx - r[:, None] + (NA_KR - 1)
    col_off = jnp.clip(col[None, :] - col[:, None], -(NA_KC - 1), NA_KC - 1) + (NA_KC - 1)
    bias = rpb[:, row_off[:, None, :, None], col_off[None, :, None, :]].transpose(1, 0, 2, 3, 4)

    kg = k[:, row_idx]
    vg = v[:, row_idx]
    n_loc = kr * GRID_W
    s_loc = jnp.einsum('brchd,brjkhd->brhcjk', q, kg).astype(jnp.float32) * scale + bias[None].astype(jnp.float32)
    s_loc = jnp.where(col_ok[:, None, :], s_loc, -jnp.inf).reshape(B, rows, H, GRID_W, n_loc)
    s_ctx = jnp.einsum('brchd,blhd->brhcl', q, kc).astype(jnp.float32) * scale
    p = jax.nn.softmax(jnp.concatenate([s_loc, s_ctx], axis=-1), axis=-1).astype(v.dtype)
    p_loc = p[..., :n_loc].reshape(B, rows, H, GRID_W, kr, GRID_W)
    o = (jnp.einsum('brhcjk,brjkhd->brchd', p_loc, vg)
         + jnp.einsum('brhcl,blhd->brchd', p[..., n_loc:], vc))
    y_lat = o.reshape(B, T, H * d)

    y_ctx = None
    if need_ctx:
        qc = pc[0].reshape(B, L, H, d)
        s = jnp.einsum('bqhd,bkhd->bhqk', qc, kc).astype(jnp.float32) * scale
        pcx = jax.nn.softmax(s, axis=-1).astype(vc.dtype)
        y_ctx = jnp.einsum('bhqk,bkhd->bqhd', pcx, vc).reshape(B, L, H * d)
    return y_lat, y_ctx


def sgu_mixer(u, v, ln_g, ln_b, w_s, b_s):
    B, T, _ = u.shape
    n = T // SGU_CHUNK
    u = jax.nn.gelu(u)
    vn = layer_norm(jax.nn.gelu(v), ln_g, ln_b).reshape(B, n, SGU_CHUNK, SGU_GROUPS, SGU_GROUP_CH)
    mixed = jnp.einsum('gpq,bnqgc->bnpgc', w_s, vn) + b_s.T[None, None, :, :, None]
    return u * mixed.reshape(B, T, SGU_WIDTH)


def merge_branches(h, branches, w_gate, b_gate, w_branch, w_out):
    acc = None
    for n in range(N_BRANCH):
        gate = jax.nn.sigmoid(h @ w_gate[n] + b_gate[n])
        term = gate * (branches[n] @ w_branch[n])
        acc = term if acc is None else acc + term
    return acc @ w_out


def expert_choice_moe(h, w_router, w_g, w_u, w_d):
    B, T, _ = h.shape
    cap = EC_CAPACITY_FACTOR * T // N_EXPERTS
    aff = jax.nn.softmax((h @ w_router).astype(jnp.float32), axis=-1)
    gate, idx = lax.top_k(jnp.swapaxes(aff, 1, 2), cap)
    bidx = jnp.arange(B)[:, None, None]
    xe = h[bidx, idx]
    hid = jax.nn.silu(jnp.einsum('becd,edf->becf', xe, w_g)) * jnp.einsum('becd,edf->becf', xe, w_u)
    ye = jnp.einsum('becf,efd->becd', hid, w_d) * gate[..., None].astype(h.dtype)
    return jnp.zeros_like(h).at[bidx, idx].add(ye)


def setup_inputs(seed: int = 0) -> dict:
    key = jax.random.key(seed)
    ks = jax.random.split(key, 30)
    f32 = jnp.float32
    L, D, F = DEPTH, D_MODEL, D_FF_EXPERT

    def nrm(k, shape, scale):
        return jax.random.normal(k, shape, f32) * scale

    return {
        'x': nrm(ks[0], (BATCH, SEQ, D), 1.0),
        'c': nrm(ks[1], (BATCH, D), 1.0),
        'ctx': nrm(ks[2], (BATCH, CTX_LEN, D), 1.0),
        'c_ctx': nrm(ks[3], (D,), 1.0),
        'w_ada': nrm(ks[4], (L, D, 6 * D), 0.5 * D ** -0.5),
        'b_ada': nrm(ks[5], (L, 6 * D), 0.02),
        'norm1_g': 1.0 + nrm(ks[6], (L, D), 0.02),
        'norm2_g': 1.0 + nrm(ks[7], (L, D), 0.02),
        'w_in': nrm(ks[8], (L, D, IN_COLS), D ** -0.5),
        'gla_w_a2': nrm(ks[9], (L, 2, GLA_LOW_RANK, GLA_HEADS * GLA_DK), GLA_LOW_RANK ** -0.5),
        'gla_b_a2': nrm(ks[10], (L, 2, GLA_HEADS * GLA_DK), 0.1),
        'gla_norm_g': 1.0 + nrm(ks[11], (L, GLA_DV), 0.02),
        'swa_sink': nrm(ks[12], (L, SWA_Q_HEADS), 0.5),
        'na_rpb': nrm(ks[13], (L, NA_HEADS, 2 * NA_KR - 1, 2 * NA_KC - 1), 0.2),
        'sgu_ln_g': 1.0 + nrm(ks[14], (L, SGU_WIDTH), 0.02),
        'sgu_ln_b': nrm(ks[15], (L, SGU_WIDTH), 0.02),
        'sgu_w_s': nrm(ks[16], (L, SGU_GROUPS, SGU_CHUNK, SGU_CHUNK), SGU_CHUNK ** -0.5),
        'sgu_b_s': 1.0 + nrm(ks[17], (L, SGU_GROUPS, SGU_CHUNK), 0.02),
        'w_gate': nrm(ks[18], (L, N_BRANCH, D, D), D ** -0.5),
        'b_gate': nrm(ks[19], (L, N_BRANCH, D), 0.02),
        'w_branch': nrm(ks[20], (L, N_BRANCH, BRANCH_WIDTH, D), BRANCH_WIDTH ** -0.5),
        'w_out': nrm(ks[21], (L, D, D), D ** -0.5),
        'w_router': nrm(ks[22], (L, D, N_EXPERTS), D ** -0.5),
        'w_exp_gate': nrm(ks[23], (L, N_EXPERTS, D, F), D ** -0.5),
        'w_exp_up': nrm(ks[24], (L, N_EXPERTS, D, F), D ** -0.5),
        'w_exp_down': nrm(ks[25], (L, N_EXPERTS, F, D), F ** -0.5),
        'final_norm_g': 1.0 + nrm(ks[26], (D,), 0.02),
    }


def reference(x, c, ctx, c_ctx, w_ada, b_ada, norm1_g, norm2_g, w_in, gla_w_a2, gla_b_a2,
              gla_norm_g, swa_sink, na_rpb, sgu_ln_g, sgu_ln_b, sgu_w_s, sgu_b_s, w_gate, b_gate,
              w_branch, w_out, w_router, w_exp_gate, w_exp_up, w_exp_down, final_norm_g):
    B, T, D = x.shape
    rows = T // GRID_W
    c_act = jax.nn.silu(c)
    cc_act = jax.nn.silu(c_ctx)
    xc = ctx
    for l in range(DEPTH):
        need_ctx = l < DEPTH - 1
        mod = (c_act @ w_ada[l] + b_ada[l]).reshape(B, 6, 1, D)
        modc = (cc_act @ w_ada[l] + b_ada[l]).reshape(6, D)
        h = modulate(x, norm1_g[l], mod[:, 0], mod[:, 1])
        hc = modulate(xc, norm1_g[l], modc[0], modc[1])
        pl = jnp.split(h @ w_in[l], IN_SPLIT_POINTS, axis=-1)
        pc = jnp.split(hc @ w_in[l], IN_SPLIT_POINTS, axis=-1)
        ya_l, ya_c = gla_mixer(pl[0:6], pc[0:6], gla_w_a2[l], gla_b_a2[l], gla_norm_g[l], need_ctx)
        yb_l, yb_c = swa_mixer(pl[6:9], pc[6:9], swa_sink[l], need_ctx)
        yn_l, yn_c = na_mixer(pl[9:12], pc[9:12], na_rpb[l], rows, need_ctx)
        yd_l = sgu_mixer(pl[12], pl[13], sgu_ln_g[l], sgu_ln_b[l], sgu_w_s[l], sgu_b_s[l])
        x = x + mod[:, 2] * merge_branches(h, (ya_l, yb_l, yn_l, yd_l), w_gate[l], b_gate[l], w_branch[l], w_out[l])
        h2 = modulate(x, norm2_g[l], mod[:, 3], mod[:, 4])
        x = x + mod[:, 5] * expert_choice_moe(h2, w_router[l], w_exp_gate[l], w_exp_up[l], w_exp_down[l])
        if need_ctx:
            yd_c = sgu_mixer(pc[12], pc[13], sgu_ln_g[l], sgu_ln_b[l], sgu_w_s[l], sgu_b_s[l])
            xc = xc + modc[2] * merge_branches(hc, (ya_c, yb_c, yn_c, yd_c), w_gate[l], b_gate[l], w_branch[l], w_out[l])
            hc2 = modulate(xc, norm2_g[l], modc[3], modc[4])
            xc = xc + modc[5] * expert_choice_moe(hc2, w_router[l], w_exp_gate[l], w_exp_up[l], w_exp_down[l])
    return rms_norm(x, final_norm_g)
```

```python
import os
import numpy as np
import ml_dtypes
import concourse.bass as bass
import concourse.mybir as mybir
from concourse.bass_utils import run_bass_kernel_spmd
from contextlib import ExitStack

F32 = mybir.dt.float32
BF16 = mybir.dt.bfloat16
I32 = mybir.dt.int32
U32 = mybir.dt.uint32
AF = mybir.ActivationFunctionType
ALU = mybir.AluOpType
AX = mybir.AxisListType

NCORES = 8
D = 2048
T = 2048
LC = 256
NS = 2
NF = NS * T + NS * LC
NTL = NF // 128
DEPTH = 2
EPS = 1e-6
NEG = -30000.0
NDS = 12
GCH = 128
GNC = 2304 // GCH
GCC = LC // GCH


def tile_row(tt):
    return 0 if tt < 16 else (1 if tt < 32 else 2)


class Res:
    __slots__ = ("w", "r", "pr")

    def __init__(self):
        self.w = {}
        self.r = {}
        self.pr = {}


class Sched:
    ENG = ("pe", "act", "dve", "pool", "sp")

    def __init__(self, nc, es):
        self.nc = nc
        self.sem = {e: es.enter_context(nc.semaphore("s_" + e)) for e in self.ENG}
        self.cnt = {e: 0 for e in self.ENG}
        self.dsem = {q: [es.enter_context(nc.semaphore("d_%s%d" % (q, i))) for i in range(NDS)]
                     for q in ("sp", "act", "pool")}
        self.dcnt = {q: [0] * NDS for q in ("sp", "act", "pool")}
        self.drr = {q: 0 for q in ("sp", "act", "pool")}
        self.waited = {e: {} for e in self.ENG}
        self.q = {e: [] for e in self.ENG}
        self.nops = 0

    def _waits(self, eng, toks):
        best = {}
        for (sem, key, val) in toks:
            if self.waited[eng].get(key, 0) >= val:
                continue
            if key not in best or best[key][1] < val:
                best[key] = (sem, val)
        out = []
        for key, (sem, val) in best.items():
            self.waited[eng][key] = val
            out.append((sem, val))
        return out

    def _deps(self, eng, reads, writes, join=False):
        toks = []
        for r in reads:
            for t in r.w.values():
                if not (t[1] == eng and eng == "pe"):
                    toks.append(t)
        for w in writes:
            if not join:
                for t in w.w.values():
                    if not (t[1] == eng and eng == "pe"):
                        toks.append(t)
            else:
                for t in w.pr.values():
                    if not (t[1] == eng and eng == "pe"):
                        toks.append(t)
            for t in w.r.values():
                if not (t[1] == eng and eng == "pe"):
                    toks.append(t)
        return toks

    def _finish(self, tok, reads, writes, join=False):
        for r in reads:
            r.r[tok[1]] = tok
        for w in writes:
            if join:
                w.w[tok[1]] = tok
            else:
                pr = dict(w.w)
                pr.update(w.r)
                w.pr = pr
                w.w = {tok[1]: tok}
                w.r = {}

    def op(self, eng, fn, reads=(), writes=(), join=False):
        waits = self._waits(eng, self._deps(eng, reads, writes, join))
        self.cnt[eng] += 1
        sem = self.sem[eng]
        tok = (sem, eng, self.cnt[eng])

        def emit(h, waits=waits, fn=fn, sem=sem):
            for (s, v) in waits:
                h.wait_ge(s, v)
            fn(h).then_inc(sem, 1)

        self.q[eng].append(emit)
        self.nops += 1
        self._finish(tok, reads, writes, join)
        return tok

    def dma(self, q, fn, reads=(), writes=(), join=False):
        i = self.drr[q]
        self.drr[q] = (i + 1) % NDS
        sem = self.dsem[q][i]
        prev = self.dcnt[q][i]
        key = "d_%s%d" % (q, i)
        toks = self._deps(q, reads, writes, join)
        if prev > 0:
            toks.append((sem, key, prev))
        waits = self._waits(q, toks)
        self.dcnt[q][i] = prev + 16
        tok = (sem, key, prev + 16)

        def emit(h, waits=waits, fn=fn, sem=sem):
            for (s, v) in waits:
                h.wait_ge(s, v)
            fn(h).then_inc(sem, 16)

        self.q[q].append(emit)
        self.nops += 1
        self._finish(tok, reads, writes, join)
        return tok

    def all_tokens(self):
        toks = [(self.sem[e], e, self.cnt[e]) for e in self.ENG if self.cnt[e] > 0]
        for q in self.dsem:
            for i in range(NDS):
                if self.dcnt[q][i] > 0:
                    toks.append((self.dsem[q][i], "d_%s%d" % (q, i), self.dcnt[q][i]))
        return toks

    def barrier(self):
        toks = self.all_tokens()
        for e in self.ENG:
            waits = self._waits(e, [t for t in toks if t[1] != e])
            if waits:
                def emit(h, waits=waits):
                    for (s, v) in waits:
                        h.wait_ge(s, v)
                self.q[e].append(emit)

    def flush(self):
        q = self.q
        with self.nc.Block() as block:
            @block.tensor
            def _(h):
                for f in q["pe"]:
                    f(h)

            @block.scalar
            def _(h):
                for f in q["act"]:
                    f(h)

            @block.vector
            def _(h):
                for f in q["dve"]:
                    f(h)

            @block.gpsimd
            def _(h):
                for f in q["pool"]:
                    f(h)

            @block.sync
            def _(h):
                for f in q["sp"]:
                    f(h)
        self.q = {e: [] for e in self.ENG}


class _Stop(Exception):
    pass


def _stop(k):
    if float(os.environ.get("DBG_STOP", "99")) <= k:
        raise _Stop()


def pipeline(n, stages):
    ns = len(stages)
    for i in range(n + ns - 1):
        for k, st in enumerate(stages):
            j = i - k
            if 0 <= j < n:
                st(j)


class TL:
    __slots__ = ("t", "r")

    def __init__(self, t):
        self.t = t
        self.r = Res()

    def __getitem__(self, k):
        return self.t[k]


class Phase:
    def __init__(self, B):
        self.B = B
        self.es = ExitStack()

    def __enter__(self):
        self.es.__enter__()
        return self

    _uid = [0]

    def sb(self, name, shape, dt):
        Phase._uid[0] += 1
        return TL(self.es.enter_context(self.B.nc.sbuf_tensor("%s_%d" % (name, Phase._uid[0]), list(shape), dt)))

    def ps(self, name, shape, dt=F32):
        Phase._uid[0] += 1
        return TL(self.es.enter_context(self.B.nc.psum_tensor("%s_%d" % (name, Phase._uid[0]), list(shape), dt)))

    def __exit__(self, *a):
        self.B.S.barrier()
        self.B.S.flush()
        return self.es.__exit__(*a)


TOK_SEGS = [(256, 1536, 0), (1568, 2336, 1280), (3360, 4896, 2048)]
PT_KG, PT_VG, PT_RG = 0, 256, 768
PT_QS, PT_KS, PT_VS = 1280, 1792, 1920
PT_VN, PT_U, PT_V = 2048, 2560, 3072
NPT = 3584
FM_SEGS = [(0, 128, 0), (128, 128, 128), (256, 128, 256), (384, 128, 384), (1536, 32, 512),
           (2080, 128, 640)] + [(2336 + 128 * i, 128, 768 + 128 * i) for i in range(8)]
FM_QG, FM_KG, FM_A, FM_KS, FM_QN, FM_KN = 0, 256, 512, 640, 768, 1280
NFM = 1792


INPUT_SPECS = {
    "xin": (lambda: [NF, D], F32),
    "cin": (lambda: [128, 48], F32),
    "w_ada": (lambda: [DEPTH, D, 6 * D], F32),
    "b_ada3": (lambda: [DEPTH, 3, 6 * D], F32),
    "gvec3": (lambda: [DEPTH, 2, 3, D], F32),
    "fing": (lambda: [1, D], F32),
    "w_in": (lambda: [DEPTH, D, 4896], F32),
    "gla_wa": (lambda: [DEPTH, 2, 17, 256], F32),
    "gla_g": (lambda: [DEPTH, 1, 128], F32),
    "swa_sink": (lambda: [DEPTH, 1, 8], F32),
    "na_g": (lambda: [DEPTH, 128, 7168], F32),
    "na_m": (lambda: [128, 7168], F32),
    "sgu_lng": (lambda: [DEPTH, 1, 512], F32),
    "sgu_lnb": (lambda: [DEPTH, 1, 512], F32),
    "sgu_wsT": (lambda: [DEPTH, 128, 512], F32),
    "sgu_bs": (lambda: [DEPTH, 128, 4], F32),
    "w_gate": (lambda: [DEPTH, 4, D, D], F32),
    "b_gate_p": (lambda: [DEPTH, 128, 64], F32),
    "w_branch": (lambda: [DEPTH, 4, 512, D], F32),
    "w_out": (lambda: [DEPTH, D, D], F32),
    "w_router": (lambda: [DEPTH, D, 16], F32),
    "w_eg": (lambda: [DEPTH, 16, D, D], F32),
    "w_eu": (lambda: [DEPTH, 16, D, D], F32),
    "w_ed": (lambda: [DEPTH, 16, D, D], F32),
    "ident_f": (lambda: [128, 128], F32),
    "ident_b": (lambda: [128, 128], BF16),
    "rope_cs": (lambda: [T, 64], F32),
    "gla_u": (lambda: [GCH, 4 * GCH], F32),
    "gla_mask": (lambda: [GCH, 4 * GCH], F32),
    "swa_negm": (lambda: [128, 2 * 512], BF16),
    "tk_off": (lambda: [32, 2], F32),
}


class Builder:
    def __init__(self, dbg=False, scr_in=(), scr_out=()):
        self.dbg = dbg
        self.scr_in = set(scr_in)
        self.scr_out = set(scr_out)
        self.dbg_outs = []
        nc = self.nc = bass.Bass("TRN2", target_bir_lowering=False)
        self.es = ExitStack()
        self.S = Sched(nc, self.es)
        self.inputs = {}
        self.out = nc.dram_tensor("out", [NS * T, D], F32, kind="ExternalOutput").ap()
        Z = self._scr
        self.X = Z("X", [NF, D], F32)
        self.MODV = Z("MODV", [DEPTH, 3, 6 * D], F32)
        self.HT = Z("HT", [D, NF], BF16)
        self.PTOK = Z("PTOK", [NF, NPT], BF16)
        self.PFM = Z("PFM", [NFM, NF], BF16)
        self.BRT = Z("BRT", [D, NF], BF16)
        self.ACCT = Z("ACCT", [D, NF], BF16)
        self.H2 = Z("H2", [NF, D], BF16)
        self.AFFT = Z("AFFT", [32, T + LC], F32)
        self.IDXT = Z("IDXT", [128, 64], I32)
        self.GT = Z("GT", [128, 64], F32)
        self.IDXC = Z("IDXC", [32, 32], I32)
        self.GC = Z("GC", [32, 32], F32)
        self.IDX4 = Z("IDX4", [128, 4 * 64], I32)
        self.IDXC4 = Z("IDXC4", [32, 4 * 32], I32)

    def __getattr__(self, name):
        if name in INPUT_SPECS:
            shp, dt = INPUT_SPECS[name]
            shape = shp()
            self.inputs[name] = (shape, dt)
            ap = self.nc.dram_tensor(name, list(shape), dt, kind="ExternalInput").ap()
            setattr(self, name, ap)
            return ap
        raise AttributeError(name)

    def _scr(self, name, shape, dt):
        kind = "Internal"
        if name in self.scr_in:
            kind = "ExternalInput"
            self.inputs[name] = (shape, dt)
        elif name in self.scr_out:
            kind = "ExternalOutput"
            self.dbg_outs.append(name)
        return self.nc.dram_tensor(name, list(shape), dt, kind=kind).ap()

    def phase(self):
        return Phase(self)

    def p_mod(self, l):
        S = self.S
        with self.phase() as P:
            ct = P.sb("ct", [128, 48], F32)
            ca = P.sb("ca", [128, 48], BF16)
            wt = [P.sb("wt%d" % i, [128, 16, 512], BF16) for i in range(2)]
            bt = P.sb("bt", [3, 6 * D], F32)
            gt = P.sb("gt", [3, 2, D], F32)
            mo = P.sb("mo", [3, 6 * D], F32)
            ps = [P.ps("ps%d" % i, [3, 512]) for i in range(2)]
            S.dma("sp", lambda h: h.dma_start(out=ct[:], in_=self.cin), writes=[ct.r])
            S.dma("sp", lambda h: h.dma_start(out=bt[:], in_=self.b_ada3[l]), writes=[bt.r])
            S.dma("sp", lambda h: h.dma_start(out=gt[:], in_=self.gvec3[l].rearrange("n r d -> r n d")), writes=[gt.r])
            S.op("act", lambda h: h.activation(out=ca[:], in_=ct[:], func=AF.Silu), reads=[ct.r], writes=[ca.r])
            for cg in range(24):
                w = wt[cg % 2]
                S.dma("pool", lambda h, w=w, cg=cg: h.dma_start(
                    out=w[:], in_=self.w_ada[l][:, cg * 512:(cg + 1) * 512].rearrange("(kc p) n -> p kc n", p=128)),
                    writes=[w.r])
                p = ps[cg % 2]

                def mm(h, w=w, p=p):
                    for kc in range(16):
                        ins = h.matmul(p[:], lhsT=ca[:, kc * 3:(kc + 1) * 3], rhs=w[:, kc, :], start=(kc == 0), stop=(kc == 15))
                    return ins
                S.op("pe", mm, reads=[ca.r, w.r], writes=[p.r])
                S.op("dve", lambda h, p=p, cg=cg: h.tensor_tensor(out=mo[:, cg * 512:(cg + 1) * 512], in0=p[:],
                                                                  in1=bt[:, cg * 512:(cg + 1) * 512], op=ALU.add),
                     reads=[p.r, bt.r], writes=[mo.r])
            for n, k in ((0, 1), (1, 4)):
                S.op("dve", lambda h, n=n, k=k: h.scalar_tensor_tensor(
                    out=mo[:, k * D:(k + 1) * D], in0=mo[:, k * D:(k + 1) * D], scalar=1.0, in1=gt[:, n, :],
                    op0=ALU.add, op1=ALU.mult), reads=[mo.r, gt.r], writes=[mo.r])
            S.dma("sp", lambda h: h.dma_start(out=self.MODV[l], in_=mo[:]), reads=[mo.r])

    def bc_load(self, P, name, l, k):
        S = self.S
        t = P.sb(name, [128, 3, D], F32)
        for r in range(3):
            S.dma("sp", lambda h, r=r: h.dma_start(out=t[:, r, :], in_=self.MODV[l][r:r + 1, k * D:(k + 1) * D].broadcast_to([128, D])),
                  writes=[t.r], join=True)
        return t

    def norm_a(self, xt, st, sq):
        S = self.S
        S.op("act", lambda h: h.activation(out=sq[:], in_=xt[:], func=AF.Square, accum_out=st[:, 0:1]),
             reads=[xt.r], writes=[sq.r, st.r])
        S.op("dve", lambda h: h.tensor_scalar(out=st[:, 1:2], in0=st[:, 0:1], scalar1=1.0 / D, scalar2=EPS,
                                              op0=ALU.mult, op1=ALU.add), reads=[st.r], writes=[st.r])

    def norm_a2(self, st):
        S = self.S
        S.op("act", lambda h: h.sqrt(out=st[:, 3:4], in_=st[:, 1:2]), reads=[st.r], writes=[st.r])
        S.op("dve", lambda h: h.reciprocal(out=st[:, 2:3], in_=st[:, 3:4]), reads=[st.r], writes=[st.r])

    def norm_b(self, xt, st, G, SH, r, tmp, hout):
        S = self.S
        S.op("dve", lambda h: h.scalar_tensor_tensor(out=tmp[:], in0=xt[:], scalar=st[:, 2:3], in1=G[:, r, :],
                                                     op0=ALU.mult, op1=ALU.mult), reads=[xt.r, st.r, G.r], writes=[tmp.r])
        S.op("pool", lambda h: h.tensor_tensor(out=hout[:, 0:1152], in0=tmp[:, 0:1152], in1=SH[:, r, 0:1152], op=ALU.add),
             reads=[tmp.r, SH.r], writes=[hout.r])
        S.op("dve", lambda h: h.tensor_tensor(out=hout[:, 1152:D], in0=tmp[:, 1152:D], in1=SH[:, r, 1152:D], op=ALU.add),
             reads=[tmp.r, SH.r], writes=[hout.r], join=True)

    def p_norm1(self, l):
        S = self.S
        src = self.xin if l == 0 else self.X
        with self.phase() as P:
            G = self.bc_load(P, "G", l, 1)
            SH = self.bc_load(P, "SH", l, 0)
            idb = P.sb("idb", [128, 128], BF16)
            S.dma("sp", lambda h: h.dma_start(out=idb[:], in_=self.ident_b), writes=[idb.r])
            xt = [P.sb("xt%d" % i, [128, D], F32) for i in range(4)]
            st = [P.sb("st%d" % i, [128, 4], F32) for i in range(4)]
            sq = [P.sb("sq%d" % i, [128, D], BF16) for i in range(2)]
            tmp = [P.sb("tmp%d" % i, [128, D], F32) for i in range(2)]
            hb = [P.sb("hb%d" % i, [128, D], BF16) for i in range(2)]
            stg = [P.sb("stg%d" % i, [128, 16, 512], BF16) for i in range(2)]
            pt = [P.ps("pt%d" % i, [128, 8, 128], BF16) for i in range(4)]

            def stA(tt):
                x = xt[tt % 4]
                S.dma("sp", lambda h: h.dma_start(out=x[:], in_=src[tt * 128:(tt + 1) * 128, :]), writes=[x.r])
                self.norm_a(x, st[tt % 4], sq[tt % 2])

            def stA2(tt):
                self.norm_a2(st[tt % 4])

            def stB(tt):
                self.norm_b(xt[tt % 4], st[tt % 4], G, SH, tile_row(tt), tmp[tt % 2], hb[tt % 2])

            def stC(tt):
                hh = hb[tt % 2]
                sg = stg[(tt // 4) % 2]
                q = tt % 4
                for half in range(2):
                    p = pt[(tt % 2) * 2 + half]

                    def tr(h, p=p, half=half):
                        for j in range(8):
                            kc = half * 8 + j
                            ins = h.transpose(p[:, j, :], hh[:, kc * 128:(kc + 1) * 128], idb[:])
                        return ins
                    S.op("pe", tr, reads=[hh.r, idb.r], writes=[p.r])
                    if half == 0:
                        S.op("act", lambda h, p=p: h.copy(out=sg[:, 0:8, q * 128:(q + 1) * 128], in_=p[:]), reads=[p.r], writes=[sg.r], join=True)
                    else:
                        S.op("dve", lambda h, p=p: h.tensor_copy(out=sg[:, 8:16, q * 128:(q + 1) * 128], in_=p[:]), reads=[p.r], writes=[sg.r], join=True)
                if q == 3:
                    g = tt // 4
                    S.dma("sp", lambda h: h.dma_start(
                        out=self.HT[:, g * 512:(g + 1) * 512].rearrange("(kc p) n -> p kc n", p=128), in_=sg[:]),
                        reads=[sg.r])
            pipeline(NTL, [stA, stA2, stB, stC])

    def p_proj(self, l):
        S = self.S
        for pz in range(2):
            f0 = pz * 2304
            with self.phase() as P:
                hT = P.sb("hT", [128, 16, 2304], BF16)
                for kq in range(4):
                    S.dma("sp", lambda h, kq=kq: h.dma_start(
                        out=hT[:, kq * 4:(kq + 1) * 4, :],
                        in_=self.HT[kq * 512:(kq + 1) * 512, f0:f0 + 2304].rearrange("(kc p) n -> p kc n", p=128)),
                        writes=[hT.r], join=True)
                wt = [P.sb("w%d" % i, [128, 16, 512], BF16) for i in range(2)]
                stg = [P.sb("sg%d" % i, [128, 18, 512], BF16) for i in range(2)]
                wf = [P.sb("wf%d" % i, [128, 16, 128], BF16) for i in range(2)]
                sgf = [P.sb("sgf%d" % i, [128, 2304], BF16) for i in range(2)]
                pss = [P.ps("ps%d" % i, [128, 512]) for i in range(4)]
                gi = 0
                pi = 0
                for (lo, hi, off) in TOK_SEGS:
                    c = lo
                    while c < hi:
                        n = min(512, hi - c)
                        w = wt[gi % 2]
                        sg = stg[gi % 2]
                        S.dma("pool", lambda h, w=w, c=c, n=n: h.dma_start(
                            out=w[:, :, 0:n], in_=self.w_in[l][:, c:c + n].rearrange("(kc p) n -> p kc n", p=128)),
                            writes=[w.r])
                        for ti in range(18):
                            p = pss[pi % 4]

                            def mm(h, w=w, p=p, ti=ti, n=n):
                                for kc in range(16):
                                    ins = h.matmul(p[:, 0:n], lhsT=hT[:, kc, ti * 128:(ti + 1) * 128], rhs=w[:, kc, 0:n],
                                                   start=(kc == 0), stop=(kc == 15))
                                return ins
                            S.op("pe", mm, reads=[hT.r, w.r], writes=[p.r])
                            if pi % 2 == 0:
                                S.op("act", lambda h, p=p, sg=sg, ti=ti, n=n: h.copy(out=sg[:, ti, 0:n], in_=p[:, 0:n]),
                                     reads=[p.r], writes=[sg.r], join=True)
                            else:
                                S.op("dve", lambda h, p=p, sg=sg, ti=ti, n=n: h.tensor_copy(out=sg[:, ti, 0:n], in_=p[:, 0:n]),
                                     reads=[p.r], writes=[sg.r], join=True)
                            pi += 1
                        co = off + (c - lo)
                        S.dma("sp", lambda h, sg=sg, co=co, n=n: h.dma_start(
                            out=self.PTOK[f0:f0 + 2304, co:co + n].rearrange("(t p) n -> p t n", p=128), in_=sg[:, :, 0:n]),
                            reads=[sg.r])
                        c += n
                        gi += 1
                for i, (lo, n, off) in enumerate(FM_SEGS):
                    w = wf[i % 2]
                    sg = sgf[i % 2]
                    S.dma("pool", lambda h, w=w, lo=lo, n=n: h.dma_start(
                        out=w[:, :, 0:n], in_=self.w_in[l][:, lo:lo + n].rearrange("(kc p) n -> p kc n", p=128)),
                        writes=[w.r])
                    for g in range(5):
                        c0 = g * 512
                        ncol = min(512, 2304 - c0)
                        p = pss[pi % 4]

                        def mm(h, w=w, p=p, c0=c0, ncol=ncol, n=n):
                            for kc in range(16):
                                ins = h.matmul(p[0:n, 0:ncol], lhsT=w[:, kc, 0:n], rhs=hT[:, kc, c0:c0 + ncol],
                                               start=(kc == 0), stop=(kc == 15))
                            return ins
                        S.op("pe", mm, reads=[hT.r, w.r], writes=[p.r])
                        if pi % 2 == 0:
                            S.op("act", lambda h, p=p, sg=sg, c0=c0, ncol=ncol, n=n: h.copy(out=sg[0:n, c0:c0 + ncol], in_=p[0:n, 0:ncol]),
                                 reads=[p.r], writes=[sg.r], join=True)
                        else:
                            S.op("dve", lambda h, p=p, sg=sg, c0=c0, ncol=ncol, n=n: h.tensor_copy(out=sg[0:n, c0:c0 + ncol], in_=p[0:n, 0:ncol]),
                                 reads=[p.r], writes=[sg.r], join=True)
                        pi += 1
                    S.dma("sp", lambda h, sg=sg, off=off, n=n: h.dma_start(out=self.PFM[off:off + n, f0:f0 + 2304], in_=sg[0:n, :]),
                          reads=[sg.r])


def _consts():
    c = {}
    c["ident_f"] = np.eye(128, dtype=np.float32)
    c["ident_b"] = np.eye(128, dtype=np.float32).astype(ml_dtypes.bfloat16)
    t = np.arange(T)
    quarter = 16
    freqs = (10000.0 ** (-np.arange(quarter, dtype=np.float32) / quarter)).astype(np.float32)
    row = (t // 64).astype(np.float32)
    col = (t % 64).astype(np.float32)
    ang = np.concatenate([row[:, None] * freqs, col[:, None] * freqs], axis=-1).astype(np.float32)
    c["rope_cs"] = np.concatenate([np.cos(ang), np.sin(ang)], axis=-1).astype(np.float32)
    m = np.arange(GCH)[:, None]
    ll = np.arange(GCH)[None, :]
    sc = np.float32(-1.0 / 16.0)
    U = np.stack([(m <= ll), (m > ll), (m >= ll), (m < ll)], axis=1).astype(np.float32) * sc
    c["gla_u"] = U.reshape(GCH, 4 * GCH)
    mk = np.stack([(ll >= m), (ll <= m)], axis=1).astype(np.float32)
    mk = np.repeat(mk[:, :, None, :], 2, axis=2)
    c["gla_mask"] = mk.reshape(GCH, 4 * GCH)
    kj = np.arange(128)[:, None]
    qi = np.arange(128)[None, :]
    prev = np.where(kj >= qi, 1.0, 0.0).astype(np.float32)
    nxt = np.where(kj <= qi, 1.0, 0.0).astype(np.float32)
    nm = np.stack([np.tile(prev[:, None, :], (1, 4, 1)), np.tile(nxt[:, None, :], (1, 4, 1))], axis=1)
    c["swa_negm"] = nm.reshape(128, 1024).astype(ml_dtypes.bfloat16)
    cc = np.arange(64)
    cstart = np.clip(cc - 8, 0, 48)
    ok = (cc[None, :] >= cstart[:, None]) & (cc[None, :] < cstart[:, None] + 16)
    negm = np.where(ok.T, 1.0, 0.0).astype(np.float32)
    negm = np.tile(negm, (2, 1))
    c["tk_off"] = np.array([[0.0, 4096.0]] * 16 + [[2048.0, 4352.0]] * 16, dtype=np.float32)
    c["na_m"] = np.ascontiguousarray(np.broadcast_to(negm[:, None, :], (128, 8 * 14, 64))).reshape(128, 7168)
    return c


def _na_gather(rpb):
    kc = np.arange(64)[:, None]
    cc = np.arange(64)[None, :]
    coff = np.clip(kc - cc, -15, 15) + 15
    out = np.empty((rpb.shape[0], 2, 64, 8, 14, 64), np.float32)
    for w in range(2):
        g = rpb[:, :, w:w + 14, :][:, :, :, coff]
        out[:, w] = g.transpose(0, 3, 1, 2, 4)
    return out.reshape(rpb.shape[0], 128, 7168)


def _shared_inputs(I):
    f = lambda a: np.ascontiguousarray(a, dtype=np.float32)
    sh = dict(_consts())
    L = DEPTH
    sh["w_ada"] = f(I["w_ada"])
    sh["b_ada3"] = f(np.broadcast_to(I["b_ada"][:, None, :], (L, 3, 6 * D)))
    g = np.stack([I["norm1_g"], I["norm2_g"]], axis=1)
    sh["gvec3"] = f(np.broadcast_to(g[:, :, None, :], (L, 2, 3, D)))
    sh["fing"] = f(I["final_norm_g"].reshape(1, D))
    sh["w_in"] = f(I["w_in"])
    sh["gla_wa"] = f(np.concatenate([I["gla_w_a2"], I["gla_b_a2"][:, :, None, :]], axis=2))
    sh["gla_g"] = f(I["gla_norm_g"].reshape(L, 1, 128))
    sh["swa_sink"] = f(I["swa_sink"].reshape(L, 1, 8))
    sh["na_g"] = _na_gather(np.asarray(I["na_rpb"], np.float32))
    sh["sgu_lng"] = f(I["sgu_ln_g"].reshape(L, 1, 512))
    sh["sgu_lnb"] = f(I["sgu_ln_b"].reshape(L, 1, 512))
    sh["sgu_wsT"] = f(np.asarray(I["sgu_w_s"]).transpose(0, 3, 1, 2).reshape(L, 128, 512))
    sh["sgu_bs"] = f(np.asarray(I["sgu_b_s"]).transpose(0, 2, 1))
    sh["w_gate"] = f(I["w_gate"])
    sh["b_gate_p"] = f(np.asarray(I["b_gate"]).reshape(L, 4, 16, 128).transpose(0, 3, 1, 2).reshape(L, 128, 64))
    sh["w_branch"] = f(I["w_branch"])
    sh["w_out"] = f(I["w_out"])
    sh["w_router"] = f(I["w_router"])
    sh["w_eg"] = f(I["w_exp_gate"])
    sh["w_eu"] = f(I["w_exp_up"])
    sh["w_ed"] = f(I["w_exp_down"])
    return sh


def _core_inputs(I, core, sh):
    m = dict(sh)
    s0, s1 = 2 * core, 2 * core + 1
    m["xin"] = np.ascontiguousarray(np.concatenate([I["x"][s0], I["x"][s1], I["ctx"][s0], I["ctx"][s1]], axis=0), dtype=np.float32)
    rows = np.stack([I["c"][s0], I["c"][s1], I["c_ctx"]], axis=0).astype(np.float32)
    m["cin"] = np.ascontiguousarray(rows.reshape(3, 16, 128).transpose(2, 1, 0).reshape(128, 48))
    return m


def _gla_phase(self, l, s, hp):
    S = self.S
    cf = 4096 + s * 256
    lf = s * 2048
    with self.phase() as P:
        def load_fm(t, row0, nrows, q="sp", after=()):
            S.dma(q, lambda h: h.dma_start(out=t[0:nrows, 0:256], in_=self.PFM[row0:row0 + nrows, cf:cf + 256]), reads=list(after), writes=[t.r], join=True)
            S.dma(q, lambda h: h.dma_start(out=t[0:nrows, 256:2304], in_=self.PFM[row0:row0 + nrows, lf:lf + 2048]), reads=list(after), writes=[t.r], join=True)

        def load_tok(t, col0, ncols):
            S.dma("sp", lambda h: h.dma_start(out=t[:, 0:GCC, :], in_=self.PTOK[cf:cf + 256, col0:col0 + ncols].rearrange("(c p) n -> p c n", p=GCH)),
                  writes=[t.r], join=True)
            S.dma("sp", lambda h: h.dma_start(out=t[:, GCC:GNC, :], in_=self.PTOK[lf:lf + 2048, col0:col0 + ncols].rearrange("(c p) n -> p c n", p=GCH)),
                  writes=[t.r], join=True)

        def load_fm2(t, hsel, row0):
            S.dma("sp", lambda h: h.dma_start(out=t[:, hsel, 0:256], in_=self.PFM[row0:row0 + 64, cf:cf + 256]), writes=[t.r], join=True)
            S.dma("sp", lambda h: h.dma_start(out=t[:, hsel, 256:2304], in_=self.PFM[row0:row0 + 64, lf:lf + 2048]), writes=[t.r], join=True)

        qT = P.sb("qT", [64, 2, 2304], BF16)
        kT = P.sb("kT", [64, 2, 2304], BF16)
        for hh in range(2):
            load_fm2(qT, hh, FM_QG + hp * 128 + hh * 64)
            load_fm2(kT, hh, FM_KG + hp * 128 + hh * 64)
        aT = [P.sb("aT%d" % d, [17, 2304], BF16) for d in range(2)]
        wa = P.sb("wa", [17, 2, 128], BF16)
        for d in range(2):
            ms = Res()
            S.op("pool", lambda h, d=d: h.memset(aT[d][:], 1.0), writes=[aT[d].r, ms])
            load_fm(aT[d], FM_A + d * 16, 16, after=[ms])
            S.dma("pool", lambda h, d=d: h.dma_start(out=wa[:, d, :], in_=self.gla_wa[l][d][:, hp * 128:(hp + 1) * 128]),
                  writes=[wa.r], join=True)
        kt = P.sb("kt", [GCH, GNC, 128], BF16)
        vt = P.sb("vt", [GCH, GNC, 256], BF16)
        load_tok(kt, PT_KG + hp * 128, 128)
        load_tok(vt, PT_VG + hp * 256, 256)
        U = P.sb("U", [GCH, 4, GCH], F32)
        MK = P.sb("MK", [GCH, 2, 2, GCH], F32)
        idb = P.sb("idb", [128, 128], BF16)
        gbc = P.sb("gbc", [GCH, 128], F32)
        S.dma("sp", lambda h: h.dma_start(out=U[:], in_=self.gla_u.rearrange("p (a b) -> p a b", b=GCH)), writes=[U.r])
        S.dma("sp", lambda h: h.dma_start(out=MK[:], in_=self.gla_mask.rearrange("p (a b c) -> p a b c", b=2, c=GCH)), writes=[MK.r])
        S.dma("sp", lambda h: h.dma_start(out=idb[:], in_=self.ident_b), writes=[idb.r])
        S.dma("sp", lambda h: h.dma_start(out=gbc[:], in_=self.gla_g[l].broadcast_to([GCH, 128])), writes=[gbc.r])
        sp = P.sb("sp", [GCH, GNC, 128], F32)
        e1 = P.sb("e1", [GCH, 4, 128], F32)
        tq = P.sb("tq", [64, 2, 256], F32)
        tk = P.sb("tk", [64, 2, 256], F32)
        qin = [P.sb("qin%d" % d, [64, 2, 2304], BF16) for d in range(2)]
        kin = [P.sb("kin%d" % d, [64, 2, 2304], BF16) for d in range(2)]
        kte = [P.sb("kte%d" % d, [GCH, GNC, 128], BF16) for d in range(2)]
        dec = [P.sb("dec%d" % d, [64, 2, GNC], F32) for d in range(2)]
        S32 = [P.sb("S32%d" % d, [64, 2, 128], F32) for d in range(2)]
        Sbf = [P.sb("Sbf%d" % d, [64, 2, 128], BF16) for d in range(2)]
        at = [P.sb("at%d" % d, [GCH, 2, GCH], BF16) for d in range(2)]
        oacc = P.sb("oacc", [GCH, GNC, 256], F32)
        ores = [Res() for _ in range(GNC)]
        bk = [P.ps("bk%d" % i, [128, 512]) for i in range(6)]
        tb = [P.ps("tb%d" % i, [128, 1024], BF16) for i in range(2)]
        S.op("pool", lambda h: h.memset(oacc[:], 0.0), writes=ores)
        for d in range(2):
            S.op("pool", lambda h, d=d: h.memset(S32[d][:], 0.0), writes=[S32[d].r])
            S.op("pool", lambda h, d=d: h.memset(Sbf[d][:], 0.0), writes=[Sbf[d].r])
        for d in range(2):
            ui, ui2 = (0, 1) if d == 0 else (2, 3)
            zb = [(c0, min(4, GNC - c0)) for c0 in range(0, GNC, 4)]
            for bi_, (c0, nb) in enumerate(zb):
                b = bk[bi_ % 2]
                pz = b[0:GCH, :].rearrange("p (a b) -> p a b", b=128)

                def mmz(h, pz=pz, c0=c0, nb=nb, d=d):
                    for j in range(nb):
                        c = c0 + j
                        ins = h.matmul(pz[:, j, :], lhsT=aT[d][:, c * GCH:(c + 1) * GCH], rhs=wa[:, d, :], start=True, stop=True)
                    return ins
                S.op("pe", mmz, reads=[aT[d].r, wa.r], writes=[b.r])
                S.op("act", lambda h, pz=pz, nb=nb: h.activation(out=e1[:, 0:nb, :], in_=pz[:, 0:nb, :], func=AF.Exp, scale=-1.0), reads=[b.r], writes=[e1.r])
                S.op("act", lambda h, c0=c0, nb=nb: h.activation(out=sp[:, c0:c0 + nb, :], in_=e1[:, 0:nb, :], func=AF.Ln, bias=1.0),
                     reads=[e1.r], writes=[sp.r], join=(c0 > 0))
            for g in range(9):
                c0 = g * (256 // GCH)
                b = bk[2 + g % 2]
                pb = b[0:64, :].rearrange("p (a b) -> p a b", b=256)

                def mmb(h, pb=pb, c0=c0, ui=ui):
                    for hh in range(2):
                        for j in range(256 // GCH):
                            ins = h.matmul(pb[:, hh, j * GCH:(j + 1) * GCH], lhsT=sp[:, c0 + j, hh * 64:(hh + 1) * 64], rhs=U[:, ui, :], start=True, stop=True)
                    return ins
                S.op("pe", mmb, reads=[sp.r, U.r], writes=[b.r])
                S.op("act", lambda h, pb=pb: h.activation(out=tq[:], in_=pb, func=AF.Exp), reads=[b.r], writes=[tq.r])
                S.op("act", lambda h, pb=pb: h.activation(out=tk[:], in_=pb, func=AF.Exp, scale=-1.0), reads=[b.r], writes=[tk.r])
                cs = slice(g * 256, g * 256 + 256)
                S.op("dve", lambda h, cs=cs, d=d: h.scalar_tensor_tensor(
                    out=qin[d][:, :, cs], in0=tq[:], scalar=0.125, in1=qT[:, :, cs], op0=ALU.mult, op1=ALU.mult),
                    reads=[tq.r, qT.r], writes=[qin[d].r], join=True)
                S.op("pool", lambda h, cs=cs, d=d: h.tensor_tensor(out=kin[d][:, :, cs], in0=tk[:], in1=kT[:, :, cs], op=ALU.mult),
                     reads=[tk.r, kT.r], writes=[kin[d].r], join=True)
            for bi_, (c0, nb) in enumerate(zb):
                b = bk[4 + bi_ % 2]
                pc = b[0:GCH, :].rearrange("p (a b) -> p a b", b=128)

                def mmc(h, pc=pc, c0=c0, nb=nb, ui2=ui2):
                    for j in range(nb):
                        ins = h.matmul(pc[:, j, :], lhsT=U[:, ui2, :], rhs=sp[:, c0 + j, :], start=True, stop=True)
                    return ins
                S.op("pe", mmc, reads=[sp.r, U.r], writes=[b.r])
                S.op("act", lambda h, pc=pc, nb=nb: h.activation(out=e1[:, 0:nb, :], in_=pc[:, 0:nb, :], func=AF.Exp), reads=[b.r], writes=[e1.r])
                S.op("dve", lambda h, c0=c0, nb=nb, d=d: h.tensor_tensor(out=kte[d][:, c0:c0 + nb, :], in0=e1[:, 0:nb, :], in1=kt[:, c0:c0 + nb, :], op=ALU.mult),
                     reads=[e1.r, kt.r], writes=[kte[d].r], join=True)
            b = bk[4]
            pd = b[0:64, 0:2 * GNC].rearrange("p (a b) -> p a b", b=GNC)

            def mmd(h, pd=pd):
                for hh in range(2):
                    for c in range(GNC):
                        ins = h.matmul(pd[:, hh, c:c + 1], lhsT=sp[:, c, hh * 64:(hh + 1) * 64], rhs=U[:, 0, GCH - 1:GCH], start=True, stop=True)
                return ins
            S.op("pe", mmd, reads=[sp.r, U.r], writes=[b.r])
            S.op("act", lambda h, pd=pd, d=d: h.activation(out=dec[d][:], in_=pd, func=AF.Exp), reads=[b.r], writes=[dec[d].r])
        order = [list(range(GNC)), list(range(GCC - 1, -1, -1)) + list(range(GNC - 1, GCC - 1, -1))]
        psA = [bk[0], bk[3]]
        psO = [bk[1], bk[4]]
        psS = [bk[2], bk[5]]
        for step in range(GNC):
            cc = [order[0][step], order[1][step]]
            for d in range(2):
                c = cc[d]

                def mm1(h, c=c, d=d):
                    cs = slice(c * GCH, (c + 1) * GCH)
                    for hh in range(2):
                        ins = h.matmul(psA[d][0:GCH, hh * GCH:(hh + 1) * GCH], lhsT=kin[d][:, hh, cs], rhs=qin[d][:, hh, cs], start=True, stop=True)
                    return ins
                S.op("pe", mm1, reads=[kin[d].r, qin[d].r], writes=[psA[d].r])
            for d in range(2):
                c = cc[d]

                def mm3(h, c=c, d=d):
                    for hh in range(2):
                        ins = h.matmul(psS[d][0:64, hh * 128:(hh + 1) * 128], lhsT=kte[d][:, c, hh * 64:(hh + 1) * 64],
                                       rhs=vt[:, c, hh * 128:(hh + 1) * 128], start=True, stop=True)
                    return ins
                S.op("pe", mm3, reads=[kte[d].r, vt.r], writes=[psS[d].r])
            for d in range(2):
                S.op("dve", lambda h, d=d: h.tensor_tensor(out=at[d][:], in0=psA[d][0:GCH, 0:2 * GCH].rearrange("p (a b) -> p a b", b=GCH),
                                                           in1=MK[:, d, :, :], op=ALU.mult),
                     reads=[psA[d].r, MK.r], writes=[at[d].r])
            for d in range(2):
                c = cc[d]

                def mm2(h, c=c, d=d):
                    cs = slice(c * GCH, (c + 1) * GCH)
                    for hh in range(2):
                        h.matmul(psO[d][0:GCH, hh * 128:(hh + 1) * 128], lhsT=at[d][:, hh, :], rhs=vt[:, c, hh * 128:(hh + 1) * 128], start=True, stop=False)
                        ins = h.matmul(psO[d][0:GCH, hh * 128:(hh + 1) * 128], lhsT=qin[d][:, hh, cs], rhs=Sbf[d][:, hh, :], start=False, stop=True)
                    return ins
                S.op("pe", mm2, reads=[at[d].r, vt.r, qin[d].r, Sbf[d].r], writes=[psO[d].r])
            for d in range(2):
                c = cc[d]
                S.op("dve", lambda h, c=c, d=d: h.tensor_tensor(out=oacc[:, c, :], in0=oacc[:, c, :], in1=psO[d][0:GCH, 0:256], op=ALU.add),
                     reads=[psO[d].r, ores[c]], writes=[ores[c]])
                for hh in range(2):
                    S.op("dve", lambda h, c=c, d=d, hh=hh: h.scalar_tensor_tensor(
                        out=S32[d][:, hh, :], in0=S32[d][:, hh, :], scalar=dec[d][:, hh, c:c + 1], in1=psS[d][0:64, hh * 128:(hh + 1) * 128],
                        op0=ALU.mult, op1=ALU.add), reads=[S32[d].r, dec[d].r, psS[d].r], writes=[S32[d].r])
                S.op("act", lambda h, d=d: h.copy(out=Sbf[d][:], in_=S32[d][:]), reads=[S32[d].r], writes=[Sbf[d].r])
        NG = 256 // GCH
        sqb = P.sb("sqb", [GCH, NG, 256], F32)
        srb = P.sb("srb", [GCH, NG, 256], F32)
        yab = P.sb("yab", [GCH, NG, 256], BF16)
        rtb = [P.sb("rtb%d" % i, [GCH, NG, 256], BF16) for i in range(2)]
        rs = P.sb("rs", [GCH, 2 * NG, 3], F32)
        brs = P.sb("brs", [128, 2, 2304], BF16)
        for g in range(9):
            c0 = g * NG
            f0 = cf if g == 0 else lf + (g - 1) * 256
            rt = rtb[g % 2]
            S.dma("sp", lambda h, rt=rt, f0=f0: h.dma_start(
                out=rt[:], in_=self.PTOK[f0:f0 + 256, PT_RG + hp * 256:PT_RG + hp * 256 + 256].rearrange("(c p) n -> p c n", p=GCH)), writes=[rt.r])
            rr = ores[c0:c0 + NG]
            ov = oacc[:, c0:c0 + NG, :]
            S.op("dve", lambda h, ov=ov: h.tensor_tensor(out=sqb[:], in0=ov, in1=ov, op=ALU.mult), reads=rr, writes=[sqb.r])
            S.op("dve", lambda h: h.tensor_reduce(out=rs[:, :, 0], in_=sqb[:].rearrange("p c (a b) -> p (c a) b", b=128),
                                                  axis=AX.X, op=ALU.add), reads=[sqb.r], writes=[rs.r])
            S.op("dve", lambda h: h.tensor_scalar(out=rs[:, :, 1], in0=rs[:, :, 0], scalar1=1.0 / 128, scalar2=EPS,
                                                  op0=ALU.mult, op1=ALU.add), reads=[rs.r], writes=[rs.r])
            S.op("act", lambda h: h.sqrt(out=rs[:, :, 2], in_=rs[:, :, 1]), reads=[rs.r], writes=[rs.r])
            S.op("dve", lambda h: h.reciprocal(out=rs[:, :, 1], in_=rs[:, :, 2]), reads=[rs.r], writes=[rs.r])
            ov3 = ov.rearrange("p c (a b) -> p (c a) b", b=128)
            sq3 = sqb[:].rearrange("p c (a b) -> p (c a) b", b=128)
            S.op("dve", lambda h, ov3=ov3, sq3=sq3: h.tensor_tensor(out=sq3, in0=ov3, in1=rs[:, :, 1:2].broadcast_to([GCH, 2 * NG, 128]), op=ALU.mult),
                 reads=rr + [rs.r], writes=[sqb.r])
            S.op("pool", lambda h, sq3=sq3: h.tensor_tensor(out=sq3, in0=sq3, in1=gbc[:, :].unsqueeze(1).broadcast_to([GCH, 2 * NG, 128]), op=ALU.mult),
                 reads=[sqb.r, gbc.r], writes=[sqb.r])
            S.op("act", lambda h, rt=rt: h.activation(out=srb[:], in_=rt[:], func=AF.Silu), reads=[rt.r], writes=[srb.r])
            S.op("dve", lambda h: h.tensor_tensor(out=yab[:], in0=sqb[:], in1=srb[:], op=ALU.mult),
                 reads=[sqb.r, srb.r], writes=[yab.r])
            for ct in range(2):
                b = tb[ct]
                pv = b[:, 0:256]

                def trf(h, pv=pv, ct=ct):
                    for j in range(NG):
                        ins = h.transpose(pv[:, j * GCH:(j + 1) * GCH], yab[:, j, ct * 128:(ct + 1) * 128], idb[0:GCH, 0:GCH])
                    return ins
                S.op("pe", trf, reads=[yab.r, idb.r], writes=[b.r])
                S.op("act", lambda h, pv=pv, ct=ct, g=g: h.copy(out=brs[:, ct, g * 256:(g + 1) * 256], in_=pv),
                     reads=[b.r], writes=[brs.r], join=True)
        for ct in range(2):
            r0 = hp * 256 + ct * 128
            S.dma("sp", lambda h, ct=ct, r0=r0: h.dma_start(out=self.BRT[r0:r0 + 128, cf:cf + 256], in_=brs[:, ct, 0:256]), reads=[brs.r])
            S.dma("sp", lambda h, ct=ct, r0=r0: h.dma_start(out=self.BRT[r0:r0 + 128, lf:lf + 2048], in_=brs[:, ct, 256:2304]), reads=[brs.r])


Builder._gla_phase = _gla_phase


def _p_gla(self, l):
    for s in range(NS):
        for hp in range(2):
            try:
                self._gla_phase(l, s, hp)
            except _Stop:
                return


Builder.p_gla = _p_gla


def _p_sgu(self, l):
    S = self.S
    with self.phase() as P:
        idb = P.sb("idb", [128, 128], BF16)
        wsT = P.sb("wsT", [128, 512], BF16)
        bs = P.sb("bs", [128, 4], F32)
        lng = P.sb("lng", [128, 512], F32)
        lnb = P.sb("lnb", [128, 512], F32)
        S.dma("sp", lambda h: h.dma_start(out=idb[:], in_=self.ident_b), writes=[idb.r])
        S.dma("pool", lambda h: h.dma_start(out=wsT[:], in_=self.sgu_wsT[l]), writes=[wsT.r])
        S.dma("sp", lambda h: h.dma_start(out=bs[:], in_=self.sgu_bs[l]), writes=[bs.r])
        S.dma("sp", lambda h: h.dma_start(out=lng[:], in_=self.sgu_lng[l].broadcast_to([128, 512])), writes=[lng.r])
        S.dma("sp", lambda h: h.dma_start(out=lnb[:], in_=self.sgu_lnb[l].broadcast_to([128, 512])), writes=[lnb.r])
        ND = 6
        uv = [P.sb("uv%d" % i, [128, 1024], BF16) for i in range(2)]
        gv = [P.sb("gv%d" % i, [128, 512], F32) for i in range(2)]
        gu = [P.sb("gu%d" % i, [128, 512], F32) for i in range(ND)]
        xc = [P.sb("xc%d" % i, [128, 512], F32) for i in range(ND)]
        sq = [P.sb("sq%d" % i, [128, 512], F32) for i in range(ND)]
        st = [P.sb("st%d" % i, [128, 6], F32) for i in range(ND)]
        vn = [P.sb("vn%d" % i, [128, 512], BF16) for i in range(2)]
        yd = [P.sb("yd%d" % i, [128, 512], BF16) for i in range(2)]
        stg = [P.sb("stg%d" % i, [128, 4, 512], BF16) for i in range(2)]
        pm = [P.ps("pm%d" % i, [128, 512]) for i in range(2)]
        pt = [P.ps("pt%d" % i, [128, 4, 128], BF16) for i in range(2)]
        tiles = list(range(NTL)) if l == 0 else list(range(32))

        def stA(i):
            tt = tiles[i]
            t_ = uv[i % 2]
            st_ = st[i % ND]
            S.dma("sp", lambda h: h.dma_start(out=t_[:], in_=self.PTOK[tt * 128:(tt + 1) * 128, PT_U:PT_U + 1024]), writes=[t_.r])
            S.op("act", lambda h: h.activation(out=gv[i % 2][:], in_=t_[:, 512:1024], func=AF.Gelu_apprx_tanh, accum_out=st_[:, 0:1]),
                 reads=[t_.r], writes=[gv[i % 2].r, st_.r])
            S.op("act", lambda h: h.activation(out=gu[i % ND][:], in_=t_[:, 0:512], func=AF.Gelu_apprx_tanh), reads=[t_.r], writes=[gu[i % ND].r])
            S.op("dve", lambda h: h.tensor_scalar(out=st_[:, 1:2], in0=st_[:, 0:1], scalar1=-1.0 / 512, scalar2=None, op0=ALU.mult),
                 reads=[st_.r], writes=[st_.r])
            S.op("dve", lambda h: h.tensor_scalar(out=xc[i % ND][:], in0=gv[i % 2][:], scalar1=st_[:, 1:2], scalar2=None, op0=ALU.add),
                 reads=[gv[i % 2].r, st_.r], writes=[xc[i % ND].r])

        def stA2(i):
            st_ = st[i % ND]
            S.op("act", lambda h: h.activation(out=sq[i % ND][:], in_=xc[i % ND][:], func=AF.Square, accum_out=st_[:, 2:3]), reads=[xc[i % ND].r], writes=[sq[i % ND].r, st_.r])
            S.op("dve", lambda h: h.tensor_scalar(out=st_[:, 3:4], in0=st_[:, 2:3], scalar1=1.0 / 512, scalar2=EPS, op0=ALU.mult, op1=ALU.add),
                 reads=[st_.r], writes=[st_.r])

        def stA3(i):
            st_ = st[i % ND]
            S.op("act", lambda h: h.sqrt(out=st_[:, 4:5], in_=st_[:, 3:4]), reads=[st_.r], writes=[st_.r])
            S.op("dve", lambda h: h.reciprocal(out=st_[:, 5:6], in_=st_[:, 4:5]), reads=[st_.r], writes=[st_.r])

        def stB(i):
            st_ = st[i % ND]
            S.op("dve", lambda h: h.scalar_tensor_tensor(out=sq[i % ND][:], in0=xc[i % ND][:], scalar=st_[:, 5:6], in1=lng[:], op0=ALU.mult, op1=ALU.mult),
                 reads=[xc[i % ND].r, st_.r, lng.r], writes=[sq[i % ND].r])
            S.op("pool", lambda h: h.tensor_tensor(out=vn[i % 2][:], in0=sq[i % ND][:], in1=lnb[:], op=ALU.add), reads=[sq[i % ND].r, lnb.r], writes=[vn[i % 2].r])
            p = pm[i % 2]
            v_ = vn[i % 2]

            def mm(h):
                for g in range(4):
                    ins = h.matmul(p[:, g * 128:(g + 1) * 128], lhsT=wsT[:, g * 128:(g + 1) * 128], rhs=v_[:, g * 128:(g + 1) * 128], start=True, stop=True)
                return ins
            S.op("pe", mm, reads=[wsT.r, v_.r], writes=[p.r])

        def stC(i):
            tt = tiles[i]
            p = pm[i % 2]
            y_ = yd[i % 2]
            g_ = gu[i % ND]
            for g in range(4):
                S.op("dve", lambda h, g=g: h.scalar_tensor_tensor(
                    out=y_[:, g * 128:(g + 1) * 128], in0=p[:, g * 128:(g + 1) * 128], scalar=bs[:, g:g + 1], in1=g_[:, g * 128:(g + 1) * 128],
                    op0=ALU.add, op1=ALU.mult), reads=[p.r, bs.r, g_.r], writes=[y_.r], join=(g > 0))
            q = pt[i % 2]

            def tr(h):
                for g in range(4):
                    ins = h.transpose(q[:, g, :], y_[:, g * 128:(g + 1) * 128], idb[:])
                return ins
            S.op("pe", tr, reads=[y_.r, idb.r], writes=[q.r])
            sg = stg[(i // 4) % 2]
            S.op("act", lambda h: h.copy(out=sg[:, :, (i % 4) * 128:(i % 4 + 1) * 128], in_=q[:]), reads=[q.r], writes=[sg.r], join=True)
            if i % 4 == 3:
                f0 = (tt - 3) * 128
                S.dma("sp", lambda h: h.dma_start(
                    out=self.BRT[1536:2048, f0:f0 + 512].rearrange("(g p) n -> p g n", p=128), in_=sg[:]), reads=[sg.r])
        pipeline(len(tiles), [stA, stA2, stA3, stB, stC])


Builder.p_sgu = _p_sgu


def _swa_phase(self, l, s):
    S = self.S
    cf = 4096 + s * 256
    lf = s * 2048
    need_ctx = (l == 0)
    with self.phase() as P:
        idb = P.sb("idb", [128, 128], BF16)
        S.dma("sp", lambda h: h.dma_start(out=idb[:], in_=self.ident_b), writes=[idb.r])
        m01 = P.sb("m01", [128, 2, 512], BF16)
        S.dma("sp", lambda h: h.dma_start(out=m01[:], in_=self.swa_negm.rearrange("p (a b) -> p a b", b=512)), writes=[m01.r])
        esk = P.sb("esk", [128, 8], F32)
        S.dma("sp", lambda h: h.dma_start(out=esk[:], in_=self.swa_sink[l].broadcast_to([128, 8])), writes=[esk.r])
        S.op("act", lambda h: h.activation(out=esk[:], in_=esk[:], func=AF.Exp), reads=[esk.r], writes=[esk.r])
        qT = P.sb("qT", [64, 8, 2048 + 256], BF16)
        kT = P.sb("kT", [64, 2, 2048 + 256], BF16)
        vaug = P.sb("vaug", [128, 18, 2, 65], BF16)
        msr = Res()
        S.op("pool", lambda h: h.memset(vaug[:], 1.0), writes=[vaug.r, msr])
        for g in range(2):
            S.dma("sp", lambda h, g=g: h.dma_start(out=vaug[:, 0:16, g, 0:64],
                                                   in_=self.PTOK[lf:lf + 2048, PT_VS + g * 64:PT_VS + (g + 1) * 64].rearrange("(t p) d -> p t d", p=128)),
                  reads=[msr], writes=[vaug.r], join=True)
            S.dma("sp", lambda h, g=g: h.dma_start(out=vaug[:, 16:18, g, 0:64],
                                                   in_=self.PTOK[cf:cf + 256, PT_VS + g * 64:PT_VS + (g + 1) * 64].rearrange("(t p) d -> p t d", p=128)),
                  reads=[msr], writes=[vaug.r], join=True)
            S.dma("sp", lambda h, g=g: h.dma_start(out=kT[:, g, 2048:2304], in_=self.PFM[FM_KS + g * 64:FM_KS + (g + 1) * 64, cf:cf + 256]),
                  writes=[kT.r], join=True)
        qk = [P.sb("qk%d" % i, [128, 640], BF16) for i in range(2)]
        cs = [P.sb("cs%d" % i, [128, 64], F32) for i in range(2)]
        t1 = P.sb("t1", [128, 10, 32], F32)
        t2 = P.sb("t2", [128, 10, 32], F32)
        t3 = P.sb("t3", [128, 10, 32], F32)
        t4 = P.sb("t4", [128, 10, 32], F32)
        qr = [P.sb("qr%d" % i, [128, 10, 2, 32], BF16) for i in range(2)]
        tqb = P.ps("tqb", [128, 8, 128], BF16)
        tkb = P.ps("tkb", [128, 2, 128], BF16)
        ntile = 18 if need_ctx else 16
        for i in range(ntile):
            x = qk[i % 2]
            f0 = lf + i * 128 if i < 16 else cf + (i - 16) * 128
            S.dma("sp", lambda h, x=x, f0=f0: h.dma_start(out=x[:], in_=self.PTOK[f0:f0 + 128, PT_QS:PT_QS + 640]), writes=[x.r])
            if i < 16:
                c_ = cs[i % 2]
                S.dma("sp", lambda h, c_=c_, i=i: h.dma_start(out=c_[:], in_=self.rope_cs[i * 128:(i + 1) * 128, :]), writes=[c_.r])
                xv = x[:].rearrange("p (h a d) -> p h a d", a=2, d=32)
                x1 = xv[:, :, 0, :]
                x2 = xv[:, :, 1, :]
                cosb = c_[:, 0:32].unsqueeze(1).broadcast_to([128, 10, 32])
                sinb = c_[:, 32:64].unsqueeze(1).broadcast_to([128, 10, 32])
                q_ = qr[i % 2]
                S.op("dve", lambda h, x1=x1, cosb=cosb: h.tensor_tensor(out=t1[:], in0=x1, in1=cosb, op=ALU.mult), reads=[x.r, c_.r], writes=[t1.r])
                S.op("pool", lambda h, x2=x2, sinb=sinb: h.tensor_tensor(out=t2[:], in0=x2, in1=sinb, op=ALU.mult), reads=[x.r, c_.r], writes=[t2.r])
                S.op("pool", lambda h, x1=x1, sinb=sinb: h.tensor_tensor(out=t3[:], in0=x1, in1=sinb, op=ALU.mult), reads=[x.r, c_.r], writes=[t3.r])
                S.op("dve", lambda h, x2=x2, cosb=cosb: h.tensor_tensor(out=t4[:], in0=x2, in1=cosb, op=ALU.mult), reads=[x.r, c_.r], writes=[t4.r])
                S.op("dve", lambda h, q_=q_: h.tensor_tensor(out=q_[:, :, 0, :], in0=t1[:], in1=t2[:], op=ALU.subtract), reads=[t1.r, t2.r], writes=[q_.r])
                S.op("pool", lambda h, q_=q_: h.tensor_tensor(out=q_[:, :, 1, :], in0=t3[:], in1=t4[:], op=ALU.add), reads=[t3.r, t4.r], writes=[q_.r], join=True)
                src = q_[:].rearrange("p h a d -> p h (a d)")
                srcr = q_.r
            else:
                src = x[:].rearrange("p (h d) -> p h d", d=64)
                srcr = x.r

            def trq(h, src=src):
                for hd in range(8):
                    ins = h.transpose(tqb[0:64, hd, :], src[:, hd, :], idb[:])
                return ins
            S.op("pe", trq, reads=[srcr, idb.r], writes=[tqb.r])
            S.op("act", lambda h, i=i: h.copy(out=qT[:, :, i * 128:(i + 1) * 128], in_=tqb[0:64, :, :]), reads=[tqb.r], writes=[qT.r], join=True)
            if i < 16:
                def trk(h, src=src):
                    for hd in range(2):
                        ins = h.transpose(tkb[0:64, hd, :], src[:, 8 + hd, :], idb[:])
                    return ins
                S.op("pe", trk, reads=[srcr, idb.r], writes=[tkb.r])
                S.op("act", lambda h, i=i: h.copy(out=kT[:, :, i * 128:(i + 1) * 128], in_=tkb[0:64, :, :]), reads=[tkb.r], writes=[kT.r], join=True)
        PT = [[P.sb("PT%d_%d" % (a, j), [128, 512], BF16) for j in range(5)] for a in range(3)]
        psc = [P.ps("psc%d" % i, [128, 512]) for i in range(3)]
        pso = [P.ps("pso%d" % i, [128, 4, 65]) for i in range(2)]
        pto = P.ps("pto", [128, 4, 128], BF16)
        yb = [P.sb("yb%d" % i, [128, 8, 64], BF16) for i in range(2)]
        den = P.sb("den", [128, 4], F32)
        rec = P.sb("rec", [128, 4], F32)
        stg = [P.sb("stg%d" % i, [128, 4, 512], BF16) for i in range(2)]
        items = []
        for n in range(ntile):
            for g in range(2):
                if n < 16:
                    kts = ([(n - 1, 0)] if n > 0 else []) + [(n, None)] + ([(n + 1, 1)] if n < 15 else []) + [(16, None), (17, None)]
                else:
                    kts = [(16, None), (17, None)]
                items.append((n, g, kts))
        sci = [0]

        def scores(it, a):
            n, g, kts = it
            for j, (ktile, mi) in enumerate(kts):
                p = psc[sci[0] % 3]
                sci[0] += 1
                S.op("pe", lambda h, p=p, ktile=ktile, g=g, n=n: h.matmul(
                    p[:], lhsT=kT[:, g, ktile * 128:(ktile + 1) * 128], rhs=qT[:, 4 * g:4 * g + 4, n * 128:(n + 1) * 128], start=True, stop=True),
                    reads=[kT.r, qT.r], writes=[p.r])
                t_ = PT[a][j]
                S.op("act", lambda h, p=p, t_=t_: h.activation(out=t_[:], in_=p[:], func=AF.Exp, scale=0.125), reads=[p.r], writes=[t_.r])
                if mi is not None:
                    S.op("pool", lambda h, t_=t_, mi=mi: h.tensor_tensor(out=t_[:], in0=t_[:], in1=m01[:, mi, :], op=ALU.mult),
                         reads=[t_.r, m01.r], writes=[t_.r])

        def pv(it, a, idx):
            n, g, kts = it
            po = pso[idx % 2]

            def mm(h, po=po, kts=kts, a=a, g=g):
                for hq in range(4):
                    for j, (ktile, mi) in enumerate(kts):
                        ins = h.matmul(po[:, hq, :], lhsT=PT[a][j][:, hq * 128:(hq + 1) * 128], rhs=vaug[:, ktile, g, :],
                                       start=(j == 0), stop=(j == len(kts) - 1))
                return ins
            S.op("pe", mm, reads=[PT[a][j].r for j in range(len(kts))] + [vaug.r], writes=[po.r])
            y = yb[n % 2]
            S.op("dve", lambda h, po=po, g=g: h.tensor_tensor(out=den[:], in0=po[:, :, 64], in1=esk[:, 4 * g:4 * g + 4], op=ALU.add),
                 reads=[po.r, esk.r], writes=[den.r])
            S.op("dve", lambda h: h.reciprocal(out=rec[:], in_=den[:]), reads=[den.r], writes=[rec.r])
            S.op("dve", lambda h, po=po, y=y, g=g: h.tensor_tensor(out=y[:, 4 * g:4 * g + 4, :], in0=po[:, :, 0:64],
                                                                   in1=rec[:, :].unsqueeze(2).broadcast_to([128, 4, 64]), op=ALU.mult),
                 reads=[po.r, rec.r], writes=[y.r], join=(g == 1))
            if g == 1:
                def tr(h, y=y):
                    yv = y[:].rearrange("p h d -> p (h d)")
                    for ct in range(4):
                        ins = h.transpose(pto[:, ct, :], yv[:, ct * 128:(ct + 1) * 128], idb[:])
                    return ins
                S.op("pe", tr, reads=[y.r, idb.r], writes=[pto.r])
                sg = stg[(n // 4) % 2]
                S.op("act", lambda h, sg=sg, n=n: h.copy(out=sg[:, :, (n % 4) * 128:(n % 4 + 1) * 128], in_=pto[:]), reads=[pto.r], writes=[sg.r], join=True)
                if n % 4 == 3 or n == 17:
                    if n < 16:
                        f0 = lf + (n - 3) * 128
                        S.dma("sp", lambda h, sg=sg, f0=f0: h.dma_start(
                            out=self.BRT[512:1024, f0:f0 + 512].rearrange("(g p) n -> p g n", p=128), in_=sg[:]), reads=[sg.r])
                    else:
                        S.dma("sp", lambda h, sg=sg: h.dma_start(
                            out=self.BRT[512:1024, cf:cf + 256].rearrange("(g p) n -> p g n", p=128), in_=sg[:, :, 0:256]), reads=[sg.r])

        scores(items[0], 0)
        scores(items[1], 1)
        for i, it in enumerate(items):
            if i + 2 < len(items):
                scores(items[i + 2], (i + 2) % 3)
            pv(it, i % 3, i)


Builder._swa_phase = _swa_phase


def _p_swa(self, l):
    for s in range(NS):
        self._swa_phase(l, s)


Builder.p_swa = _p_swa


def _na_phase(self, l, s):
    S = self.S
    cf = 4096 + s * 256
    lf = s * 2048
    need_ctx = (l == 0)
    with self.phase() as P:
        idb = P.sb("idb", [128, 128], BF16)
        S.dma("sp", lambda h: h.dma_start(out=idb[:], in_=self.ident_b), writes=[idb.r])
        qT = P.sb("qT", [64, 8, 2304], BF16)
        kT = P.sb("kT", [64, 8, 2304], BF16)
        for hd in range(8):
            for (t_, base) in ((qT, FM_QN), (kT, FM_KN)):
                r0 = base + hd * 64
                S.dma("sp", lambda h, t_=t_, r0=r0, hd=hd: h.dma_start(out=t_[:, hd, 0:2048], in_=self.PFM[r0:r0 + 64, lf:lf + 2048]), writes=[t_.r], join=True)
                S.dma("sp", lambda h, t_=t_, r0=r0, hd=hd: h.dma_start(out=t_[:, hd, 2048:2304], in_=self.PFM[r0:r0 + 64, cf:cf + 256]), writes=[t_.r], join=True)
        vE = P.sb("vE", [128, 18, 8, 65], BF16)
        vO = P.sb("vO", [128, 15, 8, 65], BF16)
        msE = Res()
        msO = Res()
        S.op("pool", lambda h: h.memset(vE[:], 1.0), writes=[vE.r, msE])
        S.op("pool", lambda h: h.memset(vO[:], 1.0), writes=[vO.r, msO])
        for hd in range(8):
            c0 = PT_VN + hd * 64
            S.dma("sp", lambda h, hd=hd, c0=c0: h.dma_start(out=vE[:, 0:16, hd, 0:64], in_=self.PTOK[lf:lf + 2048, c0:c0 + 64].rearrange("(t p) d -> p t d", p=128)),
                  reads=[msE], writes=[vE.r], join=True)
            S.dma("sp", lambda h, hd=hd, c0=c0: h.dma_start(out=vE[:, 16:18, hd, 0:64], in_=self.PTOK[cf:cf + 256, c0:c0 + 64].rearrange("(t p) d -> p t d", p=128)),
                  reads=[msE], writes=[vE.r], join=True)
            S.dma("sp", lambda h, hd=hd, c0=c0: h.dma_start(out=vO[:, 0:15, hd, 0:64], in_=self.PTOK[lf + 64:lf + 64 + 1920, c0:c0 + 64].rearrange("(t p) d -> p t d", p=128)),
                  reads=[msO], writes=[vO.r], join=True)
        E = P.sb("E", [128, 7168], BF16)
        gtmp = [P.sb("gtmp%d" % i, [128, 1792], F32) for i in range(2)]
        mtmp = [P.sb("mtmp%d" % i, [128, 1792], F32) for i in range(2)]
        for q4 in range(4):
            gt = gtmp[q4 % 2]
            mt = mtmp[q4 % 2]
            sl = slice(q4 * 1792, (q4 + 1) * 1792)
            S.dma("sp", lambda h, gt=gt, sl=sl: h.dma_start(out=gt[:], in_=self.na_g[l][:, sl]), writes=[gt.r])
            S.dma("sp", lambda h, mt=mt, sl=sl: h.dma_start(out=mt[:], in_=self.na_m[:, sl]), writes=[mt.r])
            S.op("act", lambda h, gt=gt: h.activation(out=gt[:], in_=gt[:], func=AF.Exp), reads=[gt.r], writes=[gt.r])
            S.op("dve", lambda h, gt=gt, mt=mt, sl=sl: h.tensor_tensor(out=E[:, sl], in0=gt[:], in1=mt[:], op=ALU.mult), reads=[gt.r, mt.r], writes=[E.r], join=True)
        Ev = E[:].rearrange("p (h r c) -> p h r c", h=8, r=14)
        PTt = [P.sb("PTt%d" % i, [128, 6, 64], BF16) for i in range(3)]
        psc = [P.ps("psc%d" % i, [128, 6, 64]) for i in range(3)]
        pso = [[P.ps("pso%d_%d" % (a, b), [64, 4, 65]) for b in range(2)] for a in range(2)]
        pto = P.ps("pto", [128, 4, 64], BF16)
        yn = P.sb("yn", [64, 8, 64], BF16)
        rec = P.sb("rec", [64, 8], F32)
        stg = [P.sb("stg%d" % i, [128, 4, 512], BF16) for i in range(2)]
        nrow = 36 if need_ctx else 32
        items = [(r, hd) for r in range(nrow) for hd in range(8)]

        def tiles_of(r):
            if r < 32:
                rs = min(max(r - 4, 0), 24)
                out = []
                for j in range(4):
                    if rs % 2 == 0:
                        out.append((rs * 64 + 128 * j, (vE, rs // 2 + j)))
                    else:
                        out.append((rs * 64 + 128 * j, (vO, (rs - 1) // 2 + j)))
                out += [(2048, (vE, 16)), (2048 + 128, (vE, 17))]
                return out, rs - r + 7
            return [(2048, (vE, 16)), (2048 + 128, (vE, 17))], None

        sci = [0]

        def scores(it, a):
            r, hd = it
            tl, ro0 = tiles_of(r)
            nt = len(tl)
            p = psc[sci[0] % 3]
            sci[0] += 1
            qc = r * 64 if r < 32 else 2048 + (r - 32) * 64

            def mm(h, p=p, tl=tl, hd=hd, qc=qc):
                for j, (kc, _) in enumerate(tl):
                    ins = h.matmul(p[:, j, :], lhsT=kT[:, hd, kc:kc + 128], rhs=qT[:, hd, qc:qc + 64], start=True, stop=True)
                return ins
            S.op("pe", mm, reads=[kT.r, qT.r], writes=[p.r])
            t_ = PTt[a]
            S.op("act", lambda h, p=p, t_=t_, nt=nt: h.activation(out=t_[:, 0:nt, :], in_=p[:, 0:nt, :], func=AF.Exp, scale=0.125), reads=[p.r], writes=[t_.r])
            if ro0 is not None:
                S.op("pool", lambda h, t_=t_, hd=hd, ro0=ro0: h.tensor_tensor(out=t_[:, 0:4, :], in0=t_[:, 0:4, :], in1=Ev[:, hd, ro0:ro0 + 7:2, :], op=ALU.mult),
                     reads=[t_.r, E.r], writes=[t_.r])

        def pv(it, a):
            r, hd = it
            tl, _ = tiles_of(r)
            po = pso[r % 2][hd // 4]

            def mm(h, po=po, tl=tl, a=a, hd=hd):
                for j, (_, (vt_, vi)) in enumerate(tl):
                    ins = h.matmul(po[:, hd % 4, :], lhsT=PTt[a][:, j, :], rhs=vt_[:, vi, hd, :], start=(j == 0), stop=(j == len(tl) - 1))
                return ins
            S.op("pe", mm, reads=[PTt[a].r, vE.r, vO.r], writes=[po.r])
            if hd % 4 == 3:
                b = hd // 4
                S.op("dve", lambda h, po=po, b=b: h.reciprocal(out=rec[:, 4 * b:4 * b + 4], in_=po[:, :, 64]), reads=[po.r], writes=[rec.r], join=(b == 1))
                S.op("dve", lambda h, po=po, b=b: h.tensor_tensor(out=yn[:, 4 * b:4 * b + 4, :], in0=po[:, :, 0:64],
                                                                  in1=rec[:, 4 * b:4 * b + 4].unsqueeze(2).broadcast_to([64, 4, 64]), op=ALU.mult),
                     reads=[po.r, rec.r], writes=[yn.r], join=(b == 1))
            if hd == 7:
                def tr(h):
                    yv = yn[:].rearrange("p h d -> p (h d)")
                    for ct in range(4):
                        ins = h.transpose(pto[:, ct, :], yv[:, ct * 128:(ct + 1) * 128], idb[0:64, 0:64])
                    return ins
                S.op("pe", tr, reads=[yn.r, idb.r], writes=[pto.r])
                sg = stg[(r // 8) % 2]
                S.op("act", lambda h, sg=sg, r=r: h.copy(out=sg[:, :, (r % 8) * 64:(r % 8 + 1) * 64], in_=pto[:]), reads=[pto.r], writes=[sg.r], join=True)
                if r % 8 == 7 and r < 32:
                    f0 = lf + (r - 7) * 64
                    S.dma("sp", lambda h, sg=sg, f0=f0: h.dma_start(
                        out=self.BRT[1024:1536, f0:f0 + 512].rearrange("(g p) n -> p g n", p=128), in_=sg[:]), reads=[sg.r])
                if r == 35:
                    S.dma("sp", lambda h, sg=sg: h.dma_start(
                        out=self.BRT[1024:1536, cf:cf + 256].rearrange("(g p) n -> p g n", p=128), in_=sg[:, :, 0:256]), reads=[sg.r])

        scores(items[0], 0)
        scores(items[1], 1)
        for i, it in enumerate(items):
            if i + 2 < len(items):
                scores(items[i + 2], (i + 2) % 3)
            pv(it, i % 3)


Builder._na_phase = _na_phase


def _p_na(self, l):
    for s in range(NS):
        self._na_phase(l, s)


Builder.p_na = _p_na


def _p_merge1(self, l):
    S = self.S
    ntok = NF if l == 0 else NS * T
    blocks = [(f0, min(1024, ntok - f0)) for f0 in range(0, ntok, 1024)]
    for (f0, TB) in blocks:
        with self.phase() as P:
            hT = P.sb("hT", [128, 16, TB], BF16)
            bT = P.sb("bT", [128, 16, TB], BF16)
            for kq in range(4):
                S.dma("sp", lambda h, kq=kq: h.dma_start(out=hT[:, kq * 4:(kq + 1) * 4, :],
                                                        in_=self.HT[kq * 512:(kq + 1) * 512, f0:f0 + TB].rearrange("(kc p) n -> p kc n", p=128)),
                      writes=[hT.r], join=True)
                S.dma("sp", lambda h, kq=kq: h.dma_start(out=bT[:, kq * 4:(kq + 1) * 4, :],
                                                        in_=self.BRT[kq * 512:(kq + 1) * 512, f0:f0 + TB].rearrange("(kc p) n -> p kc n", p=128)),
                      writes=[bT.r], join=True)
            bg = P.sb("bg", [128, 64], F32)
            S.dma("sp", lambda h: h.dma_start(out=bg[:], in_=self.b_gate_p[l]), writes=[bg.r])
            Wg = [P.sb("Wg%d" % i, [128, 4, 16, 128], BF16) for i in range(2)]
            Wb = [P.sb("Wb%d" % i, [128, 4, 4, 128], BF16) for i in range(2)]
            sg = [P.sb("sg%d" % i, [128, 512], F32) for i in range(2)]
            tm = [P.sb("tm%d" % i, [128, 512], F32) for i in range(2)]
            acc = P.sb("acc", [128, 512], F32)
            accs = [P.sb("accs%d" % i, [128, TB], BF16) for i in range(2)]
            pg = [P.ps("pg%d" % i, [128, 512]) for i in range(3)]
            pb = [P.ps("pb%d" % i, [128, 512]) for i in range(3)]

            def loadw(j):
                wg = Wg[j % 2]
                wb = Wb[j % 2]
                for n in range(4):
                    S.dma("pool", lambda h, wg=wg, n=n, j=j: h.dma_start(
                        out=wg[:, n, :, :], in_=self.w_gate[l][n][:, j * 128:(j + 1) * 128].rearrange("(kc p) c -> p kc c", p=128)),
                        writes=[wg.r], join=(n > 0))
                    S.dma("pool", lambda h, wb=wb, n=n, j=j: h.dma_start(
                        out=wb[:, n, :, :], in_=self.w_branch[l][n][:, j * 128:(j + 1) * 128].rearrange("(kc p) c -> p kc c", p=128)),
                        writes=[wb.r], join=(n > 0))
            loadw(0)
            k = 0
            for j in range(16):
                if j + 1 < 16:
                    loadw(j + 1)
                wg = Wg[j % 2]
                wb = Wb[j % 2]
                ao = accs[j % 2]
                for tb in range(TB // 512):
                    cs = slice(tb * 512, (tb + 1) * 512)
                    for n in range(4):
                        g_ = pg[k % 3]
                        b_ = pb[k % 3]
                        s_ = sg[k % 2]
                        t_ = tm[k % 2]
                        k += 1

                        def mmg(h, g_=g_, wg=wg, n=n, cs=cs):
                            for kc in range(16):
                                ins = h.matmul(g_[:], lhsT=wg[:, n, kc, :], rhs=hT[:, kc, cs], start=(kc == 0), stop=(kc == 15))
                            return ins
                        S.op("pe", mmg, reads=[wg.r, hT.r], writes=[g_.r])

                        def mmb(h, b_=b_, wb=wb, n=n, cs=cs):
                            for kc in range(4):
                                ins = h.matmul(b_[:], lhsT=wb[:, n, kc, :], rhs=bT[:, n * 4 + kc, cs], start=(kc == 0), stop=(kc == 3))
                            return ins
                        S.op("pe", mmb, reads=[wb.r, bT.r], writes=[b_.r])
                        S.op("act", lambda h, g_=g_, s_=s_, n=n, j=j: h.activation(out=s_[:], in_=g_[:], func=AF.Sigmoid, bias=bg[:, n * 16 + j:n * 16 + j + 1]),
                             reads=[g_.r, bg.r], writes=[s_.r])
                        if n == 0:
                            S.op("dve", lambda h, s_=s_, b_=b_: h.tensor_tensor(out=acc[:], in0=s_[:], in1=b_[:], op=ALU.mult), reads=[s_.r, b_.r], writes=[acc.r])
                        else:
                            S.op("dve", lambda h, s_=s_, b_=b_, t_=t_: h.tensor_tensor(out=t_[:], in0=s_[:], in1=b_[:], op=ALU.mult), reads=[s_.r, b_.r], writes=[t_.r])
                            if n < 3:
                                S.op("pool", lambda h, t_=t_: h.tensor_tensor(out=acc[:], in0=acc[:], in1=t_[:], op=ALU.add), reads=[acc.r, t_.r], writes=[acc.r])
                            else:
                                S.op("pool", lambda h, t_=t_, ao=ao, cs=cs: h.tensor_tensor(out=ao[:, cs], in0=acc[:], in1=t_[:], op=ALU.add),
                                     reads=[acc.r, t_.r], writes=[ao.r], join=(tb > 0))
                S.dma("sp", lambda h, ao=ao, j=j: h.dma_start(out=self.ACCT[j * 128:(j + 1) * 128, f0:f0 + TB], in_=ao[:]), reads=[ao.r])


Builder.p_merge1 = _p_merge1


def _p_merge2(self, l):
    S = self.S
    src = self.xin if l == 0 else self.X
    ntile = NTL if l == 0 else 32
    with self.phase() as P:
        wo = P.sb("wo", [128, 16, D], BF16)
        for kq in range(4):
            S.dma("pool", lambda h, kq=kq: h.dma_start(out=wo[:, kq * 4:(kq + 1) * 4, :],
                                                      in_=self.w_out[l][kq * 512:(kq + 1) * 512, :].rearrange("(kc p) n -> p kc n", p=128)),
                  writes=[wo.r], join=True)
        M2 = self.bc_load(P, "M2", l, 2)
        at = [P.sb("at%d" % i, [128, 16, 128], BF16) for i in range(2)]
        xt = [P.sb("xt%d" % i, [128, D], F32) for i in range(2)]
        xo = [P.sb("xo%d" % i, [128, D], F32) for i in range(2)]
        tmp = [P.sb("tmp%d" % i, [128, 512], F32) for i in range(2)]
        py = [P.ps("py%d" % i, [128, 512]) for i in range(4)]
        kk = [0]

        def stA(tt):
            a = at[tt % 2]
            x = xt[tt % 2]
            S.dma("sp", lambda h: h.dma_start(out=a[:], in_=self.ACCT[:, tt * 128:(tt + 1) * 128].rearrange("(kc p) n -> p kc n", p=128)), writes=[a.r])
            S.dma("sp", lambda h: h.dma_start(out=x[:], in_=src[tt * 128:(tt + 1) * 128, :]), writes=[x.r])

        def stB(tt):
            a = at[tt % 2]
            x = xt[tt % 2]
            o = xo[tt % 2]
            r = tile_row(tt)
            for cg in range(4):
                p = py[kk[0] % 4]
                t_ = tmp[kk[0] % 2]
                kk[0] += 1
                cs = slice(cg * 512, (cg + 1) * 512)

                def mm(h, p=p, cs=cs):
                    for kc in range(16):
                        ins = h.matmul(p[:], lhsT=a[:, kc, :], rhs=wo[:, kc, cs], start=(kc == 0), stop=(kc == 15))
                    return ins
                S.op("pe", mm, reads=[a.r, wo.r], writes=[p.r])
                S.op("dve", lambda h, p=p, t_=t_, cs=cs: h.tensor_tensor(out=t_[:], in0=p[:], in1=M2[:, r, cs], op=ALU.mult), reads=[p.r, M2.r], writes=[t_.r])
                S.op("pool", lambda h, t_=t_, cs=cs: h.tensor_tensor(out=o[:, cs], in0=x[:, cs], in1=t_[:], op=ALU.add),
                     reads=[t_.r, x.r], writes=[o.r], join=(cg > 0))
            S.dma("act", lambda h: h.dma_start(out=self.X[tt * 128:(tt + 1) * 128, :], in_=o[:]), reads=[o.r])
        pipeline(ntile, [stA, stB])


Builder.p_merge2 = _p_merge2


def _p_norm2(self, l):
    S = self.S
    ntile = NTL if l == 0 else 32
    with self.phase() as P:
        G = self.bc_load(P, "G", l, 4)
        SH = self.bc_load(P, "SH", l, 3)
        idf = P.sb("idf", [128, 128], F32)
        S.dma("sp", lambda h: h.dma_start(out=idf[:], in_=self.ident_f), writes=[idf.r])
        wr = P.sb("wr", [128, 16, 16], F32)
        S.dma("sp", lambda h: h.dma_start(out=wr[:], in_=self.w_router[l].rearrange("(kc p) e -> p kc e", p=128)), writes=[wr.r])
        xt = [P.sb("xt%d" % i, [128, D], F32) for i in range(4)]
        st = [P.sb("st%d" % i, [128, 4], F32) for i in range(4)]
        sq = [P.sb("sq%d" % i, [128, D], BF16) for i in range(2)]
        tmp = [P.sb("tmp%d" % i, [128, D], F32) for i in range(2)]
        h2 = [P.sb("h2%d" % i, [128, D], F32) for i in range(2)]
        h2b = [P.sb("h2b%d" % i, [128, D], BF16) for i in range(2)]
        h2T = [P.sb("h2T%d" % i, [128, 16, 128], F32) for i in range(2)]
        sm = [P.sb("sm%d" % i, [128, 4], F32) for i in range(2)]
        ex = [P.sb("ex%d" % i, [128, 16], F32) for i in range(2)]
        aff = [P.sb("aff%d" % i, [128, 16], F32) for i in range(2)]
        affs = P.sb("affs", [16, 2, T + LC], F32)
        pt = [P.ps("pt%d" % i, [128, 4, 128]) for i in range(4)]
        pl = [P.ps("pl%d" % i, [128, 16]) for i in range(2)]
        pa = [P.ps("pa%d" % i, [16, 128]) for i in range(2)]

        def stA(tt):
            x = xt[tt % 4]
            S.dma("sp", lambda h: h.dma_start(out=x[:], in_=self.X[tt * 128:(tt + 1) * 128, :]), writes=[x.r])
            self.norm_a(x, st[tt % 4], sq[tt % 2])

        def stA2(tt):
            self.norm_a2(st[tt % 4])

        def stB(tt):
            hh = h2[tt % 2]
            hb = h2b[tt % 2]
            self.norm_b(xt[tt % 4], st[tt % 4], G, SH, tile_row(tt), tmp[tt % 2], hh)
            S.op("act", lambda h: h.copy(out=hb[:], in_=hh[:]), reads=[hh.r], writes=[hb.r])
            S.dma("act", lambda h: h.dma_start(out=self.H2[tt * 128:(tt + 1) * 128, :], in_=hb[:]), reads=[hb.r])

        def stC(tt):
            hh = h2[tt % 2]
            hT = h2T[tt % 2]
            for q in range(4):
                p = pt[q]

                def tr(h, p=p, q=q):
                    for j in range(4):
                        kc = q * 4 + j
                        ins = h.transpose(p[:, j, :], hh[:, kc * 128:(kc + 1) * 128], idf[:])
                    return ins
                S.op("pe", tr, reads=[hh.r, idf.r], writes=[p.r])
                if q % 2 == 0:
                    S.op("act", lambda h, p=p, q=q: h.copy(out=hT[:, q * 4:(q + 1) * 4, :], in_=p[:]), reads=[p.r], writes=[hT.r], join=True)
                else:
                    S.op("dve", lambda h, p=p, q=q: h.tensor_copy(out=hT[:, q * 4:(q + 1) * 4, :], in_=p[:]), reads=[p.r], writes=[hT.r], join=True)
            p_l = pl[tt % 2]

            def mml(h):
                for kc in range(16):
                    ins = h.matmul(p_l[:], lhsT=hT[:, kc, :], rhs=wr[:, kc, :], start=(kc == 0), stop=(kc == 15))
                return ins
            S.op("pe", mml, reads=[hT.r, wr.r], writes=[p_l.r])

        def stD(tt):
            p_l = pl[tt % 2]
            p_a = pa[tt % 2]
            sm_ = sm[tt % 2]
            ex_ = ex[tt % 2]
            af_ = aff[tt % 2]
            S.op("dve", lambda h: h.reduce_max(out=sm_[:, 0:1], in_=p_l[:], axis=AX.X), reads=[p_l.r], writes=[sm_.r])
            S.op("dve", lambda h: h.tensor_scalar(out=sm_[:, 1:2], in0=sm_[:, 0:1], scalar1=-1.0, scalar2=None, op0=ALU.mult), reads=[sm_.r], writes=[sm_.r])
            S.op("act", lambda h: h.activation(out=ex_[:], in_=p_l[:], func=AF.Exp, bias=sm_[:, 1:2], accum_out=sm_[:, 2:3]), reads=[p_l.r, sm_.r], writes=[ex_.r, sm_.r])
            S.op("dve", lambda h: h.reciprocal(out=sm_[:, 3:4], in_=sm_[:, 2:3]), reads=[sm_.r], writes=[sm_.r])
            S.op("dve", lambda h: h.tensor_scalar(out=af_[:], in0=ex_[:], scalar1=sm_[:, 3:4], scalar2=None, op0=ALU.mult), reads=[ex_.r, sm_.r], writes=[af_.r])
            S.op("pe", lambda h: h.transpose(p_a[:], af_[:], idf[:]), reads=[af_.r, idf.r], writes=[p_a.r])
            if tt < 32:
                s_, c0 = tt // 16, (tt % 16) * 128
            else:
                s_, c0 = (tt - 32) // 2, T + ((tt - 32) % 2) * 128
            S.op("act", lambda h: h.copy(out=affs[:, s_, c0:c0 + 128], in_=p_a[:]), reads=[p_a.r], writes=[affs.r], join=True)
        pipeline(ntile, [stA, stA2, stB, stC, stD])
        ncol = T + LC if l == 0 else T
        for s_ in range(2):
            S.dma("sp", lambda h, s_=s_: h.dma_start(out=self.AFFT[s_ * 16:(s_ + 1) * 16, 0:ncol], in_=affs[:, s_, 0:ncol]), reads=[affs.r])


Builder.p_norm2 = _p_norm2


def _p_topk(self, l):
    S = self.S
    with self.phase() as P:
        A = P.sb("A", [32, T + LC], F32)
        W = P.sb("W", [32, T], F32)
        ncol = T + LC if l == 0 else T
        S.dma("sp", lambda h: h.dma_start(out=A[:, 0:ncol], in_=self.AFFT[:, 0:ncol]), writes=[A.r])
        idf = P.sb("idf", [128, 128], F32)
        S.dma("sp", lambda h: h.dma_start(out=idf[:], in_=self.ident_f), writes=[idf.r])
        off = P.sb("off", [32, 2], F32)
        S.dma("sp", lambda h: h.dma_start(out=off[:], in_=self.tk_off), writes=[off.r])
        vals = P.sb("vals", [32, 256], F32)
        idx = P.sb("idx", [32, 256], U32)
        idxf = P.sb("idxf", [32, 256], F32)
        cur = A
        for k in range(32):
            sl = slice(8 * k, 8 * k + 8)
            S.op("dve", lambda h, cur=cur, sl=sl: h.max(out=vals[:, sl], in_=cur[:, 0:T]), reads=[cur.r], writes=[vals.r], join=True)
            S.op("dve", lambda h, cur=cur, sl=sl: h.max_index(out=idx[:, sl], in_max=vals[:, sl], in_values=cur[:, 0:T]), reads=[cur.r, vals.r], writes=[idx.r], join=True)
            if k < 31:
                S.op("dve", lambda h, cur=cur, sl=sl: h.match_replace(out=W[:, 0:T], in_to_replace=vals[:, sl], in_values=cur[:, 0:T], imm_value=-1.0),
                     reads=[cur.r, vals.r], writes=[W.r])
                cur = W
        S.op("dve", lambda h: h.tensor_copy(out=idxf[:], in_=idx[:]), reads=[idx.r], writes=[idxf.r])
        S.op("dve", lambda h: h.tensor_scalar(out=idxf[:], in0=idxf[:], scalar1=off[:, 0:1], scalar2=None, op0=ALU.add), reads=[idxf.r, off.r], writes=[idxf.r])
        pti = P.ps("pti", [128, 2, 32])
        ptg = P.ps("ptg", [128, 2, 32])
        idxT = P.sb("idxT", [128, 2, 32], I32)
        gT = P.sb("gT", [128, 2, 32], F32)

        def tri(h):
            for hf in range(2):
                ins = h.transpose(pti[:, hf, :], idxf[:, hf * 128:(hf + 1) * 128], idf[0:32, 0:32])
            return ins
        S.op("pe", tri, reads=[idxf.r, idf.r], writes=[pti.r])

        def trg(h):
            for hf in range(2):
                ins = h.transpose(ptg[:, hf, :], vals[:, hf * 128:(hf + 1) * 128], idf[0:32, 0:32])
            return ins
        S.op("pe", trg, reads=[vals.r, idf.r], writes=[ptg.r])
        S.op("dve", lambda h: h.tensor_copy(out=idxT[:], in_=pti[:]), reads=[pti.r], writes=[idxT.r])
        idx4 = P.sb("idx4", [128, 4, 64], I32)
        for db in range(4):
            S.op("dve", lambda h, db=db: h.tensor_scalar(out=idx4[:, db, :], in0=pti[:].rearrange("p a b -> p (a b)"), scalar1=4.0, scalar2=float(db),
                                                        op0=ALU.mult, op1=ALU.add), reads=[pti.r], writes=[idx4.r], join=(db > 0))
        S.dma("sp", lambda h: h.dma_start(out=self.IDX4, in_=idx4[:].rearrange("p a b -> p (a b)")), reads=[idx4.r])
        S.op("act", lambda h: h.copy(out=gT[:], in_=ptg[:]), reads=[ptg.r], writes=[gT.r])
        S.dma("sp", lambda h: h.dma_start(out=self.IDXT, in_=idxT[:].rearrange("p a b -> p (a b)")), reads=[idxT.r])
        S.dma("sp", lambda h: h.dma_start(out=self.GT, in_=gT[:].rearrange("p a b -> p (a b)")), reads=[gT.r])
        if l == 0:
            Wc = P.sb("Wc", [32, LC], F32)
            valc = P.sb("valc", [32, 32], F32)
            idc = P.sb("idc", [32, 32], U32)
            idcf = P.sb("idcf", [32, 32], F32)
            cur = None
            for k in range(4):
                sl = slice(8 * k, 8 * k + 8)
                src = A[:, T:T + LC] if cur is None else Wc[:, :]
                rr = A.r if cur is None else Wc.r
                S.op("dve", lambda h, src=src, sl=sl: h.max(out=valc[:, sl], in_=src), reads=[rr], writes=[valc.r], join=True)
                S.op("dve", lambda h, src=src, sl=sl: h.max_index(out=idc[:, sl], in_max=valc[:, sl], in_values=src), reads=[rr, valc.r], writes=[idc.r], join=True)
                if k < 3:
                    S.op("dve", lambda h, src=src, sl=sl: h.match_replace(out=Wc[:, :], in_to_replace=valc[:, sl], in_values=src, imm_value=-1.0),
                         reads=[rr, valc.r], writes=[Wc.r])
                    cur = Wc
            S.op("dve", lambda h: h.tensor_copy(out=idcf[:], in_=idc[:]), reads=[idc.r], writes=[idcf.r])
            S.op("dve", lambda h: h.tensor_scalar(out=idcf[:], in0=idcf[:], scalar1=off[:, 1:2], scalar2=None, op0=ALU.add), reads=[idcf.r, off.r], writes=[idcf.r])
            ptc = P.ps("ptc", [32, 2, 32])
            S.op("pe", lambda h: h.transpose(ptc[:, 0, :], idcf[:], idf[0:32, 0:32]), reads=[idcf.r, idf.r], writes=[ptc.r])
            S.op("pe", lambda h: h.transpose(ptc[:, 1, :], valc[:], idf[0:32, 0:32]), reads=[valc.r, idf.r], writes=[ptc.r])
            icT = P.sb("icT", [32, 32], I32)
            gcT = P.sb("gcT", [32, 32], F32)
            S.op("dve", lambda h: h.tensor_copy(out=icT[:], in_=ptc[:, 0, :]), reads=[ptc.r], writes=[icT.r])
            ic4 = P.sb("ic4", [32, 4, 32], I32)
            for db in range(4):
                S.op("dve", lambda h, db=db: h.tensor_scalar(out=ic4[:, db, :], in0=ptc[:, 0, :], scalar1=4.0, scalar2=float(db),
                                                            op0=ALU.mult, op1=ALU.add), reads=[ptc.r], writes=[ic4.r], join=(db > 0))
            S.dma("sp", lambda h: h.dma_start(out=self.IDXC4, in_=ic4[:].rearrange("p a b -> p (a b)")), reads=[ic4.r])
            S.op("act", lambda h: h.copy(out=gcT[:], in_=ptc[:, 1, :]), reads=[ptc.r], writes=[gcT.r])
            S.dma("sp", lambda h: h.dma_start(out=self.IDXC, in_=icT[:]), reads=[icT.r])
            S.dma("sp", lambda h: h.dma_start(out=self.GC, in_=gcT[:]), reads=[gcT.r])


Builder.p_topk = _p_topk


def _p_moe(self, l):
    S = self.S
    nctx = 2 if l == 0 else 0
    NSL = 512 + 32 * nctx
    CG = [(0, 288), (288, 288)] if nctx else [(0, 512)]
    W3 = {"g": self.w_eg, "u": self.w_eu, "d": self.w_ed}
    with self.phase() as P:
        idb = P.sb("idb", [128, 128], BF16)
        S.dma("sp", lambda h: h.dma_start(out=idb[:], in_=self.ident_b), writes=[idb.r])
        idxT = P.sb("idxT", [128, 64], I32)
        gT = P.sb("gT", [128, 64], F32)
        S.dma("sp", lambda h: h.dma_start(out=idxT[:], in_=self.IDXT), writes=[idxT.r])
        S.dma("sp", lambda h: h.dma_start(out=gT[:], in_=self.GT), writes=[gT.r])
        idx4 = P.sb("idx4", [128, 4 * 64], I32)
        S.dma("sp", lambda h: h.dma_start(out=idx4[:], in_=self.IDX4), writes=[idx4.r])
        X4 = self.X.rearrange("n (b c) -> (n b) c", c=512)
        icT = gcT = ic4 = None
        if nctx:
            ic4 = P.sb("ic4", [32, 4 * 32], I32)
            S.dma("sp", lambda h: h.dma_start(out=ic4[:], in_=self.IDXC4), writes=[ic4.r])
            icT = P.sb("icT", [32, 32], I32)
            gcT = P.sb("gcT", [32, 32], F32)
            S.dma("sp", lambda h: h.dma_start(out=icT[:], in_=self.IDXC), writes=[icT.r])
            S.dma("sp", lambda h: h.dma_start(out=gcT[:], in_=self.GC), writes=[gcT.r])
        M5 = self.bc_load(P, "M5", l, 5)
        ntl = 4 + nctx
        xe = [P.sb("xe%d" % i, [128, D], BF16) for i in range(ntl)]
        xeT = [P.sb("xeT%d" % i, [128, 16, NSL], BF16) for i in range(2)]
        hidT = P.sb("hidT", [128, 16, NSL], BF16)
        Wb = [P.sb("Wb%d" % i, [128, 16, 512], BF16) for i in range(4)]
        sgt = [P.sb("sgt%d" % i, [128, NSL], F32) for i in range(2)]
        ye = [P.sb("ye%d" % i, [128, 512], F32) for i in range(6)]
        ptr = [P.ps("ptr%d" % i, [128, 8, 128], BF16) for i in range(2)]
        pg = P.ps("pg", [128, 2, 512])
        pu = P.ps("pu", [128, 2, 512])
        py = [P.ps("py%d" % i, [128, 512]) for i in range(2)]
        xres = [Res() for _ in range(4)]
        blocks = []
        for e in range(16):
            for fb in range(4):
                blocks += [(e, "g", fb), (e, "u", fb)]
            for db in range(4):
                blocks += [(e, "d", db)]
        issued = [0]

        def issue_upto(i):
            while issued[0] <= min(i, len(blocks) - 1):
                k = issued[0]
                e, kind, ix = blocks[k]
                w = Wb[k % 4]
                S.dma("pool", lambda h, w=w, e=e, kind=kind, ix=ix: h.dma_start(
                    out=w[:], in_=W3[kind][l][e][:, ix * 512:(ix + 1) * 512].rearrange("(kc p) f -> p kc f", p=128)), writes=[w.r])
                issued[0] += 1

        def tiles_of(e):
            tl = [(s, hf, 128, s * 256 + hf * 128, idxT, hf * 32 + s * 16 + e) for s in range(2) for hf in range(2)]
            for s in range(nctx):
                tl.append((s, None, 32, 512 + s * 32, icT, s * 16 + e))
            return tl

        def gather(e):
            for ti, (s, hf, M, c0, it, col) in enumerate(tiles_of(e)):
                x_ = xe[ti]
                S.dma("pool", lambda h, x_=x_, it=it, col=col, M=M: h.indirect_dma_start(
                    out=x_[0:M, :], out_offset=None, in_=self.H2[:, :], in_offset=bass.IndirectOffsetOnAxis(ap=it[0:M, col:col + 1], axis=0)),
                    reads=[it.r], writes=[x_.r])

        def transposes(e):
            xT = xeT[e % 2]
            for ti, (s, hf, M, c0, it, col) in enumerate(tiles_of(e)):
                x_ = xe[ti]
                for half in range(2):
                    p = ptr[half]

                    def tr(h, p=p, x_=x_, half=half, M=M):
                        for j in range(8):
                            kc = half * 8 + j
                            ins = h.transpose(p[:, j, 0:M], x_[0:M, kc * 128:(kc + 1) * 128], idb[0:M, 0:M])
                        return ins
                    S.op("pe", tr, reads=[x_.r, idb.r], writes=[p.r])
                    if half == 0:
                        S.op("act", lambda h, p=p, c0=c0, M=M, xT=xT: h.copy(out=xT[:, 0:8, c0:c0 + M], in_=p[:, :, 0:M]), reads=[p.r], writes=[xT.r], join=True)
                    else:
                        S.op("dve", lambda h, p=p, c0=c0, M=M, xT=xT: h.tensor_copy(out=xT[:, 8:16, c0:c0 + M], in_=p[:, :, 0:M]), reads=[p.r], writes=[xT.r], join=True)

        bi = 0
        yk = 0
        issue_upto(1)
        gather(0)
        transposes(0)
        for e in range(16):
            xT = xeT[e % 2]
            tiles = tiles_of(e)
            for fb in range(4):
                issue_upto(bi + 3)
                if fb == 2 and e + 1 < 16:
                    gather(e + 1)
                wg = Wb[bi % 4]
                wu = Wb[(bi + 1) % 4]
                bi += 2
                for fc in range(4):
                    F = fb * 4 + fc
                    sg_ = sgt[F % 2]

                    def mmgu(h, wt, pp, fc=fc, xT=xT):
                        for gi, (c0, n) in enumerate(CG):
                            for kc in range(16):
                                ins = h.matmul(pp[:, gi, 0:n], lhsT=wt[:, kc, fc * 128:(fc + 1) * 128], rhs=xT[:, kc, c0:c0 + n], start=(kc == 0), stop=(kc == 15))
                        return ins
                    S.op("pe", lambda h, wg=wg, fc=fc, mmgu=mmgu: mmgu(h, wg, pg, fc), reads=[wg.r, xT.r], writes=[pg.r])
                    S.op("pe", lambda h, wu=wu, fc=fc, mmgu=mmgu: mmgu(h, wu, pu, fc), reads=[wu.r, xT.r], writes=[pu.r])
                    for gi, (c0, n) in enumerate(CG):
                        S.op("act", lambda h, sg_=sg_, gi=gi, c0=c0, n=n: h.activation(out=sg_[:, c0:c0 + n], in_=pg[:, gi, 0:n], func=AF.Silu),
                             reads=[pg.r], writes=[sg_.r], join=(gi > 0))
                    for gi, (c0, n) in enumerate(CG):
                        S.op("dve", lambda h, sg_=sg_, F=F, gi=gi, c0=c0, n=n: h.tensor_tensor(out=hidT[:, F, c0:c0 + n], in0=sg_[:, c0:c0 + n], in1=pu[:, gi, 0:n], op=ALU.mult),
                             reads=[sg_.r, pu.r], writes=[hidT.r], join=True)
            if e + 1 < 16:
                transposes(e + 1)
            for db in range(4):
                issue_upto(bi + 2)
                wd = Wb[bi % 4]
                bi += 1
                dsl = slice(db * 512, (db + 1) * 512)
                for ti_, (s, hf, M, c0, it, col) in enumerate(tiles):
                    p = py[yk % 2]
                    y_ = ye[yk % 6]
                    yk += 1

                    def mmd(h, p=p, wd=wd, c0=c0, M=M):
                        for fc in range(16):
                            ins = h.matmul(p[0:M, :], lhsT=hidT[:, fc, c0:c0 + M], rhs=wd[:, fc, :], start=(fc == 0), stop=(fc == 15))
                        return ins
                    S.op("pe", mmd, reads=[hidT.r, wd.r], writes=[p.r])
                    gt_ = gT if hf is not None else gcT
                    r = s if hf is not None else 2
                    S.op("dve", lambda h, p=p, y_=y_, gt_=gt_, col=col, M=M, r=r, dsl=dsl: h.scalar_tensor_tensor(
                        out=y_[0:M, :], in0=p[0:M, :], scalar=gt_[0:M, col:col + 1], in1=M5[0:M, r, dsl], op0=ALU.mult, op1=ALU.mult),
                        reads=[p.r, gt_.r, M5.r], writes=[y_.r])
                    if hf is not None:
                        i4, c4 = idx4, db * 64 + col
                    else:
                        i4, c4 = ic4, db * 32 + col
                    S.dma("pool", lambda h, y_=y_, i4=i4, c4=c4, M=M: h.indirect_dma_start(
                        out=X4[:, :], out_offset=bass.IndirectOffsetOnAxis(ap=i4[0:M, c4:c4 + 1], axis=0),
                        in_=y_[0:M, :], in_offset=None, compute_op=ALU.add),
                        reads=[y_.r, i4.r], writes=[xres[db]], join=(ti_ > 0))


Builder.p_moe = _p_moe


def _p_final(self):
    S = self.S
    with self.phase() as P:
        g = P.sb("g", [128, D], F32)
        S.dma("sp", lambda h: h.dma_start(out=g[:], in_=self.fing.broadcast_to([128, D])), writes=[g.r])
        xt = [P.sb("xt%d" % i, [128, D], F32) for i in range(4)]
        ot = [P.sb("ot%d" % i, [128, D], F32) for i in range(2)]
        st = [P.sb("st%d" % i, [128, 4], F32) for i in range(4)]
        sq = [P.sb("sq%d" % i, [128, D], BF16) for i in range(2)]

        def stA(tt):
            x = xt[tt % 4]
            S.dma("sp", lambda h: h.dma_start(out=x[:], in_=self.X[tt * 128:(tt + 1) * 128, :]), writes=[x.r])
            self.norm_a(x, st[tt % 4], sq[tt % 2])

        def stA2(tt):
            self.norm_a2(st[tt % 4])

        def stB(tt):
            x = xt[tt % 4]
            o = ot[tt % 2]
            s_ = st[tt % 4]
            S.op("dve", lambda h: h.scalar_tensor_tensor(out=o[:], in0=x[:], scalar=s_[:, 2:3], in1=g[:], op0=ALU.mult, op1=ALU.mult),
                 reads=[x.r, s_.r, g.r], writes=[o.r])
            S.dma("act", lambda h: h.dma_start(out=self.out[tt * 128:(tt + 1) * 128, :], in_=o[:]), reads=[o.r])
        pipeline(32, [stA, stA2, stB])


Builder.p_final = _p_final


def _p_dbg_copy(self):
    S = self.S
    xi = self.nc.dram_tensor("Xinit", [NF, D], F32, kind="ExternalInput").ap()
    self.inputs["Xinit"] = ([NF, D], F32)
    with self.phase() as P:
        for i in range(9):
            S.dma("sp", lambda h, i=i: h.dma_start(out=self.X[i * 512:(i + 1) * 512, :], in_=xi[i * 512:(i + 1) * 512, :]))


Builder.p_dbg_copy = _p_dbg_copy


def build_full(B):
    for l in range(DEPTH):
        B.p_mod(l)
        B.p_norm1(l)
        B.p_proj(l)
        B.p_gla(l)
        B.p_swa(l)
        B.p_na(l)
        B.p_sgu(l)
        B.p_merge1(l)
        B.p_merge2(l)
        B.p_norm2(l)
        B.p_topk(l)
        B.p_moe(l)
    B.p_final()


_CACHE = {}


def kernel(**inputs):
    I = {k: np.asarray(v) for k, v in inputs.items()}
    if "B" not in _CACHE:
        B = Builder()
        build_full(B)
        _CACHE["B"] = B
    B = _CACHE["B"]
    sh = _shared_inputs(I)
    in_maps = []
    for c in range(NCORES):
        m = _core_inputs(I, c, sh)
        in_maps.append({k: m[k] for k in B.inputs})
    res = run_bass_kernel_spmd(B.nc, in_maps, core_ids=list(range(NCORES)))
    out = np.empty((2 * NCORES, T, D), np.float32)
    for c in range(NCORES):
        out[2 * c:2 * c + 2] = np.asarray(res.results[c]["out"], dtype=np.float32).reshape(2, T, D)
    return out
```

```python
import os
import numpy as np
import ml_dtypes
import concourse.bass as bass
import concourse.mybir as mybir
from concourse.bass_utils import run_bass_kernel_spmd
from contextlib import ExitStack

F32 = mybir.dt.float32
BF16 = mybir.dt.bfloat16
I32 = mybir.dt.int32
U32 = mybir.dt.uint32
AF = mybir.ActivationFunctionType
ALU = mybir.AluOpType
AX = mybir.AxisListType

NCORES = 8
D = 2048
T = 2048
LC = 256
NS = 2
NF = NS * T + NS * LC
NTL = NF // 128
DEPTH = 2
EPS = 1e-6
NEG = -30000.0
NDS = 12
GCH = 128
GNC = 2304 // GCH
GCC = LC // GCH


def tile_row(tt):
    return 0 if tt < 16 else (1 if tt < 32 else 2)


class Res:
    __slots__ = ("w", "r", "pr")

    def __init__(self):
        self.w = {}
        self.r = {}
        self.pr = {}


class Sched:
    ENG = ("pe", "act", "dve", "pool", "sp")

    def __init__(self, nc, es):
        self.nc = nc
        self.sem = {e: es.enter_context(nc.semaphore("s_" + e)) for e in self.ENG}
        self.cnt = {e: 0 for e in self.ENG}
        self.dsem = {q: [es.enter_context(nc.semaphore("d_%s%d" % (q, i))) for i in range(NDS)]
                     for q in ("sp", "act", "pool")}
        self.dcnt = {q: [0] * NDS for q in ("sp", "act", "pool")}
        self.drr = {q: 0 for q in ("sp", "act", "pool")}
        self.waited = {e: {} for e in self.ENG}
        self.q = {e: [] for e in self.ENG}
        self.nops = 0

    def _waits(self, eng, toks):
        best = {}
        for (sem, key, val) in toks:
            if self.waited[eng].get(key, 0) >= val:
                continue
            if key not in best or best[key][1] < val:
                best[key] = (sem, val)
        out = []
        for key, (sem, val) in best.items():
            self.waited[eng][key] = val
            out.append((sem, val))
        return out

    def _deps(self, eng, reads, writes, join=False):
        toks = []
        for r in reads:
            for t in r.w.values():
                if not (t[1] == eng and eng == "pe"):
                    toks.append(t)
        for w in writes:
            if not join:
                for t in w.w.values():
                    if not (t[1] == eng and eng == "pe"):
                        toks.append(t)
            else:
                for t in w.pr.values():
                    if not (t[1] == eng and eng == "pe"):
                        toks.append(t)
            for t in w.r.values():
                if not (t[1] == eng and eng == "pe"):
                    toks.append(t)
        return toks

    def _finish(self, tok, reads, writes, join=False):
        for r in reads:
            r.r[tok[1]] = tok
        for w in writes:
            if join:
                w.w[tok[1]] = tok
            else:
                pr = dict(w.w)
                pr.update(w.r)
                w.pr = pr
                w.w = {tok[1]: tok}
                w.r = {}

    def op(self, eng, fn, reads=(), writes=(), join=False):
        waits = self._waits(eng, self._deps(eng, reads, writes, join))
        self.cnt[eng] += 1
        sem = self.sem[eng]
        tok = (sem, eng, self.cnt[eng])

        def emit(h, waits=waits, fn=fn, sem=sem):
            for (s, v) in waits:
                h.wait_ge(s, v)
            fn(h).then_inc(sem, 1)

        self.q[eng].append(emit)
        self.nops += 1
        self._finish(tok, reads, writes, join)
        return tok

    def dma(self, q, fn, reads=(), writes=(), join=False):
        i = self.drr[q]
        self.drr[q] = (i + 1) % NDS
        sem = self.dsem[q][i]
        prev = self.dcnt[q][i]
        key = "d_%s%d" % (q, i)
        toks = self._deps(q, reads, writes, join)
        if prev > 0:
            toks.append((sem, key, prev))
        waits = self._waits(q, toks)
        self.dcnt[q][i] = prev + 16
        tok = (sem, key, prev + 16)

        def emit(h, waits=waits, fn=fn, sem=sem):
            for (s, v) in waits:
                h.wait_ge(s, v)
            fn(h).then_inc(sem, 16)

        self.q[q].append(emit)
        self.nops += 1
        self._finish(tok, reads, writes, join)
        return tok

    def all_tokens(self):
        toks = [(self.sem[e], e, self.cnt[e]) for e in self.ENG if self.cnt[e] > 0]
        for q in self.dsem:
            for i in range(NDS):
                if self.dcnt[q][i] > 0:
                    toks.append((self.dsem[q][i], "d_%s%d" % (q, i), self.dcnt[q][i]))
        return toks

    def barrier(self):
        toks = self.all_tokens()
        for e in self.ENG:
            waits = self._waits(e, [t for t in toks if t[1] != e])
            if waits:
                def emit(h, waits=waits):
                    for (s, v) in waits:
                        h.wait_ge(s, v)
                self.q[e].append(emit)

    def flush(self):
        q = self.q
        with self.nc.Block() as block:
            @block.tensor
            def _(h):
                for f in q["pe"]:
                    f(h)

            @block.scalar
            def _(h):
                for f in q["act"]:
                    f(h)

            @block.vector
            def _(h):
                for f in q["dve"]:
                    f(h)

            @block.gpsimd
            def _(h):
                for f in q["pool"]:
                    f(h)

            @block.sync
            def _(h):
                for f in q["sp"]:
                    f(h)
        self.q = {e: [] for e in self.ENG}


class _Stop(Exception):
    pass


def _stop(k):
    if float(os.environ.get("DBG_STOP", "99")) <= k:
        raise _Stop()


def pipeline(n, stages):
    ns = len(stages)
    for i in range(n + ns - 1):
        for k, st in enumerate(stages):
            j = i - k
            if 0 <= j < n:
                st(j)


class TL:
    __slots__ = ("t", "r")

    def __init__(self, t):
        self.t = t
        self.r = Res()

    def __getitem__(self, k):
        return self.t[k]


class Phase:
    def __init__(self, B):
        self.B = B
        self.es = ExitStack()

    def __enter__(self):
        self.es.__enter__()
        return self

    _uid = [0]

    def sb(self, name, shape, dt):
        Phase._uid[0] += 1
        return TL(self.es.enter_context(self.B.nc.sbuf_tensor("%s_%d" % (name, Phase._uid[0]), list(shape), dt)))

    def ps(self, name, shape, dt=F32):
        Phase._uid[0] += 1
        return TL(self.es.enter_context(self.B.nc.psum_tensor("%s_%d" % (name, Phase._uid[0]), list(shape), dt)))

    def __exit__(self, *a):
        self.B.S.barrier()
        self.B.S.flush()
        return self.es.__exit__(*a)


TOK_SEGS = [(256, 1536, 0), (1568, 2336, 1280), (3360, 4896, 2048)]
PT_KG, PT_VG, PT_RG = 0, 256, 768
PT_QS, PT_KS, PT_VS = 1280, 1792, 1920
PT_VN, PT_U, PT_V = 2048, 2560, 3072
NPT = 3584
FM_SEGS = [(0, 128, 0), (128, 128, 128), (256, 128, 256), (384, 128, 384), (1536, 32, 512),
           (2080, 128, 640)] + [(2336 + 128 * i, 128, 768 + 128 * i) for i in range(8)]
FM_QG, FM_KG, FM_A, FM_KS, FM_QN, FM_KN = 0, 256, 512, 640, 768, 1280
NFM = 1792


INPUT_SPECS = {
    "xin": (lambda: [NF, D], F32),
    "cin": (lambda: [128, 48], F32),
    "w_ada": (lambda: [DEPTH, D, 6 * D], F32),
    "b_ada3": (lambda: [DEPTH, 3, 6 * D], F32),
    "gvec3": (lambda: [DEPTH, 2, 3, D], F32),
    "fing": (lambda: [1, D], F32),
    "w_in": (lambda: [DEPTH, D, 4896], F32),
    "gla_wa": (lambda: [DEPTH, 2, 17, 256], F32),
    "gla_g": (lambda: [DEPTH, 1, 128], F32),
    "swa_sink": (lambda: [DEPTH, 1, 8], F32),
    "na_g": (lambda: [DEPTH, 128, 7168], F32),
    "na_m": (lambda: [128, 7168], F32),
    "sgu_lng": (lambda: [DEPTH, 1, 512], F32),
    "sgu_lnb": (lambda: [DEPTH, 1, 512], F32),
    "sgu_wsT": (lambda: [DEPTH, 128, 512], F32),
    "sgu_bs": (lambda: [DEPTH, 128, 4], F32),
    "w_gate": (lambda: [DEPTH, 4, D, D], F32),
    "b_gate_p": (lambda: [DEPTH, 128, 64], F32),
    "w_branch": (lambda: [DEPTH, 4, 512, D], F32),
    "w_out": (lambda: [DEPTH, D, D], F32),
    "w_router": (lambda: [DEPTH, D, 16], F32),
    "w_eg": (lambda: [DEPTH, 16, D, D], F32),
    "w_eu": (lambda: [DEPTH, 16, D, D], F32),
    "w_ed": (lambda: [DEPTH, 16, D, D], F32),
    "ident_f": (lambda: [128, 128], F32),
    "ident_b": (lambda: [128, 128], BF16),
    "rope_cs": (lambda: [T, 64], F32),
    "gla_u": (lambda: [GCH, 4 * GCH], F32),
    "gla_mask": (lambda: [GCH, 4 * GCH], F32),
    "swa_negm": (lambda: [128, 2 * 512], BF16),
    "tk_off": (lambda: [32, 2], F32),
}


class Builder:
    def __init__(self, dbg=False, scr_in=(), scr_out=()):
        self.dbg = dbg
        self.scr_in = set(scr_in)
        self.scr_out = set(scr_out)
        self.dbg_outs = []
        nc = self.nc = bass.Bass("TRN2", target_bir_lowering=False)
        self.es = ExitStack()
        self.S = Sched(nc, self.es)
        self.inputs = {}
        self.out = nc.dram_tensor("out", [NS * T, D], F32, kind="ExternalOutput").ap()
        Z = self._scr
        self.X = Z("X", [NF, D], F32)
        self.MODV = Z("MODV", [DEPTH, 3, 6 * D], F32)
        self.HT = Z("HT", [D, NF], BF16)
        self.PTOK = Z("PTOK", [NF, NPT], BF16)
        self.PFM = Z("PFM", [NFM, NF], BF16)
        self.BRT = Z("BRT", [D, NF], BF16)
        self.ACCT = Z("ACCT", [D, NF], BF16)
        self.H2 = Z("H2", [NF, D], BF16)
        self.AFFT = Z("AFFT", [32, T + LC], F32)
        self.IDXT = Z("IDXT", [128, 64], I32)
        self.GT = Z("GT", [128, 64], F32)
        self.IDXC = Z("IDXC", [32, 32], I32)
        self.GC = Z("GC", [32, 32], F32)
        self.IDX4 = Z("IDX4", [128, 4 * 64], I32)
        self.IDXC4 = Z("IDXC4", [32, 4 * 32], I32)

    def __getattr__(self, name):
        if name in INPUT_SPECS:
            shp, dt = INPUT_SPECS[name]
            shape = shp()
            self.inputs[name] = (shape, dt)
            ap = self.nc.dram_tensor(name, list(shape), dt, kind="ExternalInput").ap()
            setattr(self, name, ap)
            return ap
        raise AttributeError(name)

    def _scr(self, name, shape, dt):
        kind = "Internal"
        if name in self.scr_in:
            kind = "ExternalInput"
            self.inputs[name] = (shape, dt)
        elif name in self.scr_out:
            kind = "ExternalOutput"
            self.dbg_outs.append(name)
        return self.nc.dram_tensor(name, list(shape), dt, kind=kind).ap()

    def phase(self):
        return Phase(self)

    def p_mod(self, l):
        S = self.S
        with self.phase() as P:
            ct = P.sb("ct", [128, 48], F32)
            ca = P.sb("ca", [128, 48], BF16)
            wt = [P.sb("wt%d" % i, [128, 16, 512], BF16) for i in range(2)]
            bt = P.sb("bt", [3, 6 * D], F32)
            gt = P.sb("gt", [3, 2, D], F32)
            mo = P.sb("mo", [3, 6 * D], F32)
            ps = [P.ps("ps%d" % i, [3, 512]) for i in range(2)]
            S.dma("sp", lambda h: h.dma_start(out=ct[:], in_=self.cin), writes=[ct.r])
            S.dma("sp", lambda h: h.dma_start(out=bt[:], in_=self.b_ada3[l]), writes=[bt.r])
            S.dma("sp", lambda h: h.dma_start(out=gt[:], in_=self.gvec3[l].rearrange("n r d -> r n d")), writes=[gt.r])
            S.op("act", lambda h: h.activation(out=ca[:], in_=ct[:], func=AF.Silu), reads=[ct.r], writes=[ca.r])
            for cg in range(24):
                w = wt[cg % 2]
                S.dma("pool", lambda h, w=w, cg=cg: h.dma_start(
                    out=w[:], in_=self.w_ada[l][:, cg * 512:(cg + 1) * 512].rearrange("(kc p) n -> p kc n", p=128)),
                    writes=[w.r])
                p = ps[cg % 2]

                def mm(h, w=w, p=p):
                    for kc in range(16):
                        ins = h.matmul(p[:], lhsT=ca[:, kc * 3:(kc + 1) * 3], rhs=w[:, kc, :], start=(kc == 0), stop=(kc == 15))
                    return ins
                S.op("pe", mm, reads=[ca.r, w.r], writes=[p.r])
                S.op("dve", lambda h, p=p, cg=cg: h.tensor_tensor(out=mo[:, cg * 512:(cg + 1) * 512], in0=p[:],
                                                                  in1=bt[:, cg * 512:(cg + 1) * 512], op=ALU.add),
                     reads=[p.r, bt.r], writes=[mo.r])
            for n, k in ((0, 1), (1, 4)):
                S.op("dve", lambda h, n=n, k=k: h.scalar_tensor_tensor(
                    out=mo[:, k * D:(k + 1) * D], in0=mo[:, k * D:(k + 1) * D], scalar=1.0, in1=gt[:, n, :],
                    op0=ALU.add, op1=ALU.mult), reads=[mo.r, gt.r], writes=[mo.r])
            S.dma("sp", lambda h: h.dma_start(out=self.MODV[l], in_=mo[:]), reads=[mo.r])

    def bc_load(self, P, name, l, k):
        S = self.S
        t = P.sb(name, [128, 3, D], F32)
        for r in range(3):
            S.dma("sp", lambda h, r=r: h.dma_start(out=t[:, r, :], in_=self.MODV[l][r:r + 1, k * D:(k + 1) * D].broadcast_to([128, D])),
                  writes=[t.r], join=True)
        return t

    def norm_a(self, xt, st, sq):
        S = self.S
        S.op("act", lambda h: h.activation(out=sq[:], in_=xt[:], func=AF.Square, accum_out=st[:, 0:1]),
             reads=[xt.r], writes=[sq.r, st.r])
        S.op("dve", lambda h: h.tensor_scalar(out=st[:, 1:2], in0=st[:, 0:1], scalar1=1.0 / D, scalar2=EPS,
                                              op0=ALU.mult, op1=ALU.add), reads=[st.r], writes=[st.r])

    def norm_a2(self, st):
        S = self.S
        S.op("act", lambda h: h.sqrt(out=st[:, 3:4], in_=st[:, 1:2]), reads=[st.r], writes=[st.r])
        S.op("dve", lambda h: h.reciprocal(out=st[:, 2:3], in_=st[:, 3:4]), reads=[st.r], writes=[st.r])

    def norm_b(self, xt, st, G, SH, r, tmp, hout):
        S = self.S
        S.op("dve", lambda h: h.scalar_tensor_tensor(out=tmp[:], in0=xt[:], scalar=st[:, 2:3], in1=G[:, r, :],
                                                     op0=ALU.mult, op1=ALU.mult), reads=[xt.r, st.r, G.r], writes=[tmp.r])
        S.op("pool", lambda h: h.tensor_tensor(out=hout[:, 0:1152], in0=tmp[:, 0:1152], in1=SH[:, r, 0:1152], op=ALU.add),
             reads=[tmp.r, SH.r], writes=[hout.r])
        S.op("dve", lambda h: h.tensor_tensor(out=hout[:, 1152:D], in0=tmp[:, 1152:D], in1=SH[:, r, 1152:D], op=ALU.add),
             reads=[tmp.r, SH.r], writes=[hout.r], join=True)

    def p_norm1(self, l):
        S = self.S
        src = self.xin if l == 0 else self.X
        with self.phase() as P:
            G = self.bc_load(P, "G", l, 1)
            SH = self.bc_load(P, "SH", l, 0)
            idb = P.sb("idb", [128, 128], BF16)
            S.dma("sp", lambda h: h.dma_start(out=idb[:], in_=self.ident_b), writes=[idb.r])
            xt = [P.sb("xt%d" % i, [128, D], F32) for i in range(4)]
            st = [P.sb("st%d" % i, [128, 4], F32) for i in range(4)]
            sq = [P.sb("sq%d" % i, [128, D], BF16) for i in range(2)]
            tmp = [P.sb("tmp%d" % i, [128, D], F32) for i in range(2)]
            hb = [P.sb("hb%d" % i, [128, D], BF16) for i in range(2)]
            stg = [P.sb("stg%d" % i, [128, 16, 512], BF16) for i in range(2)]
            pt = [P.ps("pt%d" % i, [128, 8, 128], BF16) for i in range(4)]

            def stA(tt):
                x = xt[tt % 4]
                S.dma("sp", lambda h: h.dma_start(out=x[:], in_=src[tt * 128:(tt + 1) * 128, :]), writes=[x.r])
                self.norm_a(x, st[tt % 4], sq[tt % 2])

            def stA2(tt):
                self.norm_a2(st[tt % 4])

            def stB(tt):
                self.norm_b(xt[tt % 4], st[tt % 4], G, SH, tile_row(tt), tmp[tt % 2], hb[tt % 2])

            def stC(tt):
                hh = hb[tt % 2]
                sg = stg[(tt // 4) % 2]
                q = tt % 4
                for half in range(2):
                    p = pt[(tt % 2) * 2 + half]

                    def tr(h, p=p, half=half):
                        for j in range(8):
                            kc = half * 8 + j
                            ins = h.transpose(p[:, j, :], hh[:, kc * 128:(kc + 1) * 128], idb[:])
                        return ins
                    S.op("pe", tr, reads=[hh.r, idb.r], writes=[p.r])
                    if half == 0:
                        S.op("act", lambda h, p=p: h.copy(out=sg[:, 0:8, q * 128:(q + 1) * 128], in_=p[:]), reads=[p.r], writes=[sg.r], join=True)
                    else:
                        S.op("dve", lambda h, p=p: h.tensor_copy(out=sg[:, 8:16, q * 128:(q + 1) * 128], in_=p[:]), reads=[p.r], writes=[sg.r], join=True)
                if q == 3:
                    g = tt // 4
                    S.dma("sp", lambda h: h.dma_start(
                        out=self.HT[:, g * 512:(g + 1) * 512].rearrange("(kc p) n -> p kc n", p=128), in_=sg[:]),
                        reads=[sg.r])
            pipeline(NTL, [stA, stA2, stB, stC])

    def p_proj(self, l):
        S = self.S
        for pz in range(2):
            f0 = pz * 2304
            with self.phase() as P:
                hT = P.sb("hT", [128, 16, 2304], BF16)
                for kq in range(4):
                    S.dma("sp", lambda h, kq=kq: h.dma_start(
                        out=hT[:, kq * 4:(kq + 1) * 4, :],
                        in_=self.HT[kq * 512:(kq + 1) * 512, f0:f0 + 2304].rearrange("(kc p) n -> p kc n", p=128)),
                        writes=[hT.r], join=True)
                wt = [P.sb("w%d" % i, [128, 16, 512], BF16) for i in range(2)]
                stg = [P.sb("sg%d" % i, [128, 18, 512], BF16) for i in range(2)]
                wf = [P.sb("wf%d" % i, [128, 16, 128], BF16) for i in range(2)]
                sgf = [P.sb("sgf%d" % i, [128, 2304], BF16) for i in range(2)]
                pss = [P.ps("ps%d" % i, [128, 512]) for i in range(4)]
                gi = 0
                pi = 0
                for (lo, hi, off) in TOK_SEGS:
                    c = lo
                    while c < hi:
                        n = min(512, hi - c)
                        w = wt[gi % 2]
                        sg = stg[gi % 2]
                        S.dma("pool", lambda h, w=w, c=c, n=n: h.dma_start(
                            out=w[:, :, 0:n], in_=self.w_in[l][:, c:c + n].rearrange("(kc p) n -> p kc n", p=128)),
                            writes=[w.r])
                        for ti in range(18):
                            p = pss[pi % 4]

                            def mm(h, w=w, p=p, ti=ti, n=n):
                                for kc in range(16):
                                    ins = h.matmul(p[:, 0:n], lhsT=hT[:, kc, ti * 128:(ti + 1) * 128], rhs=w[:, kc, 0:n],
                                                   start=(kc == 0), stop=(kc == 15))
                                return ins
                            S.op("pe", mm, reads=[hT.r, w.r], writes=[p.r])
                            if pi % 2 == 0:
                                S.op("act", lambda h, p=p, sg=sg, ti=ti, n=n: h.copy(out=sg[:, ti, 0:n], in_=p[:, 0:n]),
                                     reads=[p.r], writes=[sg.r], join=True)
                            else:
                                S.op("dve", lambda h, p=p, sg=sg, ti=ti, n=n: h.tensor_copy(out=sg[:, ti, 0:n], in_=p[:, 0:n]),
                                     reads=[p.r], writes=[sg.r], join=True)
                            pi += 1
                        co = off + (c - lo)
                        S.dma("sp", lambda h, sg=sg, co=co, n=n: h.dma_start(
                            out=self.PTOK[f0:f0 + 2304, co:co + n].rearrange("(t p) n -> p t n", p=128), in_=sg[:, :, 0:n]),
                            reads=[sg.r])
                        c += n
                        gi += 1
                for i, (lo, n, off) in enumerate(FM_SEGS):
                    w = wf[i % 2]
                    sg = sgf[i % 2]
                    S.dma("pool", lambda h, w=w, lo=lo, n=n: h.dma_start(
                        out=w[:, :, 0:n], in_=self.w_in[l][:, lo:lo + n].rearrange("(kc p) n -> p kc n", p=128)),
                        writes=[w.r])
                    for g in range(5):
                        c0 = g * 512
                        ncol = min(512, 2304 - c0)
                        p = pss[pi % 4]

                        def mm(h, w=w, p=p, c0=c0, ncol=ncol, n=n):
                            for kc in range(16):
                                ins = h.matmul(p[0:n, 0:ncol], lhsT=w[:, kc, 0:n], rhs=hT[:, kc, c0:c0 + ncol],
                                               start=(kc == 0), stop=(kc == 15))
                            return ins
                        S.op("pe", mm, reads=[hT.r, w.r], writes=[p.r])
                        if pi % 2 == 0:
                            S.op("act", lambda h, p=p, sg=sg, c0=c0, ncol=ncol, n=n: h.copy(out=sg[0:n, c0:c0 + ncol], in_=p[0:n, 0:ncol]),
                                 reads=[p.r], writes=[sg.r], join=True)
                        else:
                            S.op("dve", lambda h, p=p, sg=sg, c0=c0, ncol=ncol, n=n: h.tensor_copy(out=sg[0:n, c0:c0 + ncol], in_=p[0:n, 0:ncol]),
                                 reads=[p.r], writes=[sg.r], join=True)
                        pi += 1
                    S.dma("sp", lambda h, sg=sg, off=off, n=n: h.dma_start(out=self.PFM[off:off + n, f0:f0 + 2304], in_=sg[0:n, :]),
                          reads=[sg.r])


def _consts():
    c = {}
    c["ident_f"] = np.eye(128, dtype=np.float32)
    c["ident_b"] = np.eye(128, dtype=np.float32).astype(ml_dtypes.bfloat16)
    t = np.arange(T)
    quarter = 16
    freqs = (10000.0 ** (-np.arange(quarter, dtype=np.float32) / quarter)).astype(np.float32)
    row = (t // 64).astype(np.float32)
    col = (t % 64).astype(np.float32)
    ang = np.concatenate([row[:, None] * freqs, col[:, None] * freqs], axis=-1).astype(np.float32)
    c["rope_cs"] = np.concatenate([np.cos(ang), np.sin(ang)], axis=-1).astype(np.float32)
    m = np.arange(GCH)[:, None]
    ll = np.arange(GCH)[None, :]
    sc = np.float32(-1.0 / 16.0)
    U = np.stack([(m <= ll), (m > ll), (m >= ll), (m < ll)], axis=1).astype(np.float32) * sc
    c["gla_u"] = U.reshape(GCH, 4 * GCH)
    mk = np.stack([(ll >= m), (ll <= m)], axis=1).astype(np.float32)
    mk = np.repeat(mk[:, :, None, :], 2, axis=2)
    c["gla_mask"] = mk.reshape(GCH, 4 * GCH)
    kj = np.arange(128)[:, None]
    qi = np.arange(128)[None, :]
    prev = np.where(kj >= qi, 1.0, 0.0).astype(np.float32)
    nxt = np.where(kj <= qi, 1.0, 0.0).astype(np.float32)
    nm = np.stack([np.tile(prev[:, None, :], (1, 4, 1)), np.tile(nxt[:, None, :], (1, 4, 1))], axis=1)
    c["swa_negm"] = nm.reshape(128, 1024).astype(ml_dtypes.bfloat16)
    cc = np.arange(64)
    cstart = np.clip(cc - 8, 0, 48)
    ok = (cc[None, :] >= cstart[:, None]) & (cc[None, :] < cstart[:, None] + 16)
    negm = np.where(ok.T, 1.0, 0.0).astype(np.float32)
    negm = np.tile(negm, (2, 1))
    c["tk_off"] = np.array([[0.0, 4096.0]] * 16 + [[2048.0, 4352.0]] * 16, dtype=np.float32)
    c["na_m"] = np.ascontiguousarray(np.broadcast_to(negm[:, None, :], (128, 8 * 14, 64))).reshape(128, 7168)
    return c


def _na_gather(rpb):
    kc = np.arange(64)[:, None]
    cc = np.arange(64)[None, :]
    coff = np.clip(kc - cc, -15, 15) + 15
    out = np.empty((rpb.shape[0], 2, 64, 8, 14, 64), np.float32)
    for w in range(2):
        g = rpb[:, :, w:w + 14, :][:, :, :, coff]
        out[:, w] = g.transpose(0, 3, 1, 2, 4)
    return out.reshape(rpb.shape[0], 128, 7168)


def _shared_inputs(I):
    f = lambda a: np.ascontiguousarray(a, dtype=np.float32)
    sh = dict(_consts())
    L = DEPTH
    sh["w_ada"] = f(I["w_ada"])
    sh["b_ada3"] = f(np.broadcast_to(I["b_ada"][:, None, :], (L, 3, 6 * D)))
    g = np.stack([I["norm1_g"], I["norm2_g"]], axis=1)
    sh["gvec3"] = f(np.broadcast_to(g[:, :, None, :], (L, 2, 3, D)))
    sh["fing"] = f(I["final_norm_g"].reshape(1, D))
    sh["w_in"] = f(I["w_in"])
    sh["gla_wa"] = f(np.concatenate([I["gla_w_a2"], I["gla_b_a2"][:, :, None, :]], axis=2))
    sh["gla_g"] = f(I["gla_norm_g"].reshape(L, 1, 128))
    sh["swa_sink"] = f(I["swa_sink"].reshape(L, 1, 8))
    sh["na_g"] = _na_gather(np.asarray(I["na_rpb"], np.float32))
    sh["sgu_lng"] = f(I["sgu_ln_g"].reshape(L, 1, 512))
    sh["sgu_lnb"] = f(I["sgu_ln_b"].reshape(L, 1, 512))
    sh["sgu_wsT"] = f(np.asarray(I["sgu_w_s"]).transpose(0, 3, 1, 2).reshape(L, 128, 512))
    sh["sgu_bs"] = f(np.asarray(I["sgu_b_s"]).transpose(0, 2, 1))
    sh["w_gate"] = f(I["w_gate"])
    sh["b_gate_p"] = f(np.asarray(I["b_gate"]).reshape(L, 4, 16, 128).transpose(0, 3, 1, 2).reshape(L, 128, 64))
    sh["w_branch"] = f(I["w_branch"])
    sh["w_out"] = f(I["w_out"])
    sh["w_router"] = f(I["w_router"])
    sh["w_eg"] = f(I["w_exp_gate"])
    sh["w_eu"] = f(I["w_exp_up"])
    sh["w_ed"] = f(I["w_exp_down"])
    return sh


def _core_inputs(I, core, sh):
    m = dict(sh)
    s0, s1 = 2 * core, 2 * core + 1
    m["xin"] = np.ascontiguousarray(np.concatenate([I["x"][s0], I["x"][s1], I["ctx"][s0], I["ctx"][s1]], axis=0), dtype=np.float32)
    rows = np.stack([I["c"][s0], I["c"][s1], I["c_ctx"]], axis=0).astype(np.float32)
    m["cin"] = np.ascontiguousarray(rows.reshape(3, 16, 128).transpose(2, 1, 0).reshape(128, 48))
    return m


def _gla_phase(self, l, s, hp):
    S = self.S
    cf = 4096 + s * 256
    lf = s * 2048
    with self.phase() as P:
        def load_fm(t, row0, nrows, q="sp", after=()):
            S.dma(q, lambda h: h.dma_start(out=t[0:nrows, 0:256], in_=self.PFM[row0:row0 + nrows, cf:cf + 256]), reads=list(after), writes=[t.r], join=True)
            S.dma(q, lambda h: h.dma_start(out=t[0:nrows, 256:2304], in_=self.PFM[row0:row0 + nrows, lf:lf + 2048]), reads=list(after), writes=[t.r], join=True)

        def load_tok(t, col0, ncols):
            S.dma("sp", lambda h: h.dma_start(out=t[:, 0:GCC, :], in_=self.PTOK[cf:cf + 256, col0:col0 + ncols].rearrange("(c p) n -> p c n", p=GCH)),
                  writes=[t.r], join=True)
            S.dma("sp", lambda h: h.dma_start(out=t[:, GCC:GNC, :], in_=self.PTOK[lf:lf + 2048, col0:col0 + ncols].rearrange("(c p) n -> p c n", p=GCH)),
                  writes=[t.r], join=True)

        def load_fm2(t, hsel, row0):
            S.dma("sp", lambda h: h.dma_start(out=t[:, hsel, 0:256], in_=self.PFM[row0:row0 + 64, cf:cf + 256]), writes=[t.r], join=True)
            S.dma("sp", lambda h: h.dma_start(out=t[:, hsel, 256:2304], in_=self.PFM[row0:row0 + 64, lf:lf + 2048]), writes=[t.r], join=True)

        qT = P.sb("qT", [64, 2, 2304], BF16)
        kT = P.sb("kT", [64, 2, 2304], BF16)
        for hh in range(2):
            load_fm2(qT, hh, FM_QG + hp * 128 + hh * 64)
            load_fm2(kT, hh, FM_KG + hp * 128 + hh * 64)
        aT = [P.sb("aT%d" % d, [17, 2304], BF16) for d in range(2)]
        wa = P.sb("wa", [17, 2, 128], BF16)
        for d in range(2):
            ms = Res()
            S.op("pool", lambda h, d=d: h.memset(aT[d][:], 1.0), writes=[aT[d].r, ms])
            load_fm(aT[d], FM_A + d * 16, 16, after=[ms])
            S.dma("pool", lambda h, d=d: h.dma_start(out=wa[:, d, :], in_=self.gla_wa[l][d][:, hp * 128:(hp + 1) * 128]),
                  writes=[wa.r], join=True)
        kt = P.sb("kt", [GCH, GNC, 128], BF16)
        vt = P.sb("vt", [GCH, GNC, 256], BF16)
        load_tok(kt, PT_KG + hp * 128, 128)
        load_tok(vt, PT_VG + hp * 256, 256)
        U = P.sb("U", [GCH, 4, GCH], F32)
        MK = P.sb("MK", [GCH, 2, 2, GCH], F32)
        idb = P.sb("idb", [128, 128], BF16)
        gbc = P.sb("gbc", [GCH, 128], F32)
        S.dma("sp", lambda h: h.dma_start(out=U[:], in_=self.gla_u.rearrange("p (a b) -> p a b", b=GCH)), writes=[U.r])
        S.dma("sp", lambda h: h.dma_start(out=MK[:], in_=self.gla_mask.rearrange("p (a b c) -> p a b c", b=2, c=GCH)), writes=[MK.r])
        S.dma("sp", lambda h: h.dma_start(out=idb[:], in_=self.ident_b), writes=[idb.r])
        S.dma("sp", lambda h: h.dma_start(out=gbc[:], in_=self.gla_g[l].broadcast_to([GCH, 128])), writes=[gbc.r])
        sp = P.sb("sp", [GCH, GNC, 128], F32)
        e1 = P.sb("e1", [GCH, 4, 128], F32)
        tq = P.sb("tq", [64, 2, 256], F32)
        tk = P.sb("tk", [64, 2, 256], F32)
        qin = [P.sb("qin%d" % d, [64, 2, 2304], BF16) for d in range(2)]
        kin = [P.sb("kin%d" % d, [64, 2, 2304], BF16) for d in range(2)]
        kte = [P.sb("kte%d" % d, [GCH, GNC, 128], BF16) for d in range(2)]
        dec = [P.sb("dec%d" % d, [64, 2, GNC], F32) for d in range(2)]
        S32 = [P.sb("S32%d" % d, [64, 2, 128], F32) for d in range(2)]
        Sbf = [P.sb("Sbf%d" % d, [64, 2, 128], BF16) for d in range(2)]
        at = [P.sb("at%d" % d, [GCH, 2, GCH], BF16) for d in range(2)]
        oacc = P.sb("oacc", [GCH, GNC, 256], F32)
        ores = [Res() for _ in range(GNC)]
        bk = [P.ps("bk%d" % i, [128, 512]) for i in range(6)]
        tb = [P.ps("tb%d" % i, [128, 1024], BF16) for i in range(2)]
        S.op("pool", lambda h: h.memset(oacc[:], 0.0), writes=ores)
        for d in range(2):
            S.op("pool", lambda h, d=d: h.memset(S32[d][:], 0.0), writes=[S32[d].r])
            S.op("pool", lambda h, d=d: h.memset(Sbf[d][:], 0.0), writes=[Sbf[d].r])
        for d in range(2):
            ui, ui2 = (0, 1) if d == 0 else (2, 3)
            zb = [(c0, min(4, GNC - c0)) for c0 in range(0, GNC, 4)]
            for bi_, (c0, nb) in enumerate(zb):
                b = bk[bi_ % 2]
                pz = b[0:GCH, :].rearrange("p (a b) -> p a b", b=128)

                def mmz(h, pz=pz, c0=c0, nb=nb, d=d):
                    for j in range(nb):
                        c = c0 + j
                        ins = h.matmul(pz[:, j, :], lhsT=aT[d][:, c * GCH:(c + 1) * GCH], rhs=wa[:, d, :], start=True, stop=True)
                    return ins
                S.op("pe", mmz, reads=[aT[d].r, wa.r], writes=[b.r])
                S.op("act", lambda h, pz=pz, nb=nb: h.activation(out=e1[:, 0:nb, :], in_=pz[:, 0:nb, :], func=AF.Exp, scale=-1.0), reads=[b.r], writes=[e1.r])
                S.op("act", lambda h, c0=c0, nb=nb: h.activation(out=sp[:, c0:c0 + nb, :], in_=e1[:, 0:nb, :], func=AF.Ln, bias=1.0),
                     reads=[e1.r], writes=[sp.r], join=(c0 > 0))
            for g in range(9):
                c0 = g * (256 // GCH)
                b = bk[2 + g % 2]
                pb = b[0:64, :].rearrange("p (a b) -> p a b", b=256)

                def mmb(h, pb=pb, c0=c0, ui=ui):
                    for hh in range(2):
                        for j in range(256 // GCH):
                            ins = h.matmul(pb[:, hh, j * GCH:(j + 1) * GCH], lhsT=sp[:, c0 + j, hh * 64:(hh + 1) * 64], rhs=U[:, ui, :], start=True, stop=True)
                    return ins
                S.op("pe", mmb, reads=[sp.r, U.r], writes=[b.r])
                S.op("act", lambda h, pb=pb: h.activation(out=tq[:], in_=pb, func=AF.Exp), reads=[b.r], writes=[tq.r])
                S.op("act", lambda h, pb=pb: h.activation(out=tk[:], in_=pb, func=AF.Exp, scale=-1.0), reads=[b.r], writes=[tk.r])
                cs = slice(g * 256, g * 256 + 256)
                S.op("dve", lambda h, cs=cs, d=d: h.scalar_tensor_tensor(
                    out=qin[d][:, :, cs], in0=tq[:], scalar=0.125, in1=qT[:, :, cs], op0=ALU.mult, op1=ALU.mult),
                    reads=[tq.r, qT.r], writes=[qin[d].r], join=True)
                S.op("pool", lambda h, cs=cs, d=d: h.tensor_tensor(out=kin[d][:, :, cs], in0=tk[:], in1=kT[:, :, cs], op=ALU.mult),
                     reads=[tk.r, kT.r], writes=[kin[d].r], join=True)
            for bi_, (c0, nb) in enumerate(zb):
                b = bk[4 + bi_ % 2]
                pc = b[0:GCH, :].rearrange("p (a b) -> p a b", b=128)

                def mmc(h, pc=pc, c0=c0, nb=nb, ui2=ui2):
                    for j in range(nb):
                        ins = h.matmul(pc[:, j, :], lhsT=U[:, ui2, :], rhs=sp[:, c0 + j, :], start=True, stop=True)
                    return ins
                S.op("pe", mmc, reads=[sp.r, U.r], writes=[b.r])
                S.op("act", lambda h, pc=pc, nb=nb: h.activation(out=e1[:, 0:nb, :], in_=pc[:, 0:nb, :], func=AF.Exp), reads=[b.r], writes=[e1.r])
                S.op("dve", lambda h, c0=c0, nb=nb, d=d: h.tensor_tensor(out=kte[d][:, c0:c0 + nb, :], in0=e1[:, 0:nb, :], in1=kt[:, c0:c0 + nb, :], op=ALU.mult),
                     reads=[e1.r, kt.r], writes=[kte[d].r], join=True)
            b = bk[4]
            pd = b[0:64, 0:2 * GNC].rearrange("p (a b) -> p a b", b=GNC)

            def mmd(h, pd=pd):
                for hh in range(2):
                    for c in range(GNC):
                        ins = h.matmul(pd[:, hh, c:c + 1], lhsT=sp[:, c, hh * 64:(hh + 1) * 64], rhs=U[:, 0, GCH - 1:GCH], start=True, stop=True)
                return ins
            S.op("pe", mmd, reads=[sp.r, U.r], writes=[b.r])
            S.op("act", lambda h, pd=pd, d=d: h.activation(out=dec[d][:], in_=pd, func=AF.Exp), reads=[b.r], writes=[dec[d].r])
        order = [list(range(GNC)), list(range(GCC - 1, -1, -1)) + list(range(GNC - 1, GCC - 1, -1))]
        psA = [bk[0], bk[3]]
        psO = [bk[1], bk[4]]
        psS = [bk[2], bk[5]]
        for step in range(GNC):
            cc = [order[0][step], order[1][step]]
            for d in range(2):
                c = cc[d]

                def mm1(h, c=c, d=d):
                    cs = slice(c * GCH, (c + 1) * GCH)
                    for hh in range(2):
                        ins = h.matmul(psA[d][0:GCH, hh * GCH:(hh + 1) * GCH], lhsT=kin[d][:, hh, cs], rhs=qin[d][:, hh, cs], start=True, stop=True)
                    return ins
                S.op("pe", mm1, reads=[kin[d].r, qin[d].r], writes=[psA[d].r])
            for d in range(2):
                c = cc[d]

                def mm3(h, c=c, d=d):
                    for hh in range(2):
                        ins = h.matmul(psS[d][0:64, hh * 128:(hh + 1) * 128], lhsT=kte[d][:, c, hh * 64:(hh + 1) * 64],
                                       rhs=vt[:, c, hh * 128:(hh + 1) * 128], start=True, stop=True)
                    return ins
                S.op("pe", mm3, reads=[kte[d].r, vt.r], writes=[psS[d].r])
            for d in range(2):
                S.op("dve", lambda h, d=d: h.tensor_tensor(out=at[d][:], in0=psA[d][0:GCH, 0:2 * GCH].rearrange("p (a b) -> p a b", b=GCH),
                                                           in1=MK[:, d, :, :], op=ALU.mult),
                     reads=[psA[d].r, MK.r], writes=[at[d].r])
            for d in range(2):
                c = cc[d]

                def mm2(h, c=c, d=d):
                    cs = slice(c * GCH, (c + 1) * GCH)
                    for hh in range(2):
                        h.matmul(psO[d][0:GCH, hh * 128:(hh + 1) * 128], lhsT=at[d][:, hh, :], rhs=vt[:, c, hh * 128:(hh + 1) * 128], start=True, stop=False)
                        ins = h.matmul(psO[d][0:GCH, hh * 128:(hh + 1) * 128], lhsT=qin[d][:, hh, cs], rhs=Sbf[d][:, hh, :], start=False, stop=True)
                    return ins
                S.op("pe", mm2, reads=[at[d].r, vt.r, qin[d].r, Sbf[d].r], writes=[psO[d].r])
            for d in range(2):
                c = cc[d]
                S.op("dve", lambda h, c=c, d=d: h.tensor_tensor(out=oacc[:, c, :], in0=oacc[:, c, :], in1=psO[d][0:GCH, 0:256], op=ALU.add),
                     reads=[psO[d].r, ores[c]], writes=[ores[c]])
                for hh in range(2):
                    S.op("dve", lambda h, c=c, d=d, hh=hh: h.scalar_tensor_tensor(
                        out=S32[d][:, hh, :], in0=S32[d][:, hh, :], scalar=dec[d][:, hh, c:c + 1], in1=psS[d][0:64, hh * 128:(hh + 1) * 128],
                        op0=ALU.mult, op1=ALU.add), reads=[S32[d].r, dec[d].r, psS[d].r], writes=[S32[d].r])
                S.op("act", lambda h, d=d: h.copy(out=Sbf[d][:], in_=S32[d][:]), reads=[S32[d].r], writes=[Sbf[d].r])
        NG = 256 // GCH
        sqb = P.sb("sqb", [GCH, NG, 256], F32)
        srb = P.sb("srb", [GCH, NG, 256], F32)
        yab = P.sb("yab", [GCH, NG, 256], BF16)
        rtb = [P.sb("rtb%d" % i, [GCH, NG, 256], BF16) for i in range(2)]
        rs = P.sb("rs", [GCH, 2 * NG, 3], F32)
        brs = P.sb("brs", [128, 2, 2304], BF16)
        for g in range(9):
            c0 = g * NG
            f0 = cf if g == 0 else lf + (g - 1) * 256
            rt = rtb[g % 2]
            S.dma("sp", lambda h, rt=rt, f0=f0: h.dma_start(
                out=rt[:], in_=self.PTOK[f0:f0 + 256, PT_RG + hp * 256:PT_RG + hp * 256 + 256].rearrange("(c p) n -> p c n", p=GCH)), writes=[rt.r])
            rr = ores[c0:c0 + NG]
            ov = oacc[:, c0:c0 + NG, :]
            S.op("dve", lambda h, ov=ov: h.tensor_tensor(out=sqb[:], in0=ov, in1=ov, op=ALU.mult), reads=rr, writes=[sqb.r])
            S.op("dve", lambda h: h.tensor_reduce(out=rs[:, :, 0], in_=sqb[:].rearrange("p c (a b) -> p (c a) b", b=128),
                                                  axis=AX.X, op=ALU.add), reads=[sqb.r], writes=[rs.r])
            S.op("dve", lambda h: h.tensor_scalar(out=rs[:, :, 1], in0=rs[:, :, 0], scalar1=1.0 / 128, scalar2=EPS,
                                                  op0=ALU.mult, op1=ALU.add), reads=[rs.r], writes=[rs.r])
            S.op("act", lambda h: h.sqrt(out=rs[:, :, 2], in_=rs[:, :, 1]), reads=[rs.r], writes=[rs.r])
            S.op("dve", lambda h: h.reciprocal(out=rs[:, :, 1], in_=rs[:, :, 2]), reads=[rs.r], writes=[rs.r])
            ov3 = ov.rearrange("p c (a b) -> p (c a) b", b=128)
            sq3 = sqb[:].rearrange("p c (a b) -> p (c a) b", b=128)
            S.op("dve", lambda h, ov3=ov3, sq3=sq3: h.tensor_tensor(out=sq3, in0=ov3, in1=rs[:, :, 1:2].broadcast_to([GCH, 2 * NG, 128]), op=ALU.mult),
                 reads=rr + [rs.r], writes=[sqb.r])
            S.op("pool", lambda h, sq3=sq3: h.tensor_tensor(out=sq3, in0=sq3, in1=gbc[:, :].unsqueeze(1).broadcast_to([GCH, 2 * NG, 128]), op=ALU.mult),
                 reads=[sqb.r, gbc.r], writes=[sqb.r])
            S.op("act", lambda h, rt=rt: h.activation(out=srb[:], in_=rt[:], func=AF.Silu), reads=[rt.r], writes=[srb.r])
            S.op("dve", lambda h: h.tensor_tensor(out=yab[:], in0=sqb[:], in1=srb[:], op=ALU.mult),
                 reads=[sqb.r, srb.r], writes=[yab.r])
            for ct in range(2):
                b = tb[ct]
                pv = b[:, 0:256]

                def trf(h, pv=pv, ct=ct):
                    for j in range(NG):
                        ins = h.transpose(pv[:, j * GCH:(j + 1) * GCH], yab[:, j, ct * 128:(ct + 1) * 128], idb[0:GCH, 0:GCH])
                    return ins
                S.op("pe", trf, reads=[yab.r, idb.r], writes=[b.r])
                S.op("act", lambda h, pv=pv, ct=ct, g=g: h.copy(out=brs[:, ct, g * 256:(g + 1) * 256], in_=pv),
                     reads=[b.r], writes=[brs.r], join=True)
        for ct in range(2):
            r0 = hp * 256 + ct * 128
            S.dma("sp", lambda h, ct=ct, r0=r0: h.dma_start(out=self.BRT[r0:r0 + 128, cf:cf + 256], in_=brs[:, ct, 0:256]), reads=[brs.r])
            S.dma("sp", lambda h, ct=ct, r0=r0: h.dma_start(out=self.BRT[r0:r0 + 128, lf:lf + 2048], in_=brs[:, ct, 256:2304]), reads=[brs.r])


Builder._gla_phase = _gla_phase


def _p_gla(self, l):
    for s in range(NS):
        for hp in range(2):
            try:
                self._gla_phase(l, s, hp)
            except _Stop:
                return


Builder.p_gla = _p_gla


def _p_sgu(self, l):
    S = self.S
    with self.phase() as P:
        idb = P.sb("idb", [128, 128], BF16)
        wsT = P.sb("wsT", [128, 512], BF16)
        bs = P.sb("bs", [128, 4], F32)
        lng = P.sb("lng", [128, 512], F32)
        lnb = P.sb("lnb", [128, 512], F32)
        S.dma("sp", lambda h: h.dma_start(out=idb[:], in_=self.ident_b), writes=[idb.r])
        S.dma("pool", lambda h: h.dma_start(out=wsT[:], in_=self.sgu_wsT[l]), writes=[wsT.r])
        S.dma("sp", lambda h: h.dma_start(out=bs[:], in_=self.sgu_bs[l]), writes=[bs.r])
        S.dma("sp", lambda h: h.dma_start(out=lng[:], in_=self.sgu_lng[l].broadcast_to([128, 512])), writes=[lng.r])
        S.dma("sp", lambda h: h.dma_start(out=lnb[:], in_=self.sgu_lnb[l].broadcast_to([128, 512])), writes=[lnb.r])
        ND = 6
        uv = [P.sb("uv%d" % i, [128, 1024], BF16) for i in range(2)]
        gv = [P.sb("gv%d" % i, [128, 512], F32) for i in range(2)]
        gu = [P.sb("gu%d" % i, [128, 512], F32) for i in range(ND)]
        xc = [P.sb("xc%d" % i, [128, 512], F32) for i in range(ND)]
        sq = [P.sb("sq%d" % i, [128, 512], F32) for i in range(ND)]
        st = [P.sb("st%d" % i, [128, 6], F32) for i in range(ND)]
        vn = [P.sb("vn%d" % i, [128, 512], BF16) for i in range(2)]
        yd = [P.sb("yd%d" % i, [128, 512], BF16) for i in range(2)]
        stg = [P.sb("stg%d" % i, [128, 4, 512], BF16) for i in range(2)]
        pm = [P.ps("pm%d" % i, [128, 512]) for i in range(2)]
        pt = [P.ps("pt%d" % i, [128, 4, 128], BF16) for i in range(2)]
        tiles = list(range(NTL)) if l == 0 else list(range(32))

        def stA(i):
            tt = tiles[i]
            t_ = uv[i % 2]
            st_ = st[i % ND]
            S.dma("sp", lambda h: h.dma_start(out=t_[:], in_=self.PTOK[tt * 128:(tt + 1) * 128, PT_U:PT_U + 1024]), writes=[t_.r])
            S.op("act", lambda h: h.activation(out=gv[i % 2][:], in_=t_[:, 512:1024], func=AF.Gelu_apprx_tanh, accum_out=st_[:, 0:1]),
                 reads=[t_.r], writes=[gv[i % 2].r, st_.r])
            S.op("act", lambda h: h.activation(out=gu[i % ND][:], in_=t_[:, 0:512], func=AF.Gelu_apprx_tanh), reads=[t_.r], writes=[gu[i % ND].r])
            S.op("dve", lambda h: h.tensor_scalar(out=st_[:, 1:2], in0=st_[:, 0:1], scalar1=-1.0 / 512, scalar2=None, op0=ALU.mult),
                 reads=[st_.r], writes=[st_.r])
            S.op("dve", lambda h: h.tensor_scalar(out=xc[i % ND][:], in0=gv[i % 2][:], scalar1=st_[:, 1:2], scalar2=None, op0=ALU.add),
                 reads=[gv[i % 2].r, st_.r], writes=[xc[i % ND].r])

        def stA2(i):
            st_ = st[i % ND]
            S.op("act", lambda h: h.activation(out=sq[i % ND][:], in_=xc[i % ND][:], func=AF.Square, accum_out=st_[:, 2:3]), reads=[xc[i % ND].r], writes=[sq[i % ND].r, st_.r])
            S.op("dve", lambda h: h.tensor_scalar(out=st_[:, 3:4], in0=st_[:, 2:3], scalar1=1.0 / 512, scalar2=EPS, op0=ALU.mult, op1=ALU.add),
                 reads=[st_.r], writes=[st_.r])

        def stA3(i):
            st_ = st[i % ND]
            S.op("act", lambda h: h.sqrt(out=st_[:, 4:5], in_=st_[:, 3:4]), reads=[st_.r], writes=[st_.r])
            S.op("dve", lambda h: h.reciprocal(out=st_[:, 5:6], in_=st_[:, 4:5]), reads=[st_.r], writes=[st_.r])

        def stB(i):
            st_ = st[i % ND]
            S.op("dve", lambda h: h.scalar_tensor_tensor(out=sq[i % ND][:], in0=xc[i % ND][:], scalar=st_[:, 5:6], in1=lng[:], op0=ALU.mult, op1=ALU.mult),
                 reads=[xc[i % ND].r, st_.r, lng.r], writes=[sq[i % ND].r])
            S.op("pool", lambda h: h.tensor_tensor(out=vn[i % 2][:], in0=sq[i % ND][:], in1=lnb[:], op=ALU.add), reads=[sq[i % ND].r, lnb.r], writes=[vn[i % 2].r])
            p = pm[i % 2]
            v_ = vn[i % 2]

            def mm(h):
                for g in range(4):
                    ins = h.matmul(p[:, g * 128:(g + 1) * 128], lhsT=wsT[:, g * 128:(g + 1) * 128], rhs=v_[:, g * 128:(g + 1) * 128], start=True, stop=True)
                return ins
            S.op("pe", mm, reads=[wsT.r, v_.r], writes=[p.r])

        def stC(i):
            tt = tiles[i]
            p = pm[i % 2]
            y_ = yd[i % 2]
            g_ = gu[i % ND]
            for g in range(4):
                S.op("dve", lambda h, g=g: h.scalar_tensor_tensor(
                    out=y_[:, g * 128:(g + 1) * 128], in0=p[:, g * 128:(g + 1) * 128], scalar=bs[:, g:g + 1], in1=g_[:, g * 128:(g + 1) * 128],
                    op0=ALU.add, op1=ALU.mult), reads=[p.r, bs.r, g_.r], writes=[y_.r], join=(g > 0))
            q = pt[i % 2]

            def tr(h):
                for g in range(4):
                    ins = h.transpose(q[:, g, :], y_[:, g * 128:(g + 1) * 128], idb[:])
                return ins
            S.op("pe", tr, reads=[y_.r, idb.r], writes=[q.r])
            sg = stg[(i // 4) % 2]
            S.op("act", lambda h: h.copy(out=sg[:, :, (i % 4) * 128:(i % 4 + 1) * 128], in_=q[:]), reads=[q.r], writes=[sg.r], join=True)
            if i % 4 == 3:
                f0 = (tt - 3) * 128
                S.dma("sp", lambda h: h.dma_start(
                    out=self.BRT[1536:2048, f0:f0 + 512].rearrange("(g p) n -> p g n", p=128), in_=sg[:]), reads=[sg.r])
        pipeline(len(tiles), [stA, stA2, stA3, stB, stC])


Builder.p_sgu = _p_sgu


def _swa_phase(self, l, s):
    S = self.S
    cf = 4096 + s * 256
    lf = s * 2048
    need_ctx = (l == 0)
    with self.phase() as P:
        idb = P.sb("idb", [128, 128], BF16)
        S.dma("sp", lambda h: h.dma_start(out=idb[:], in_=self.ident_b), writes=[idb.r])
        m01 = P.sb("m01", [128, 2, 512], BF16)
        S.dma("sp", lambda h: h.dma_start(out=m01[:], in_=self.swa_negm.rearrange("p (a b) -> p a b", b=512)), writes=[m01.r])
        esk = P.sb("esk", [128, 8], F32)
        S.dma("sp", lambda h: h.dma_start(out=esk[:], in_=self.swa_sink[l].broadcast_to([128, 8])), writes=[esk.r])
        S.op("act", lambda h: h.activation(out=esk[:], in_=esk[:], func=AF.Exp), reads=[esk.r], writes=[esk.r])
        qT = P.sb("qT", [64, 8, 2048 + 256], BF16)
        kT = P.sb("kT", [64, 2, 2048 + 256], BF16)
        vaug = P.sb("vaug", [128, 18, 2, 65], BF16)
        msr = Res()
        S.op("pool", lambda h: h.memset(vaug[:], 1.0), writes=[vaug.r, msr])
        for g in range(2):
            S.dma("sp", lambda h, g=g: h.dma_start(out=vaug[:, 0:16, g, 0:64],
                                                   in_=self.PTOK[lf:lf + 2048, PT_VS + g * 64:PT_VS + (g + 1) * 64].rearrange("(t p) d -> p t d", p=128)),
                  reads=[msr], writes=[vaug.r], join=True)
            S.dma("sp", lambda h, g=g: h.dma_start(out=vaug[:, 16:18, g, 0:64],
                                                   in_=self.PTOK[cf:cf + 256, PT_VS + g * 64:PT_VS + (g + 1) * 64].rearrange("(t p) d -> p t d", p=128)),
                  reads=[msr], writes=[vaug.r], join=True)
            S.dma("sp", lambda h, g=g: h.dma_start(out=kT[:, g, 2048:2304], in_=self.PFM[FM_KS + g * 64:FM_KS + (g + 1) * 64, cf:cf + 256]),
                  writes=[kT.r], join=True)
        qk = [P.sb("qk%d" % i, [128, 640], BF16) for i in range(2)]
        cs = [P.sb("cs%d" % i, [128, 64], F32) for i in range(2)]
        t1 = P.sb("t1", [128, 10, 32], F32)
        t2 = P.sb("t2", [128, 10, 32], F32)
        t3 = P.sb("t3", [128, 10, 32], F32)
        t4 = P.sb("t4", [128, 10, 32], F32)
        qr = [P.sb("qr%d" % i, [128, 10, 2, 32], BF16) for i in range(2)]
        tqb = P.ps("tqb", [128, 8, 128], BF16)
        tkb = P.ps("tkb", [128, 2, 128], BF16)
        ntile = 18 if need_ctx else 16
        for i in range(ntile):
            x = qk[i % 2]
            f0 = lf + i * 128 if i < 16 else cf + (i - 16) * 128
            S.dma("sp", lambda h, x=x, f0=f0: h.dma_start(out=x[:], in_=self.PTOK[f0:f0 + 128, PT_QS:PT_QS + 640]), writes=[x.r])
            if i < 16:
                c_ = cs[i % 2]
                S.dma("sp", lambda h, c_=c_, i=i: h.dma_start(out=c_[:], in_=self.rope_cs[i * 128:(i + 1) * 128, :]), writes=[c_.r])
                xv = x[:].rearrange("p (h a d) -> p h a d", a=2, d=32)
                x1 = xv[:, :, 0, :]
                x2 = xv[:, :, 1, :]
                cosb = c_[:, 0:32].unsqueeze(1).broadcast_to([128, 10, 32])
                sinb = c_[:, 32:64].unsqueeze(1).broadcast_to([128, 10, 32])
                q_ = qr[i % 2]
                S.op("dve", lambda h, x1=x1, cosb=cosb: h.tensor_tensor(out=t1[:], in0=x1, in1=cosb, op=ALU.mult), reads=[x.r, c_.r], writes=[t1.r])
                S.op("pool", lambda h, x2=x2, sinb=sinb: h.tensor_tensor(out=t2[:], in0=x2, in1=sinb, op=ALU.mult), reads=[x.r, c_.r], writes=[t2.r])
                S.op("pool", lambda h, x1=x1, sinb=sinb: h.tensor_tensor(out=t3[:], in0=x1, in1=sinb, op=ALU.mult), reads=[x.r, c_.r], writes=[t3.r])
                S.op("dve", lambda h, x2=x2, cosb=cosb: h.tensor_tensor(out=t4[:], in0=x2, in1=cosb, op=ALU.mult), reads=[x.r, c_.r], writes=[t4.r])
                S.op("dve", lambda h, q_=q_: h.tensor_tensor(out=q_[:, :, 0, :], in0=t1[:], in1=t2[:], op=ALU.subtract), reads=[t1.r, t2.r], writes=[q_.r])
                S.op("pool", lambda h, q_=q_: h.tensor_tensor(out=q_[:, :, 1, :], in0=t3[:], in1=t4[:], op=ALU.add), reads=[t3.r, t4.r], writes=[q_.r], join=True)
                src = q_[:].rearrange("p h a d -> p h (a d)")
                srcr = q_.r
            else:
                src = x[:].rearrange("p (h d) -> p h d", d=64)
                srcr = x.r

            def trq(h, src=src):
                for hd in range(8):
                    ins = h.transpose(tqb[0:64, hd, :], src[:, hd, :], idb[:])
                return ins
            S.op("pe", trq, reads=[srcr, idb.r], writes=[tqb.r])
            S.op("act", lambda h, i=i: h.copy(out=qT[:, :, i * 128:(i + 1) * 128], in_=tqb[0:64, :, :]), reads=[tqb.r], writes=[qT.r], join=True)
            if i < 16:
                def trk(h, src=src):
                    for hd in range(2):
                        ins = h.transpose(tkb[0:64, hd, :], src[:, 8 + hd, :], idb[:])
                    return ins
                S.op("pe", trk, reads=[srcr, idb.r], writes=[tkb.r])
                S.op("act", lambda h, i=i: h.copy(out=kT[:, :, i * 128:(i + 1) * 128], in_=tkb[0:64, :, :]), reads=[tkb.r], writes=[kT.r], join=True)
        PT = [[P.sb("PT%d_%d" % (a, j), [128, 512], BF16) for j in range(5)] for a in range(3)]
        psc = [P.ps("psc%d" % i, [128, 512]) for i in range(3)]
        pso = [P.ps("pso%d" % i, [128, 4, 65]) for i in range(2)]
        pto = P.ps("pto", [128, 4, 128], BF16)
        yb = [P.sb("yb%d" % i, [128, 8, 64], BF16) for i in range(2)]
        den = P.sb("den", [128, 4], F32)
        rec = P.sb("rec", [128, 4], F32)
        stg = [P.sb("stg%d" % i, [128, 4, 512], BF16) for i in range(2)]
        items = []
        for n in range(ntile):
            for g in range(2):
                if n < 16:
                    kts = ([(n - 1, 0)] if n > 0 else []) + [(n, None)] + ([(n + 1, 1)] if n < 15 else []) + [(16, None), (17, None)]
                else:
                    kts = [(16, None), (17, None)]
                items.append((n, g, kts))
        sci = [0]

        def scores(it, a):
            n, g, kts = it
            for j, (ktile, mi) in enumerate(kts):
                p = psc[sci[0] % 3]
                sci[0] += 1
                S.op("pe", lambda h, p=p, ktile=ktile, g=g, n=n: h.matmul(
                    p[:], lhsT=kT[:, g, ktile * 128:(ktile + 1) * 128], rhs=qT[:, 4 * g:4 * g + 4, n * 128:(n + 1) * 128], start=True, stop=True),
                    reads=[kT.r, qT.r], writes=[p.r])
                t_ = PT[a][j]
                S.op("act", lambda h, p=p, t_=t_: h.activation(out=t_[:], in_=p[:], func=AF.Exp, scale=0.125), reads=[p.r], writes=[t_.r])
                if mi is not None:
                    S.op("pool", lambda h, t_=t_, mi=mi: h.tensor_tensor(out=t_[:], in0=t_[:], in1=m01[:, mi, :], op=ALU.mult),
                         reads=[t_.r, m01.r], writes=[t_.r])

        def pv(it, a, idx):
            n, g, kts = it
            po = pso[idx % 2]

            def mm(h, po=po, kts=kts, a=a, g=g):
                for hq in range(4):
                    for j, (ktile, mi) in enumerate(kts):
                        ins = h.matmul(po[:, hq, :], lhsT=PT[a][j][:, hq * 128:(hq + 1) * 128], rhs=vaug[:, ktile, g, :],
                                       start=(j == 0), stop=(j == len(kts) - 1))
                return ins
            S.op("pe", mm, reads=[PT[a][j].r for j in range(len(kts))] + [vaug.r], writes=[po.r])
            y = yb[n % 2]
            S.op("dve", lambda h, po=po, g=g: h.tensor_tensor(out=den[:], in0=po[:, :, 64], in1=esk[:, 4 * g:4 * g + 4], op=ALU.add),
                 reads=[po.r, esk.r], writes=[den.r])
            S.op("dve", lambda h: h.reciprocal(out=rec[:], in_=den[:]), reads=[den.r], writes=[rec.r])
            S.op("dve", lambda h, po=po, y=y, g=g: h.tensor_tensor(out=y[:, 4 * g:4 * g + 4, :], in0=po[:, :, 0:64],
                                                                   in1=rec[:, :].unsqueeze(2).broadcast_to([128, 4, 64]), op=ALU.mult),
                 reads=[po.r, rec.r], writes=[y.r], join=(g == 1))
            if g == 1:
                def tr(h, y=y):
                    yv = y[:].rearrange("p h d -> p (h d)")
                    for ct in range(4):
                        ins = h.transpose(pto[:, ct, :], yv[:, ct * 128:(ct + 1) * 128], idb[:])
                    return ins
                S.op("pe", tr, reads=[y.r, idb.r], writes=[pto.r])
                sg = stg[(n // 4) % 2]
                S.op("act", lambda h, sg=sg, n=n: h.copy(out=sg[:, :, (n % 4) * 128:(n % 4 + 1) * 128], in_=pto[:]), reads=[pto.r], writes=[sg.r], join=True)
                if n % 4 == 3 or n == 17:
                    if n < 16:
                        f0 = lf + (n - 3) * 128
                        S.dma("sp", lambda h, sg=sg, f0=f0: h.dma_start(
                            out=self.BRT[512:1024, f0:f0 + 512].rearrange("(g p) n -> p g n", p=128), in_=sg[:]), reads=[sg.r])
                    else:
                        S.dma("sp", lambda h, sg=sg: h.dma_start(
                            out=self.BRT[512:1024, cf:cf + 256].rearrange("(g p) n -> p g n", p=128), in_=sg[:, :, 0:256]), reads=[sg.r])

        scores(items[0], 0)
        scores(items[1], 1)
        for i, it in enumerate(items):
            if i + 2 < len(items):
                scores(items[i + 2], (i + 2) % 3)
            pv(it, i % 3, i)


Builder._swa_phase = _swa_phase


def _p_swa(self, l):
    for s in range(NS):
        self._swa_phase(l, s)


Builder.p_swa = _p_swa


def _na_phase(self, l, s):
    S = self.S
    cf = 4096 + s * 256
    lf = s * 2048
    need_ctx = (l == 0)
    with self.phase() as P:
        idb = P.sb("idb", [128, 128], BF16)
        S.dma("sp", lambda h: h.dma_start(out=idb[:], in_=self.ident_b), writes=[idb.r])
        qT = P.sb("qT", [64, 8, 2304], BF16)
        kT = P.sb("kT", [64, 8, 2304], BF16)
        for hd in range(8):
            for (t_, base) in ((qT, FM_QN), (kT, FM_KN)):
                r0 = base + hd * 64
                S.dma("sp", lambda h, t_=t_, r0=r0, hd=hd: h.dma_start(out=t_[:, hd, 0:2048], in_=self.PFM[r0:r0 + 64, lf:lf + 2048]), writes=[t_.r], join=True)
                S.dma("sp", lambda h, t_=t_, r0=r0, hd=hd: h.dma_start(out=t_[:, hd, 2048:2304], in_=self.PFM[r0:r0 + 64, cf:cf + 256]), writes=[t_.r], join=True)
        vE = P.sb("vE", [128, 18, 8, 65], BF16)
        vO = P.sb("vO", [128, 15, 8, 65], BF16)
        msE = Res()
        msO = Res()
        S.op("pool", lambda h: h.memset(vE[:], 1.0), writes=[vE.r, msE])
        S.op("pool", lambda h: h.memset(vO[:], 1.0), writes=[vO.r, msO])
        for hd in range(8):
            c0 = PT_VN + hd * 64
            S.dma("sp", lambda h, hd=hd, c0=c0: h.dma_start(out=vE[:, 0:16, hd, 0:64], in_=self.PTOK[lf:lf + 2048, c0:c0 + 64].rearrange("(t p) d -> p t d", p=128)),
                  reads=[msE], writes=[vE.r], join=True)
            S.dma("sp", lambda h, hd=hd, c0=c0: h.dma_start(out=vE[:, 16:18, hd, 0:64], in_=self.PTOK[cf:cf + 256, c0:c0 + 64].rearrange("(t p) d -> p t d", p=128)),
                  reads=[msE], writes=[vE.r], join=True)
            S.dma("sp", lambda h, hd=hd, c0=c0: h.dma_start(out=vO[:, 0:15, hd, 0:64], in_=self.PTOK[lf + 64:lf + 64 + 1920, c0:c0 + 64].rearrange("(t p) d -> p t d", p=128)),
                  reads=[msO], writes=[vO.r], join=True)
        E = P.sb("E", [128, 7168], BF16)
        gtmp = [P.sb("gtmp%d" % i, [128, 1792], F32) for i in range(2)]
        mtmp = [P.sb("mtmp%d" % i, [128, 1792], F32) for i in range(2)]
        for q4 in range(4):
            gt = gtmp[q4 % 2]
            mt = mtmp[q4 % 2]
            sl = slice(q4 * 1792, (q4 + 1) * 1792)
            S.dma("sp", lambda h, gt=gt, sl=sl: h.dma_start(out=gt[:], in_=self.na_g[l][:, sl]), writes=[gt.r])
            S.dma("sp", lambda h, mt=mt, sl=sl: h.dma_start(out=mt[:], in_=self.na_m[:, sl]), writes=[mt.r])
            S.op("act", lambda h, gt=gt: h.activation(out=gt[:], in_=gt[:], func=AF.Exp), reads=[gt.r], writes=[gt.r])
            S.op("dve", lambda h, gt=gt, mt=mt, sl=sl: h.tensor_tensor(out=E[:, sl], in0=gt[:], in1=mt[:], op=ALU.mult), reads=[gt.r, mt.r], writes=[E.r], join=True)
        Ev = E[:].rearrange("p (h r c) -> p h r c", h=8, r=14)
        PTt = [P.sb("PTt%d" % i, [128, 6, 64], BF16) for i in range(3)]
        psc = [P.ps("psc%d" % i, [128, 6, 64]) for i in range(3)]
        pso = [[P.ps("pso%d_%d" % (a, b), [64, 4, 65]) for b in range(2)] for a in range(2)]
        pto = P.ps("pto", [128, 4, 64], BF16)
        yn = P.sb("yn", [64, 8, 64], BF16)
        rec = P.sb("rec", [64, 8], F32)
        stg = [P.sb("stg%d" % i, [128, 4, 512], BF16) for i in range(2)]
        nrow = 36 if need_ctx else 32
        items = [(r, hd) for r in range(nrow) for hd in range(8)]

        def tiles_of(r):
            if r < 32:
                rs = min(max(r - 4, 0), 24)
                out = []
                for j in range(4):
                    if rs % 2 == 0:
                        out.append((rs * 64 + 128 * j, (vE, rs // 2 + j)))
                    else:
                        out.append((rs * 64 + 128 * j, (vO, (rs - 1) // 2 + j)))
                out += [(2048, (vE, 16)), (2048 + 128, (vE, 17))]
                return out, rs - r + 7
            return [(2048, (vE, 16)), (2048 + 128, (vE, 17))], None

        sci = [0]

        def scores(it, a):
            r, hd = it
            tl, ro0 = tiles_of(r)
            nt = len(tl)
            p = psc[sci[0] % 3]
            sci[0] += 1
            qc = r * 64 if r < 32 else 2048 + (r - 32) * 64

            def mm(h, p=p, tl=tl, hd=hd, qc=qc):
                for j, (kc, _) in enumerate(tl):
                    ins = h.matmul(p[:, j, :], lhsT=kT[:, hd, kc:kc + 128], rhs=qT[:, hd, qc:qc + 64], start=True, stop=True)
                return ins
            S.op("pe", mm, reads=[kT.r, qT.r], writes=[p.r])
            t_ = PTt[a]
            S.op("act", lambda h, p=p, t_=t_, nt=nt: h.activation(out=t_[:, 0:nt, :], in_=p[:, 0:nt, :], func=AF.Exp, scale=0.125), reads=[p.r], writes=[t_.r])
            if ro0 is not None:
                S.op("pool", lambda h, t_=t_, hd=hd, ro0=ro0: h.tensor_tensor(out=t_[:, 0:4, :], in0=t_[:, 0:4, :], in1=Ev[:, hd, ro0:ro0 + 7:2, :], op=ALU.mult),
                     reads=[t_.r, E.r], writes=[t_.r])

        def pv(it, a):
            r, hd = it
            tl, _ = tiles_of(r)
            po = pso[r % 2][hd // 4]

            def mm(h, po=po, tl=tl, a=a, hd=hd):
                for j, (_, (vt_, vi)) in enumerate(tl):
                    ins = h.matmul(po[:, hd % 4, :], lhsT=PTt[a][:, j, :], rhs=vt_[:, vi, hd, :], start=(j == 0), stop=(j == len(tl) - 1))
                return ins
            S.op("pe", mm, reads=[PTt[a].r, vE.r, vO.r], writes=[po.r])
            if hd % 4 == 3:
                b = hd // 4
                S.op("dve", lambda h, po=po, b=b: h.reciprocal(out=rec[:, 4 * b:4 * b + 4], in_=po[:, :, 64]), reads=[po.r], writes=[rec.r], join=(b == 1))
                S.op("dve", lambda h, po=po, b=b: h.tensor_tensor(out=yn[:, 4 * b:4 * b + 4, :], in0=po[:, :, 0:64],
                                                                  in1=rec[:, 4 * b:4 * b + 4].unsqueeze(2).broadcast_to([64, 4, 64]), op=ALU.mult),
                     reads=[po.r, rec.r], writes=[yn.r], join=(b == 1))
            if hd == 7:
                def tr(h):
                    yv = yn[:].rearrange("p h d -> p (h d)")
                    for ct in range(4):
                        ins = h.transpose(pto[:, ct, :], yv[:, ct * 128:(ct + 1) * 128], idb[0:64, 0:64])
                    return ins
                S.op("pe", tr, reads=[yn.r, idb.r], writes=[pto.r])
                sg = stg[(r // 8) % 2]
                S.op("act", lambda h, sg=sg, r=r: h.copy(out=sg[:, :, (r % 8) * 64:(r % 8 + 1) * 64], in_=pto[:]), reads=[pto.r], writes=[sg.r], join=True)
                if r % 8 == 7 and r < 32:
                    f0 = lf + (r - 7) * 64
                    S.dma("sp", lambda h, sg=sg, f0=f0: h.dma_start(
                        out=self.BRT[1024:1536, f0:f0 + 512].rearrange("(g p) n -> p g n", p=128), in_=sg[:]), reads=[sg.r])
                if r == 35:
                    S.dma("sp", lambda h, sg=sg: h.dma_start(
                        out=self.BRT[1024:1536, cf:cf + 256].rearrange("(g p) n -> p g n", p=128), in_=sg[:, :, 0:256]), reads=[sg.r])

        scores(items[0], 0)
        scores(items[1], 1)
        for i, it in enumerate(items):
            if i + 2 < len(items):
                scores(items[i + 2], (i + 2) % 3)
            pv(it, i % 3)


Builder._na_phase = _na_phase


def _p_na(self, l):
    for s in range(NS):
        self._na_phase(l, s)


Builder.p_na = _p_na


def _p_merge1(self, l):
    S = self.S
    ntok = NF if l == 0 else NS * T
    TBS = 1152 if l == 0 else 1024
    blocks = [(f0, TBS) for f0 in range(0, ntok, TBS)]
    for (f0, TB) in blocks:
        with self.phase() as P:
            hT = P.sb("hT", [128, 16, TB], BF16)
            bT = P.sb("bT", [128, 16, TB], BF16)
            for kq in range(4):
                S.dma("sp", lambda h, kq=kq: h.dma_start(out=hT[:, kq * 4:(kq + 1) * 4, :],
                                                        in_=self.HT[kq * 512:(kq + 1) * 512, f0:f0 + TB].rearrange("(kc p) n -> p kc n", p=128)),
                      writes=[hT.r], join=True)
                S.dma("sp", lambda h, kq=kq: h.dma_start(out=bT[:, kq * 4:(kq + 1) * 4, :],
                                                        in_=self.BRT[kq * 512:(kq + 1) * 512, f0:f0 + TB].rearrange("(kc p) n -> p kc n", p=128)),
                      writes=[bT.r], join=True)
            bg = P.sb("bg", [128, 64], F32)
            S.dma("sp", lambda h: h.dma_start(out=bg[:], in_=self.b_gate_p[l]), writes=[bg.r])
            Wg = [P.sb("Wg%d" % i, [128, 4, 16, 128], BF16) for i in range(2)]
            Wb = [P.sb("Wb%d" % i, [128, 4, 4, 128], BF16) for i in range(2)]
            sg = [P.sb("sg%d" % i, [128, 512], F32) for i in range(2)]
            tm = [P.sb("tm%d" % i, [128, 512], F32) for i in range(2)]
            acc = P.sb("acc", [128, 512], F32)
            accs = [P.sb("accs%d" % i, [128, TB], BF16) for i in range(2)]
            pg = [P.ps("pg%d" % i, [128, 512]) for i in range(3)]
            pb = [P.ps("pb%d" % i, [128, 512]) for i in range(3)]

            def loadw(j):
                wg = Wg[j % 2]
                wb = Wb[j % 2]
                for n in range(4):
                    S.dma("pool", lambda h, wg=wg, n=n, j=j: h.dma_start(
                        out=wg[:, n, :, :], in_=self.w_gate[l][n][:, j * 128:(j + 1) * 128].rearrange("(kc p) c -> p kc c", p=128)),
                        writes=[wg.r], join=(n > 0))
                    S.dma("pool", lambda h, wb=wb, n=n, j=j: h.dma_start(
                        out=wb[:, n, :, :], in_=self.w_branch[l][n][:, j * 128:(j + 1) * 128].rearrange("(kc p) c -> p kc c", p=128)),
                        writes=[wb.r], join=(n > 0))
            loadw(0)
            k = 0
            for j in range(16):
                if j + 1 < 16:
                    loadw(j + 1)
                wg = Wg[j % 2]
                wb = Wb[j % 2]
                ao = accs[j % 2]
                for tb in range((TB + 511) // 512):
                    ncw = min(512, TB - tb * 512)
                    cs = slice(tb * 512, tb * 512 + ncw)
                    for n in range(4):
                        g_ = pg[k % 3]
                        b_ = pb[k % 3]
                        s_ = sg[k % 2]
                        t_ = tm[k % 2]
                        k += 1

                        def mmg(h, g_=g_, wg=wg, n=n, cs=cs, ncw=ncw):
                            for kc in range(16):
                                ins = h.matmul(g_[:, 0:ncw], lhsT=wg[:, n, kc, :], rhs=hT[:, kc, cs], start=(kc == 0), stop=(kc == 15))
                            return ins
                        S.op("pe", mmg, reads=[wg.r, hT.r], writes=[g_.r])

                        def mmb(h, b_=b_, wb=wb, n=n, cs=cs, ncw=ncw):
                            for kc in range(4):
                                ins = h.matmul(b_[:, 0:ncw], lhsT=wb[:, n, kc, :], rhs=bT[:, n * 4 + kc, cs], start=(kc == 0), stop=(kc == 3))
                            return ins
                        S.op("pe", mmb, reads=[wb.r, bT.r], writes=[b_.r])
                        w_ = slice(0, ncw)
                        S.op("act", lambda h, g_=g_, s_=s_, n=n, j=j, w_=w_: h.activation(out=s_[:, w_], in_=g_[:, w_], func=AF.Sigmoid, bias=bg[:, n * 16 + j:n * 16 + j + 1]),
                             reads=[g_.r, bg.r], writes=[s_.r])
                        if n == 0:
                            S.op("dve", lambda h, s_=s_, b_=b_, w_=w_: h.tensor_tensor(out=acc[:, w_], in0=s_[:, w_], in1=b_[:, w_], op=ALU.mult), reads=[s_.r, b_.r], writes=[acc.r])
                        else:
                            S.op("dve", lambda h, s_=s_, b_=b_, t_=t_, w_=w_: h.tensor_tensor(out=t_[:, w_], in0=s_[:, w_], in1=b_[:, w_], op=ALU.mult), reads=[s_.r, b_.r], writes=[t_.r])
                            if n < 3:
                                S.op("pool", lambda h, t_=t_, w_=w_: h.tensor_tensor(out=acc[:, w_], in0=acc[:, w_], in1=t_[:, w_], op=ALU.add), reads=[acc.r, t_.r], writes=[acc.r])
                            else:
                                S.op("pool", lambda h, t_=t_, ao=ao, cs=cs, w_=w_: h.tensor_tensor(out=ao[:, cs], in0=acc[:, w_], in1=t_[:, w_], op=ALU.add),
                                     reads=[acc.r, t_.r], writes=[ao.r], join=(tb > 0))
                S.dma("sp", lambda h, ao=ao, j=j: h.dma_start(out=self.ACCT[j * 128:(j + 1) * 128, f0:f0 + TB], in_=ao[:]), reads=[ao.r])


Builder.p_merge1 = _p_merge1


def _p_merge2(self, l):
    S = self.S
    src = self.xin if l == 0 else self.X
    ntile = NTL if l == 0 else 32
    with self.phase() as P:
        wo = P.sb("wo", [128, 16, D], BF16)
        for kq in range(4):
            S.dma("pool", lambda h, kq=kq: h.dma_start(out=wo[:, kq * 4:(kq + 1) * 4, :],
                                                      in_=self.w_out[l][kq * 512:(kq + 1) * 512, :].rearrange("(kc p) n -> p kc n", p=128)),
                  writes=[wo.r], join=True)
        M2 = self.bc_load(P, "M2", l, 2)
        at = [P.sb("at%d" % i, [128, 16, 128], BF16) for i in range(2)]
        xt = [P.sb("xt%d" % i, [128, D], F32) for i in range(2)]
        xo = [P.sb("xo%d" % i, [128, D], F32) for i in range(2)]
        tmp = [P.sb("tmp%d" % i, [128, 512], F32) for i in range(2)]
        py = [P.ps("py%d" % i, [128, 512]) for i in range(4)]
        kk = [0]

        def stA(tt):
            a = at[tt % 2]
            x = xt[tt % 2]
            S.dma("sp", lambda h: h.dma_start(out=a[:], in_=self.ACCT[:, tt * 128:(tt + 1) * 128].rearrange("(kc p) n -> p kc n", p=128)), writes=[a.r])
            S.dma("sp", lambda h: h.dma_start(out=x[:], in_=src[tt * 128:(tt + 1) * 128, :]), writes=[x.r])

        def stB(tt):
            a = at[tt % 2]
            x = xt[tt % 2]
            o = xo[tt % 2]
            r = tile_row(tt)
            for cg in range(4):
                p = py[kk[0] % 4]
                t_ = tmp[kk[0] % 2]
                kk[0] += 1
                cs = slice(cg * 512, (cg + 1) * 512)

                def mm(h, p=p, cs=cs):
                    for kc in range(16):
                        ins = h.matmul(p[:], lhsT=a[:, kc, :], rhs=wo[:, kc, cs], start=(kc == 0), stop=(kc == 15))
                    return ins
                S.op("pe", mm, reads=[a.r, wo.r], writes=[p.r])
                S.op("dve", lambda h, p=p, t_=t_, cs=cs: h.tensor_tensor(out=t_[:], in0=p[:], in1=M2[:, r, cs], op=ALU.mult), reads=[p.r, M2.r], writes=[t_.r])
                S.op("pool", lambda h, t_=t_, cs=cs: h.tensor_tensor(out=o[:, cs], in0=x[:, cs], in1=t_[:], op=ALU.add),
                     reads=[t_.r, x.r], writes=[o.r], join=(cg > 0))
            S.dma("act", lambda h: h.dma_start(out=self.X[tt * 128:(tt + 1) * 128, :], in_=o[:]), reads=[o.r])
        pipeline(ntile, [stA, stB])


Builder.p_merge2 = _p_merge2


def _p_norm2(self, l):
    S = self.S
    ntile = NTL if l == 0 else 32
    with self.phase() as P:
        G = self.bc_load(P, "G", l, 4)
        SH = self.bc_load(P, "SH", l, 3)
        idf = P.sb("idf", [128, 128], F32)
        S.dma("sp", lambda h: h.dma_start(out=idf[:], in_=self.ident_f), writes=[idf.r])
        wr = P.sb("wr", [128, 16, 16], F32)
        S.dma("sp", lambda h: h.dma_start(out=wr[:], in_=self.w_router[l].rearrange("(kc p) e -> p kc e", p=128)), writes=[wr.r])
        xt = [P.sb("xt%d" % i, [128, D], F32) for i in range(4)]
        st = [P.sb("st%d" % i, [128, 4], F32) for i in range(4)]
        sq = [P.sb("sq%d" % i, [128, D], BF16) for i in range(2)]
        tmp = [P.sb("tmp%d" % i, [128, D], F32) for i in range(2)]
        h2 = [P.sb("h2%d" % i, [128, D], F32) for i in range(2)]
        h2b = [P.sb("h2b%d" % i, [128, D], BF16) for i in range(2)]
        h2T = [P.sb("h2T%d" % i, [128, 16, 128], F32) for i in range(2)]
        sm = [P.sb("sm%d" % i, [128, 4], F32) for i in range(2)]
        ex = [P.sb("ex%d" % i, [128, 16], F32) for i in range(2)]
        aff = [P.sb("aff%d" % i, [128, 16], F32) for i in range(2)]
        affs = P.sb("affs", [16, 2, T + LC], F32)
        pt = [P.ps("pt%d" % i, [128, 4, 128]) for i in range(4)]
        pl = [P.ps("pl%d" % i, [128, 16]) for i in range(2)]
        pa = [P.ps("pa%d" % i, [16, 128]) for i in range(2)]

        def stA(tt):
            x = xt[tt % 4]
            S.dma("sp", lambda h: h.dma_start(out=x[:], in_=self.X[tt * 128:(tt + 1) * 128, :]), writes=[x.r])
            self.norm_a(x, st[tt % 4], sq[tt % 2])

        def stA2(tt):
            self.norm_a2(st[tt % 4])

        def stB(tt):
            hh = h2[tt % 2]
            hb = h2b[tt % 2]
            self.norm_b(xt[tt % 4], st[tt % 4], G, SH, tile_row(tt), tmp[tt % 2], hh)
            S.op("act", lambda h: h.copy(out=hb[:], in_=hh[:]), reads=[hh.r], writes=[hb.r])
            S.dma("act", lambda h: h.dma_start(out=self.H2[tt * 128:(tt + 1) * 128, :], in_=hb[:]), reads=[hb.r])

        def stC(tt):
            hh = h2[tt % 2]
            hT = h2T[tt % 2]
            for q in range(4):
                p = pt[q]

                def tr(h, p=p, q=q):
                    for j in range(4):
                        kc = q * 4 + j
                        ins = h.transpose(p[:, j, :], hh[:, kc * 128:(kc + 1) * 128], idf[:])
                    return ins
                S.op("pe", tr, reads=[hh.r, idf.r], writes=[p.r])
                if q % 2 == 0:
                    S.op("act", lambda h, p=p, q=q: h.copy(out=hT[:, q * 4:(q + 1) * 4, :], in_=p[:]), reads=[p.r], writes=[hT.r], join=True)
                else:
                    S.op("dve", lambda h, p=p, q=q: h.tensor_copy(out=hT[:, q * 4:(q + 1) * 4, :], in_=p[:]), reads=[p.r], writes=[hT.r], join=True)
            p_l = pl[tt % 2]

            def mml(h):
                for kc in range(16):
                    ins = h.matmul(p_l[:], lhsT=hT[:, kc, :], rhs=wr[:, kc, :], start=(kc == 0), stop=(kc == 15))
                return ins
            S.op("pe", mml, reads=[hT.r, wr.r], writes=[p_l.r])

        def stD(tt):
            p_l = pl[tt % 2]
            p_a = pa[tt % 2]
            sm_ = sm[tt % 2]
            ex_ = ex[tt % 2]
            af_ = aff[tt % 2]
            S.op("dve", lambda h: h.reduce_max(out=sm_[:, 0:1], in_=p_l[:], axis=AX.X), reads=[p_l.r], writes=[sm_.r])
            S.op("dve", lambda h: h.tensor_scalar(out=sm_[:, 1:2], in0=sm_[:, 0:1], scalar1=-1.0, scalar2=None, op0=ALU.mult), reads=[sm_.r], writes=[sm_.r])
            S.op("act", lambda h: h.activation(out=ex_[:], in_=p_l[:], func=AF.Exp, bias=sm_[:, 1:2], accum_out=sm_[:, 2:3]), reads=[p_l.r, sm_.r], writes=[ex_.r, sm_.r])
            S.op("dve", lambda h: h.reciprocal(out=sm_[:, 3:4], in_=sm_[:, 2:3]), reads=[sm_.r], writes=[sm_.r])
            S.op("dve", lambda h: h.tensor_scalar(out=af_[:], in0=ex_[:], scalar1=sm_[:, 3:4], scalar2=None, op0=ALU.mult), reads=[ex_.r, sm_.r], writes=[af_.r])
            S.op("pe", lambda h: h.transpose(p_a[:], af_[:], idf[:]), reads=[af_.r, idf.r], writes=[p_a.r])
            if tt < 32:
                s_, c0 = tt // 16, (tt % 16) * 128
            else:
                s_, c0 = (tt - 32) // 2, T + ((tt - 32) % 2) * 128
            S.op("act", lambda h: h.copy(out=affs[:, s_, c0:c0 + 128], in_=p_a[:]), reads=[p_a.r], writes=[affs.r], join=True)
        pipeline(ntile, [stA, stA2, stB, stC, stD])
        ncol = T + LC if l == 0 else T
        for s_ in range(2):
            S.dma("sp", lambda h, s_=s_: h.dma_start(out=self.AFFT[s_ * 16:(s_ + 1) * 16, 0:ncol], in_=affs[:, s_, 0:ncol]), reads=[affs.r])


Builder.p_norm2 = _p_norm2


def _p_topk(self, l):
    S = self.S
    with self.phase() as P:
        A = P.sb("A", [32, T + LC], F32)
        W = P.sb("W", [32, T], F32)
        ncol = T + LC if l == 0 else T
        S.dma("sp", lambda h: h.dma_start(out=A[:, 0:ncol], in_=self.AFFT[:, 0:ncol]), writes=[A.r])
        idf = P.sb("idf", [128, 128], F32)
        S.dma("sp", lambda h: h.dma_start(out=idf[:], in_=self.ident_f), writes=[idf.r])
        off = P.sb("off", [32, 2], F32)
        S.dma("sp", lambda h: h.dma_start(out=off[:], in_=self.tk_off), writes=[off.r])
        vals = P.sb("vals", [32, 256], F32)
        idx = P.sb("idx", [32, 256], U32)
        idxf = P.sb("idxf", [32, 256], F32)
        cur = A
        for k in range(32):
            sl = slice(8 * k, 8 * k + 8)
            S.op("dve", lambda h, cur=cur, sl=sl: h.max(out=vals[:, sl], in_=cur[:, 0:T]), reads=[cur.r], writes=[vals.r], join=True)
            S.op("dve", lambda h, cur=cur, sl=sl: h.max_index(out=idx[:, sl], in_max=vals[:, sl], in_values=cur[:, 0:T]), reads=[cur.r, vals.r], writes=[idx.r], join=True)
            if k < 31:
                S.op("dve", lambda h, cur=cur, sl=sl: h.match_replace(out=W[:, 0:T], in_to_replace=vals[:, sl], in_values=cur[:, 0:T], imm_value=-1.0),
                     reads=[cur.r, vals.r], writes=[W.r])
                cur = W
        S.op("dve", lambda h: h.tensor_copy(out=idxf[:], in_=idx[:]), reads=[idx.r], writes=[idxf.r])
        S.op("dve", lambda h: h.tensor_scalar(out=idxf[:], in0=idxf[:], scalar1=off[:, 0:1], scalar2=None, op0=ALU.add), reads=[idxf.r, off.r], writes=[idxf.r])
        pti = P.ps("pti", [128, 2, 32])
        ptg = P.ps("ptg", [128, 2, 32])
        idxT = P.sb("idxT", [128, 2, 32], I32)
        gT = P.sb("gT", [128, 2, 32], F32)

        def tri(h):
            for hf in range(2):
                ins = h.transpose(pti[:, hf, :], idxf[:, hf * 128:(hf + 1) * 128], idf[0:32, 0:32])
            return ins
        S.op("pe", tri, reads=[idxf.r, idf.r], writes=[pti.r])

        def trg(h):
            for hf in range(2):
                ins = h.transpose(ptg[:, hf, :], vals[:, hf * 128:(hf + 1) * 128], idf[0:32, 0:32])
            return ins
        S.op("pe", trg, reads=[vals.r, idf.r], writes=[ptg.r])
        S.op("dve", lambda h: h.tensor_copy(out=idxT[:], in_=pti[:]), reads=[pti.r], writes=[idxT.r])
        idx4 = P.sb("idx4", [128, 4, 64], I32)
        for db in range(4):
            S.op("dve", lambda h, db=db: h.tensor_scalar(out=idx4[:, db, :], in0=pti[:].rearrange("p a b -> p (a b)"), scalar1=4.0, scalar2=float(db),
                                                        op0=ALU.mult, op1=ALU.add), reads=[pti.r], writes=[idx4.r], join=(db > 0))
        S.dma("sp", lambda h: h.dma_start(out=self.IDX4, in_=idx4[:].rearrange("p a b -> p (a b)")), reads=[idx4.r])
        S.op("act", lambda h: h.copy(out=gT[:], in_=ptg[:]), reads=[ptg.r], writes=[gT.r])
        S.dma("sp", lambda h: h.dma_start(out=self.IDXT, in_=idxT[:].rearrange("p a b -> p (a b)")), reads=[idxT.r])
        S.dma("sp", lambda h: h.dma_start(out=self.GT, in_=gT[:].rearrange("p a b -> p (a b)")), reads=[gT.r])
        if l == 0:
            Wc = P.sb("Wc", [32, LC], F32)
            valc = P.sb("valc", [32, 32], F32)
            idc = P.sb("idc", [32, 32], U32)
            idcf = P.sb("idcf", [32, 32], F32)
            cur = None
            for k in range(4):
                sl = slice(8 * k, 8 * k + 8)
                src = A[:, T:T + LC] if cur is None else Wc[:, :]
                rr = A.r if cur is None else Wc.r
                S.op("dve", lambda h, src=src, sl=sl: h.max(out=valc[:, sl], in_=src), reads=[rr], writes=[valc.r], join=True)
                S.op("dve", lambda h, src=src, sl=sl: h.max_index(out=idc[:, sl], in_max=valc[:, sl], in_values=src), reads=[rr, valc.r], writes=[idc.r], join=True)
                if k < 3:
                    S.op("dve", lambda h, src=src, sl=sl: h.match_replace(out=Wc[:, :], in_to_replace=valc[:, sl], in_values=src, imm_value=-1.0),
                         reads=[rr, valc.r], writes=[Wc.r])
                    cur = Wc
            S.op("dve", lambda h: h.tensor_copy(out=idcf[:], in_=idc[:]), reads=[idc.r], writes=[idcf.r])
            S.op("dve", lambda h: h.tensor_scalar(out=idcf[:], in0=idcf[:], scalar1=off[:, 1:2], scalar2=None, op0=ALU.add), reads=[idcf.r, off.r], writes=[idcf.r])
            ptc = P.ps("ptc", [32, 2, 32])
            S.op("pe", lambda h: h.transpose(ptc[:, 0, :], idcf[:], idf[0:32, 0:32]), reads=[idcf.r, idf.r], writes=[ptc.r])
            S.op("pe", lambda h: h.transpose(ptc[:, 1, :], valc[:], idf[0:32, 0:32]), reads=[valc.r, idf.r], writes=[ptc.r])
            icT = P.sb("icT", [32, 32], I32)
            gcT = P.sb("gcT", [32, 32], F32)
            S.op("dve", lambda h: h.tensor_copy(out=icT[:], in_=ptc[:, 0, :]), reads=[ptc.r], writes=[icT.r])
            ic4 = P.sb("ic4", [32, 4, 32], I32)
            for db in range(4):
                S.op("dve", lambda h, db=db: h.tensor_scalar(out=ic4[:, db, :], in0=ptc[:, 0, :], scalar1=4.0, scalar2=float(db),
                                                            op0=ALU.mult, op1=ALU.add), reads=[ptc.r], writes=[ic4.r], join=(db > 0))
            S.dma("sp", lambda h: h.dma_start(out=self.IDXC4, in_=ic4[:].rearrange("p a b -> p (a b)")), reads=[ic4.r])
            S.op("act", lambda h: h.copy(out=gcT[:], in_=ptc[:, 1, :]), reads=[ptc.r], writes=[gcT.r])
            S.dma("sp", lambda h: h.dma_start(out=self.IDXC, in_=icT[:]), reads=[icT.r])
            S.dma("sp", lambda h: h.dma_start(out=self.GC, in_=gcT[:]), reads=[gcT.r])


Builder.p_topk = _p_topk


def _p_moe(self, l):
    S = self.S
    nctx = 2 if l == 0 else 0
    NSL = 512 + 32 * nctx
    CG = [(0, 288), (288, 288)] if nctx else [(0, 512)]
    W3 = {"g": self.w_eg, "u": self.w_eu, "d": self.w_ed}
    with self.phase() as P:
        idb = P.sb("idb", [128, 128], BF16)
        S.dma("sp", lambda h: h.dma_start(out=idb[:], in_=self.ident_b), writes=[idb.r])
        idxT = P.sb("idxT", [128, 64], I32)
        gT = P.sb("gT", [128, 64], F32)
        S.dma("sp", lambda h: h.dma_start(out=idxT[:], in_=self.IDXT), writes=[idxT.r])
        S.dma("sp", lambda h: h.dma_start(out=gT[:], in_=self.GT), writes=[gT.r])
        idx4 = P.sb("idx4", [128, 4 * 64], I32)
        S.dma("sp", lambda h: h.dma_start(out=idx4[:], in_=self.IDX4), writes=[idx4.r])
        X4 = self.X.rearrange("n (b c) -> (n b) c", c=512)
        icT = gcT = ic4 = None
        if nctx:
            ic4 = P.sb("ic4", [64, 4, 16], I32)
            icT = P.sb("icT", [64, 16], I32)
            gcT = P.sb("gcT", [64, 16], F32)
            i4v = self.IDXC4.rearrange("p (d c) -> p d c", d=4)
            for s_ in range(2):
                S.dma("sp", lambda h, s_=s_: h.dma_start(out=ic4[s_ * 32:(s_ + 1) * 32, :, :], in_=i4v[:, :, s_ * 16:(s_ + 1) * 16]), writes=[ic4.r], join=True)
                S.dma("sp", lambda h, s_=s_: h.dma_start(out=icT[s_ * 32:(s_ + 1) * 32, :], in_=self.IDXC[:, s_ * 16:(s_ + 1) * 16]), writes=[icT.r], join=True)
                S.dma("sp", lambda h, s_=s_: h.dma_start(out=gcT[s_ * 32:(s_ + 1) * 32, :], in_=self.GC[:, s_ * 16:(s_ + 1) * 16]), writes=[gcT.r], join=True)
        M5 = self.bc_load(P, "M5", l, 5)
        ntl = 4 + (1 if nctx else 0)
        xe = [P.sb("xe%d" % i, [128, D], BF16) for i in range(ntl)]
        xeT = [P.sb("xeT%d" % i, [128, 16, NSL], BF16) for i in range(2)]
        hidT = P.sb("hidT", [128, 16, NSL], BF16)
        Wb = [P.sb("Wb%d" % i, [128, 16, 512], BF16) for i in range(4)]
        sgt = [P.sb("sgt%d" % i, [128, NSL], F32) for i in range(2)]
        ye = [P.sb("ye%d" % i, [128, 512], F32) for i in range(6)]
        ptr = [P.ps("ptr%d" % i, [128, 8, 128], BF16) for i in range(2)]
        pg = P.ps("pg", [128, 2, 512])
        pu = P.ps("pu", [128, 2, 512])
        py = [P.ps("py%d" % i, [128, 512]) for i in range(2)]
        xres = [Res() for _ in range(4)]
        blocks = []
        for e in range(16):
            for fb in range(4):
                blocks += [(e, "g", fb), (e, "u", fb)]
            for db in range(4):
                blocks += [(e, "d", db)]
        issued = [0]

        def issue_upto(i):
            while issued[0] <= min(i, len(blocks) - 1):
                k = issued[0]
                e, kind, ix = blocks[k]
                w = Wb[k % 4]
                S.dma("pool", lambda h, w=w, e=e, kind=kind, ix=ix: h.dma_start(
                    out=w[:], in_=W3[kind][l][e][:, ix * 512:(ix + 1) * 512].rearrange("(kc p) f -> p kc f", p=128)), writes=[w.r])
                issued[0] += 1

        def tiles_of(e):
            tl = [(s, hf, 128, s * 256 + hf * 128, idxT, hf * 32 + s * 16 + e) for s in range(2) for hf in range(2)]
            if nctx:
                tl.append((2, None, 64, 512, icT, e))
            return tl

        def gather(e):
            for ti, (s, hf, M, c0, it, col) in enumerate(tiles_of(e)):
                x_ = xe[ti]
                S.dma("pool", lambda h, x_=x_, it=it, col=col, M=M: h.indirect_dma_start(
                    out=x_[0:M, :], out_offset=None, in_=self.H2[:, :], in_offset=bass.IndirectOffsetOnAxis(ap=it[0:M, col:col + 1], axis=0)),
                    reads=[it.r], writes=[x_.r])

        def transposes(e):
            xT = xeT[e % 2]
            for ti, (s, hf, M, c0, it, col) in enumerate(tiles_of(e)):
                x_ = xe[ti]
                for half in range(2):
                    p = ptr[half]

                    def tr(h, p=p, x_=x_, half=half, M=M):
                        for j in range(8):
                            kc = half * 8 + j
                            ins = h.transpose(p[:, j, 0:M], x_[0:M, kc * 128:(kc + 1) * 128], idb[0:M, 0:M])
                        return ins
                    S.op("pe", tr, reads=[x_.r, idb.r], writes=[p.r])
                    if half == 0:
                        S.op("act", lambda h, p=p, c0=c0, M=M, xT=xT: h.copy(out=xT[:, 0:8, c0:c0 + M], in_=p[:, :, 0:M]), reads=[p.r], writes=[xT.r], join=True)
                    else:
                        S.op("dve", lambda h, p=p, c0=c0, M=M, xT=xT: h.tensor_copy(out=xT[:, 8:16, c0:c0 + M], in_=p[:, :, 0:M]), reads=[p.r], writes=[xT.r], join=True)

        bi = 0
        yk = 0
        issue_upto(1)
        gather(0)
        transposes(0)
        for e in range(16):
            xT = xeT[e % 2]
            tiles = tiles_of(e)
            for fb in range(4):
                issue_upto(bi + 3)
                if fb == 2 and e + 1 < 16:
                    gather(e + 1)
                wg = Wb[bi % 4]
                wu = Wb[(bi + 1) % 4]
                bi += 2
                for fc in range(4):
                    F = fb * 4 + fc
                    sg_ = sgt[F % 2]

                    def mmgu(h, wt, pp, fc=fc, xT=xT):
                        for gi, (c0, n) in enumerate(CG):
                            for kc in range(16):
                                ins = h.matmul(pp[:, gi, 0:n], lhsT=wt[:, kc, fc * 128:(fc + 1) * 128], rhs=xT[:, kc, c0:c0 + n], start=(kc == 0), stop=(kc == 15))
                        return ins
                    S.op("pe", lambda h, wg=wg, fc=fc, mmgu=mmgu: mmgu(h, wg, pg, fc), reads=[wg.r, xT.r], writes=[pg.r])
                    S.op("pe", lambda h, wu=wu, fc=fc, mmgu=mmgu: mmgu(h, wu, pu, fc), reads=[wu.r, xT.r], writes=[pu.r])
                    for gi, (c0, n) in enumerate(CG):
                        S.op("act", lambda h, sg_=sg_, gi=gi, c0=c0, n=n: h.activation(out=sg_[:, c0:c0 + n], in_=pg[:, gi, 0:n], func=AF.Silu),
                             reads=[pg.r], writes=[sg_.r], join=(gi > 0))
                    for gi, (c0, n) in enumerate(CG):
                        S.op("dve", lambda h, sg_=sg_, F=F, gi=gi, c0=c0, n=n: h.tensor_tensor(out=hidT[:, F, c0:c0 + n], in0=sg_[:, c0:c0 + n], in1=pu[:, gi, 0:n], op=ALU.mult),
                             reads=[sg_.r, pu.r], writes=[hidT.r], join=True)
            if e + 1 < 16:
                transposes(e + 1)
            for db in range(4):
                issue_upto(bi + 2)
                wd = Wb[bi % 4]
                bi += 1
                dsl = slice(db * 512, (db + 1) * 512)
                for ti_, (s, hf, M, c0, it, col) in enumerate(tiles):
                    p = py[yk % 2]
                    y_ = ye[yk % 6]
                    yk += 1

                    def mmd(h, p=p, wd=wd, c0=c0, M=M):
                        for fc in range(16):
                            ins = h.matmul(p[0:M, :], lhsT=hidT[:, fc, c0:c0 + M], rhs=wd[:, fc, :], start=(fc == 0), stop=(fc == 15))
                        return ins
                    S.op("pe", mmd, reads=[hidT.r, wd.r], writes=[p.r])
                    gt_ = gT if hf is not None else gcT
                    r = s if hf is not None else 2
                    S.op("dve", lambda h, p=p, y_=y_, gt_=gt_, col=col, M=M, r=r, dsl=dsl: h.scalar_tensor_tensor(
                        out=y_[0:M, :], in0=p[0:M, :], scalar=gt_[0:M, col:col + 1], in1=M5[0:M, r, dsl], op0=ALU.mult, op1=ALU.mult),
                        reads=[p.r, gt_.r, M5.r], writes=[y_.r])
                    if hf is not None:
                        i4ap = idx4[0:M, db * 64 + col:db * 64 + col + 1]
                        i4 = idx4
                    else:
                        i4ap = ic4[0:M, db, col:col + 1]
                        i4 = ic4
                    S.dma("pool", lambda h, y_=y_, i4ap=i4ap, M=M: h.indirect_dma_start(
                        out=X4[:, :], out_offset=bass.IndirectOffsetOnAxis(ap=i4ap, axis=0),
                        in_=y_[0:M, :], in_offset=None, compute_op=ALU.add),
                        reads=[y_.r, i4.r], writes=[xres[db]], join=(ti_ > 0))


Builder.p_moe = _p_moe


def _p_final(self):
    S = self.S
    with self.phase() as P:
        g = P.sb("g", [128, D], F32)
        S.dma("sp", lambda h: h.dma_start(out=g[:], in_=self.fing.broadcast_to([128, D])), writes=[g.r])
        xt = [P.sb("xt%d" % i, [128, D], F32) for i in range(4)]
        ot = [P.sb("ot%d" % i, [128, D], F32) for i in range(2)]
        st = [P.sb("st%d" % i, [128, 4], F32) for i in range(4)]
        sq = [P.sb("sq%d" % i, [128, D], BF16) for i in range(2)]

        def stA(tt):
            x = xt[tt % 4]
            S.dma("sp", lambda h: h.dma_start(out=x[:], in_=self.X[tt * 128:(tt + 1) * 128, :]), writes=[x.r])
            self.norm_a(x, st[tt % 4], sq[tt % 2])

        def stA2(tt):
            self.norm_a2(st[tt % 4])

        def stB(tt):
            x = xt[tt % 4]
            o = ot[tt % 2]
            s_ = st[tt % 4]
            S.op("dve", lambda h: h.scalar_tensor_tensor(out=o[:], in0=x[:], scalar=s_[:, 2:3], in1=g[:], op0=ALU.mult, op1=ALU.mult),
                 reads=[x.r, s_.r, g.r], writes=[o.r])
            S.dma("act", lambda h: h.dma_start(out=self.out[tt * 128:(tt + 1) * 128, :], in_=o[:]), reads=[o.r])
        pipeline(32, [stA, stA2, stB])


Builder.p_final = _p_final


def _p_dbg_copy(self):
    S = self.S
    xi = self.nc.dram_tensor("Xinit", [NF, D], F32, kind="ExternalInput").ap()
    self.inputs["Xinit"] = ([NF, D], F32)
    with self.phase() as P:
        for i in range(9):
            S.dma("sp", lambda h, i=i: h.dma_start(out=self.X[i * 512:(i + 1) * 512, :], in_=xi[i * 512:(i + 1) * 512, :]))


Builder.p_dbg_copy = _p_dbg_copy


def build_full(B):
    for l in range(DEPTH):
        B.p_mod(l)
        B.p_norm1(l)
        B.p_proj(l)
        B.p_gla(l)
        B.p_swa(l)
        B.p_na(l)
        B.p_sgu(l)
        B.p_merge1(l)
        B.p_merge2(l)
        B.p_norm2(l)
        B.p_topk(l)
        B.p_moe(l)
    B.p_final()


_CACHE = {}


def kernel(**inputs):
    I = {k: np.asarray(v) for k, v in inputs.items()}
    if "B" not in _CACHE:
        B = Builder()
        build_full(B)
        _CACHE["B"] = B
    B = _CACHE["B"]
    sh = _shared_inputs(I)
    in_maps = []
    for c in range(NCORES):
        m = _core_inputs(I, c, sh)
        in_maps.append({k: m[k] for k in B.inputs})
    res = run_bass_kernel_spmd(B.nc, in_maps, core_ids=list(range(NCORES)))
    out = np.empty((2 * NCORES, T, D), np.float32)
    for c in range(NCORES):
        out[2 * c:2 * c + 2] = np.asarray(res.results[c]["out"], dtype=np.float32).reshape(2, T, D)
    return out
```
